# Optimizing a Trainium2 kernel written in Bass

```python
import math
import jax, jax.numpy as jnp
from jax import lax
import numpy as np

D_MODEL = 1024
BATCH = 4
SEQ = 4096
DEPTH = 4

HEAD_DIM = 64
DIL_GROUPS = ((128, 1), (512, 4), (2048, 16))
A_HEADS_PER_GROUP = 4
A_HEADS = A_HEADS_PER_GROUP * len(DIL_GROUPS)
A_OUT = A_HEADS_PER_GROUP * HEAD_DIM
B_HEADS = 4
B_V_DIM = 2 * HEAD_DIM
B_OUT = B_HEADS * B_V_DIM
C_HEADS = 4
C_OUT = C_HEADS * HEAD_DIM
IDX_HEADS = 8
IDX_DIM = 64
TOPK_MAX = 256
D_FF = 2816
NUM_BUCKETS = 32
MAX_DISTANCE = 2048
BIAS_HEADS = A_HEADS + B_HEADS + C_HEADS
Q_BLOCK = 128
RMS_EPS = 1e-6

A_QKV_COLS = 3 * A_HEADS * HEAD_DIM
B_QK_COLS = 4 * B_HEADS * HEAD_DIM
B_V_COLS = B_HEADS * B_V_DIM
C_COLS = C_HEADS * HEAD_DIM + 2 * HEAD_DIM
IDX_COLS = IDX_HEADS * IDX_DIM + IDX_DIM + IDX_HEADS
GATE_COLS = 3 * D_MODEL
N_IN = A_QKV_COLS + B_QK_COLS + B_V_COLS + C_COLS + IDX_COLS + GATE_COLS
SPLIT_POINTS = [A_QKV_COLS,
                A_QKV_COLS + B_QK_COLS,
                A_QKV_COLS + B_QK_COLS + B_V_COLS,
                A_QKV_COLS + B_QK_COLS + B_V_COLS + C_COLS,
                A_QKV_COLS + B_QK_COLS + B_V_COLS + C_COLS + IDX_COLS]

kernel_name = "hybrid_gated_dilated_diff_dsa_macaron"


def rms_norm(x, g):
    xf = x.astype(jnp.float32)
    y = xf * lax.rsqrt(jnp.mean(xf * xf, axis=-1, keepdims=True) + RMS_EPS)
    return (y * g.astype(jnp.float32)).astype(x.dtype)


def swiglu(x, w_gate, w_up, w_down):
    return (jax.nn.silu(x @ w_gate) * (x @ w_up)) @ w_down


def rel_bucket(dist):
    n = jnp.maximum(dist, 0)
    max_exact = NUM_BUCKETS // 2
    nf = jnp.maximum(n, 1).astype(jnp.float32)
    large = max_exact + (jnp.log(nf / max_exact) / math.log(MAX_DISTANCE / max_exact)
                         * (NUM_BUCKETS - max_exact)).astype(jnp.int32)
    large = jnp.minimum(large, NUM_BUCKETS - 1)
    return jnp.where(n < max_exact, n, large)


def dilated_window_group(q, k, v, window, dilation, bias_table):
    b, t, h, dh = q.shape
    n = t // dilation
    wn = window // dilation
    nb = -(-n // wn)
    pad = nb * wn - n

    def to_sub(z):
        z = z.reshape(b, n, dilation, h, dh).transpose(0, 2, 1, 3, 4)
        return jnp.pad(z, ((0, 0), (0, 0), (0, pad), (0, 0), (0, 0)))

    qs, ks, vs = to_sub(q), to_sub(k), to_sub(v)
    qb = qs.reshape(b, dilation, nb, wn, h, dh)

    def band(z):
        zp = jnp.pad(z, ((0, 0), (0, 0), (wn, 0), (0, 0), (0, 0))).reshape(b, dilation, nb + 1, wn, h, dh)
        return jnp.concatenate([zp[:, :, :-1], zp[:, :, 1:]], axis=3)

    kb, vb = band(ks), band(vs)
    logits = jnp.einsum('brnqhe,brnkhe->brnhqk', qb, kb).astype(jnp.float32) * (dh ** -0.5)
    a = jnp.arange(wn)[:, None]
    c = jnp.arange(2 * wn)[None, :]
    sub_dist = wn + a - c
    bias = bias_table[rel_bucket(sub_dist * dilation)].astype(jnp.float32)
    logits = logits + bias.transpose(2, 0, 1)
    key_pos = (jnp.arange(nb)[:, None] - 1) * wn + c
    mask = ((sub_dist >= 0) & (sub_dist <= wn))[None] & (key_pos >= 0)[:, None, :]
    logits = jnp.where(mask[:, None], logits, -jnp.inf)
    m = jnp.max(logits, axis=-1)
    p = jnp.exp(logits - m[..., None])
    s = jnp.sum(p, axis=-1)
    o = jnp.einsum('brnhqk,brnkhe->brnqhe', p, vb.astype(jnp.float32))

    def from_sub(z):
        z = z.reshape((b, dilation, nb * wn) + z.shape[4:])[:, :, :n]
        z = jnp.moveaxis(z, 1, 2)
        return z.reshape((b, t) + z.shape[3:])

    return from_sub(o), from_sub(jnp.swapaxes(m, 3, 4)), from_sub(jnp.swapaxes(s, 3, 4))


def dilated_attention(q, k, v, bias_table):
    outs = []
    for g, (w, d) in enumerate(DIL_GROUPS):
        sl = slice(g * A_HEADS_PER_GROUP, (g + 1) * A_HEADS_PER_GROUP)
        outs.append(dilated_window_group(q[:, :, sl], k[:, :, sl], v[:, :, sl], w, d, bias_table[:, sl]))
    m_all = jnp.stack([m for _, m, _ in outs])
    scale = jnp.exp(m_all - jnp.max(m_all, axis=0))
    num = sum(scale[g][..., None] * outs[g][0] for g in range(len(DIL_GROUPS)))
    den = sum(scale[g] * outs[g][2] for g in range(len(DIL_GROUPS)))
    return num / den[..., None]


def diff_attention(qs, ks, v, lam, bias_table):
    t = qs.shape[1]
    dh = qs.shape[-1]
    outs = []
    for start in range(0, t, Q_BLOCK):
        end = start + Q_BLOCK
        logits = jnp.einsum('bqmhe,bkmhe->bmhqk', qs[:, start:end], ks[:, :end]).astype(jnp.float32) * (dh ** -0.5)
        dist = jnp.arange(start, end)[:, None] - jnp.arange(end)[None, :]
        bias = bias_table[rel_bucket(dist)].astype(jnp.float32).transpose(2, 0, 1)
        logits = jnp.where(dist >= 0, logits + bias, -jnp.inf)
        p = jax.nn.softmax(logits, axis=-1)
        w = p[:, 0] - lam * p[:, 1]
        outs.append(jnp.einsum('bhqk,bkhe->bqhe', w, v[:, :end].astype(jnp.float32)))
    return jnp.concatenate(outs, axis=1)


def dsa_attention(q, k, v, q_idx, k_idx, w_idx, bias_table):
    t = q.shape[1]
    dh = q.shape[-1]
    k_sel = min(TOPK_MAX, t // 4)
    gather = jax.vmap(lambda z, i: z[i])
    outs = []
    for start in range(0, t, Q_BLOCK):
        end = start + Q_BLOCK
        n_sel = min(k_sel, end)
        qpos = jnp.arange(start, end)
        raw = jnp.einsum('bqhe,bke->bqhk', q_idx[:, start:end], k_idx[:, :end]).astype(jnp.float32) * (IDX_DIM ** -0.5)
        score = jnp.einsum('bqh,bqhk->bqk', w_idx[:, start:end].astype(jnp.float32) * (IDX_HEADS ** -0.5),
                           jax.nn.relu(raw))
        score = jnp.where(jnp.arange(end)[None, :] <= qpos[:, None], score, -jnp.inf)
        top_val, top_idx = lax.top_k(score, n_sel)
        valid = jnp.isfinite(top_val)
        k_g = gather(k[:, :end], top_idx)
        v_g = gather(v[:, :end], top_idx)
        logits = jnp.einsum('bqhe,bqne->bhqn', q[:, start:end], k_g).astype(jnp.float32) * (dh ** -0.5)
        bias = bias_table[rel_bucket(qpos[None, :, None] - top_idx)].astype(jnp.float32)
        logits = jnp.where(valid[:, None], logits + jnp.moveaxis(bias, 3, 1), -jnp.inf)
        p = jax.nn.softmax(logits, axis=-1)
        outs.append(jnp.einsum('bhqn,bqne->bqhe', p, v_g.astype(jnp.float32)))
    return jnp.concatenate(outs, axis=1)


def token_mixer(h, w_in, qk_gain, diff_lambda, diff_out_norm, w_branch_a, w_branch_b, w_branch_c,
                w_out, rel_bias, layer_idx):
    b, t, _ = h.shape
    proj = h @ w_in
    a_qkv, b_qk, b_v, c_qkv, ix, gates = jnp.split(proj, SPLIT_POINTS, axis=-1)

    a_qkv = a_qkv.reshape(b, t, 3, A_HEADS, HEAD_DIM)
    qa = rms_norm(a_qkv[:, :, 0], qk_gain[0, 0])
    ka = rms_norm(a_qkv[:, :, 1], qk_gain[0, 1])
    o_a = dilated_attention(qa, ka, a_qkv[:, :, 2], rel_bias[:, :A_HEADS])
    o_a = o_a.reshape(b, t, A_OUT).astype(h.dtype)

    b_qk = b_qk.reshape(b, t, 4, B_HEADS, HEAD_DIM)
    qb = rms_norm(b_qk[:, :, 0:2], qk_gain[1, 0])
    kb = rms_norm(b_qk[:, :, 2:4], qk_gain[1, 1])
    lam_init = 0.8 - 0.6 * math.exp(-0.3 * layer_idx)
    lv = diff_lambda.astype(jnp.float32)
    lam = jnp.exp(jnp.sum(lv[0] * lv[1])) - jnp.exp(jnp.sum(lv[2] * lv[3])) + lam_init
    o_b = diff_attention(qb, kb, b_v.reshape(b, t, B_HEADS, B_V_DIM), lam, rel_bias[:, A_HEADS:A_HEADS + B_HEADS])
    o_b = rms_norm(o_b, diff_out_norm) * (1.0 - lam_init)
    o_b = o_b.reshape(b, t, B_OUT).astype(h.dtype)

    qc = rms_norm(c_qkv[..., :C_HEADS * HEAD_DIM].reshape(b, t, C_HEADS, HEAD_DIM), qk_gain[2, 0])
    kc = rms_norm(c_qkv[..., C_HEADS * HEAD_DIM:C_HEADS * HEAD_DIM + HEAD_DIM], qk_gain[2, 1])
    vc = c_qkv[..., C_HEADS * HEAD_DIM + HEAD_DIM:]
    q_idx = ix[..., :IDX_HEADS * IDX_DIM].reshape(b, t, IDX_HEADS, IDX_DIM)
    k_idx = ix[..., IDX_HEADS * IDX_DIM:IDX_HEADS * IDX_DIM + IDX_DIM]
    w_idx = ix[..., IDX_HEADS * IDX_DIM + IDX_DIM:]
    o_c = dsa_attention(qc, kc, vc, q_idx, k_idx, w_idx, rel_bias[:, A_HEADS + B_HEADS:])
    o_c = o_c.reshape(b, t, C_OUT).astype(h.dtype)

    g = jax.nn.sigmoid(gates.reshape(b, t, 3, D_MODEL))
    y = (g[:, :, 0] * (o_a @ w_branch_a) + g[:, :, 1] * (o_b @ w_branch_b)
         + g[:, :, 2] * (o_c @ w_branch_c))
    return y @ w_out


def setup_inputs(seed: int = 0) -> dict:
    key = jax.random.key(seed)
    ks = jax.random.split(key, 20)
    f32 = jnp.float32

    def dense(k, shape, fan_in):
        return jax.random.normal(k, shape, f32) * fan_in ** -0.5

    def gain(k, shape):
        return 1.0 + 0.05 * jax.random.normal(k, shape, f32)

    return {
        "x": jax.random.normal(ks[0], (BATCH, SEQ, D_MODEL), f32),
        "rel_bias": 0.5 * jax.random.normal(ks[1], (NUM_BUCKETS, BIAS_HEADS), f32),
        "ffn1_norm": gain(ks[2], (DEPTH, D_MODEL)),
        "ffn1_w_gate": dense(ks[3], (DEPTH, D_MODEL, D_FF), D_MODEL),
        "ffn1_w_up": dense(ks[4], (DEPTH, D_MODEL, D_FF), D_MODEL),
        "ffn1_w_down": dense(ks[5], (DEPTH, D_FF, D_MODEL), D_FF),
        "mix_norm": gain(ks[6], (DEPTH, D_MODEL)),
        "w_in": dense(ks[7], (DEPTH, D_MODEL, N_IN), D_MODEL),
        "qk_gain": gain(ks[8], (DEPTH, 3, 2, HEAD_DIM)),
        "diff_lambda": 0.1 * jax.random.normal(ks[9], (DEPTH, 4, HEAD_DIM), f32),
        "diff_out_norm": gain(ks[10], (DEPTH, B_V_DIM)),
        "w_branch_a": dense(ks[11], (DEPTH, A_OUT, D_MODEL), A_OUT),
        "w_branch_b": dense(ks[12], (DEPTH, B_OUT, D_MODEL), B_OUT),
        "w_branch_c": dense(ks[13], (DEPTH, C_OUT, D_MODEL), C_OUT),
        "w_out": dense(ks[14], (DEPTH, D_MODEL, D_MODEL), D_MODEL),
        "ffn2_norm": gain(ks[15], (DEPTH, D_MODEL)),
        "ffn2_w_gate": dense(ks[16], (DEPTH, D_MODEL, D_FF), D_MODEL),
        "ffn2_w_up": dense(ks[17], (DEPTH, D_MODEL, D_FF), D_MODEL),
        "ffn2_w_down": dense(ks[18], (DEPTH, D_FF, D_MODEL), D_FF),
    }


def reference(x, rel_bias, ffn1_norm, ffn1_w_gate, ffn1_w_up, ffn1_w_down, mix_norm, w_in, qk_gain,
              diff_lambda, diff_out_norm, w_branch_a, w_branch_b, w_branch_c, w_out,
              ffn2_norm, ffn2_w_gate, ffn2_w_up, ffn2_w_down):
    for i in range(DEPTH):
        x = x + 0.5 * swiglu(rms_norm(x, ffn1_norm[i]), ffn1_w_gate[i], ffn1_w_up[i], ffn1_w_down[i])
        x = x + token_mixer(rms_norm(x, mix_norm[i]), w_in[i], qk_gain[i], diff_lambda[i], diff_out_norm[i],
                            w_branch_a[i], w_branch_b[i], w_branch_c[i], w_out[i], rel_bias, i)
        x = x + 0.5 * swiglu(rms_norm(x, ffn2_norm[i]), ffn2_w_gate[i], ffn2_w_up[i], ffn2_w_down[i])
    return x
```

```python
import math
from contextlib import ExitStack
import numpy as np
import concourse.bass as bass
import concourse.mybir as mybir
from concourse.bass_utils import run_bass_kernel_spmd

F32 = mybir.dt.float32
BF16 = mybir.dt.bfloat16
AF = mybir.ActivationFunctionType
ALU = mybir.AluOpType
AX = mybir.AxisListType

D = 1024
DFF = 2816
NF = DFF // 128
NIN = 7880
EPS = 1e-6
BIG = 30000.0
LV = 3840
WD = 3712
VOFF = 511
NKIND = 4
DIL = ((128, 1), (512, 4), (2048, 16))
R_DMA = 8
MIX = "abc"


class Buf:
    __slots__ = ("w", "r")

    def __init__(self):
        self.w = None
        self.r = {}


class Eng:
    def __init__(self, name, h, sem, same):
        self.name = name
        self.h = h
        self.sem = sem
        self.cnt = 0
        self.seen = {}
        self.same = same


class Sched:
    def __init__(self, nc, es):
        self.nc = nc
        mk = lambda n: es.enter_context(nc.semaphore(n))
        self.pe = Eng("pe", nc.tensor, mk("s_pe"), False)
        self.act = Eng("act", nc.scalar, mk("s_act"), True)
        self.dve = Eng("dve", nc.vector, mk("s_dve"), True)
        self.pool = Eng("pool", nc.gpsimd, mk("s_pool"), True)
        self.sp = Eng("sp", nc.sync, mk("s_sp"), False)
        self.engs = [self.pe, self.act, self.dve, self.pool, self.sp]
        self.dq = {}
        for e in (self.sp, self.pool):
            self.dq[e.name] = {"sems": [mk(f"d_{e.name}{i}") for i in range(R_DMA)], "vals": [0] * R_DMA, "i": 0}
        self.n_ins = 0
        self.cc_sem = mk("cc_sem")
        self.cc_val = 0

    def _deps(self, reads, writes):
        d = {}

        def add(t):
            if t is None:
                return
            k, s, v = t
            if k not in d or d[k][1] < v:
                d[k] = (s, v)

        for b in reads:
            add(b.w)
        for b in writes:
            add(b.w)
            for t in b.r.values():
                add(t)
        return d

    def _filter(self, eng, deps):
        out = []
        for key, (sem, val) in deps.items():
            if key == eng.name and not eng.same:
                continue
            if eng.seen.get(key, 0) >= val:
                continue
            eng.seen[key] = val
            out.append((key, sem, val))
        out.sort(key=lambda t: 0 if t[0] == eng.name else 1)
        return out

    def _emit(self, eng, fn, waits):
        for (_, sem, val) in waits[1:]:
            eng.h.wait_ge(sem, val)
        ins = fn(eng.h)
        if waits:
            ins._wait_ge(waits[0][1], waits[0][2])
        self.n_ins += 1 + max(0, len(waits) - 1)
        return ins

    def op(self, eng, fn, reads=(), writes=()):
        waits = self._filter(eng, self._deps(reads, writes))
        ins = self._emit(eng, fn, waits)
        ins.then_inc(eng.sem, 1)
        eng.cnt += 1
        tok = (eng.name, eng.sem, eng.cnt)
        for b in reads:
            b.r[eng.name] = tok
        for b in writes:
            b.w = tok
            b.r = {}
        return tok

    def dma(self, eng, out, in_, reads=(), writes=(), **kw):
        q = self.dq[eng.name]
        i = q["i"]
        q["i"] = (i + 1) % R_DMA
        sem = q["sems"][i]
        key = f"d_{eng.name}{i}"
        deps = self._deps(reads, writes)
        if q["vals"][i] > 0:
            deps[key] = (sem, q["vals"][i])
        waits = self._filter(eng, deps)
        ins = self._emit(eng, lambda h: h.dma_start(out=out, in_=in_, **kw), waits)
        q["vals"][i] += 16
        ins.then_inc(sem, 16)
        tok = (key, sem, q["vals"][i])
        for b in reads:
            b.r[key] = tok
        for b in writes:
            b.w = tok
            b.r = {}
        return tok

    def drain(self, eng):
        for e in self.engs:
            if e.cnt > 0 and eng.seen.get(e.name, 0) < e.cnt and e is not eng:
                eng.h.wait_ge(e.sem, e.cnt)
                eng.seen[e.name] = e.cnt
        if self.cc_val > 0 and eng.seen.get("cc", 0) < self.cc_val:
            eng.h.wait_ge(self.cc_sem, self.cc_val)
            eng.seen["cc"] = self.cc_val
        for qn, q in self.dq.items():
            for i in range(R_DMA):
                key = f"d_{qn}{i}"
                if q["vals"][i] > 0 and eng.seen.get(key, 0) < q["vals"][i]:
                    eng.h.wait_ge(q["sems"][i], q["vals"][i])
                    eng.seen[key] = q["vals"][i]

    def barrier(self):
        for e in self.engs:
            self.drain(e)


def gpos(gs):
    r = 0 if gs % 4 in (0, 3) else 1
    li = gs // 2
    return r, li


def build(T, L, parts, fused, lbase=0, ncores=8):
    NT = T // 2
    NS = T // 512
    NM = NS // 2
    NTT = NT // 512
    nc = bass.Bass("TRN2", target_bir_lowering=False)

    def din(name, shape, dt=F32):
        return nc.dram_tensor(name, list(shape), dt, kind="ExternalInput").ap()

    def dout(name, shape, dt=F32):
        return nc.dram_tensor(name, list(shape), dt, kind="ExternalOutput").ap()

    def dscr(name, shape, dt, io):
        if fused:
            return nc.dram_tensor(name, list(shape), dt, kind="Internal").ap()
        return nc.dram_tensor(name, list(shape), dt, kind="ExternalInput" if io == "in" else "ExternalOutput").ap()

    has_a = any(p[0] == "a" for p in parts)
    has_b = any(p[0] == "b" for p in parts)
    first_is_b = parts[0][0] == "b"
    last_is_a = parts[-1][0] == "a"

    xT_in = din("xT_in", [D, NT])
    xT_out = dout("xT_out", [D, NT])
    consts = din("consts", [4, 128, 128])
    normsT = din("normsT", [128, L * 24])
    qkg = din("qkg", [128, L * 6])
    dnorm = din("dnorm", [128, L])
    dlam = din("dlam", [128, L * 256])
    w_gate = [din("ffn1_w_gate", [L, D, DFF]) if has_a else None, din("ffn2_w_gate", [L, D, DFF]) if has_b else None]
    w_up = [din("ffn1_w_up", [L, D, DFF]) if has_a else None, din("ffn2_w_up", [L, D, DFF]) if has_b else None]
    w_down = [din("ffn1_w_down", [L, DFF, D]) if has_a else None, din("ffn2_w_down", [L, DFF, D]) if has_b else None]
    w_in = din("w_in", [L, D, NIN])
    if has_b:
        w_br = [din("w_branch_a", [L, 256, D]), din("w_branch_b", [L, 512, D]), din("w_branch_c", [L, 256, D])]
        w_out = din("w_out", [L, D, D])
        rel_bias = din("rel_bias", [32, 20])
        onehot = din("onehot", [NKIND * 2, 33, LV])
        cm_in = din("cm", [2, 128, 1024])

    NQC = 16
    NKC = 12
    VW = 1344
    if fused:
        hT_s = dscr("hT_s", [8 * 128, NT], BF16, None)
        QT_s = dscr("QT_s", [NQC * 128, NT], BF16, None)
        KT_l2 = [dscr(f"KT_l{i}", [NKC * 128, NT], BF16, None) for i in range(2)]
        V_l2 = [dscr(f"V_l{i}", [NT, VW], BF16, None) for i in range(2)]
        KT_f2 = [dscr(f"KT_f{i}", [2 * NKC * 128, NT], BF16, None) for i in range(2)]
        V_f2 = [dscr(f"V_f{i}", [2 * NT, VW], BF16, None) for i in range(2)]
        KT_l, V_l, KT_f, V_f = KT_l2[0], V_l2[0], KT_f2[0], V_f2[0]
        hT_si = hT_s
        QT_si = QT_s
    else:
        if last_is_a:
            hT_s = dscr("hT_o", [8 * 128, NT], BF16, "out")
            QT_s = dscr("QT_o", [NQC * 128, NT], BF16, "out")
            KT_l = dscr("KT_o", [NKC * 128, NT], BF16, "out")
            V_l = dscr("V_o", [NT, VW], BF16, "out")
            widx_o = dscr("widx_o", [128, NT // 128 * 8], F32, "out")
        if first_is_b:
            hT_si = dscr("hT_i", [8 * 128, NT], BF16, "in")
            QT_si = dscr("QT_i", [NQC * 128, NT], BF16, "in")
            KT_f = dscr("KT_f", [2 * NKC * 128, NT], BF16, "in")
            V_f = dscr("V_f", [2 * NT, VW], BF16, "in")
            widx_i = dscr("widx_i", [128, NT // 128 * 8], F32, "in")
    vec_s = nc.dram_tensor("vec_s", [NKIND * 2 * 20, LV], BF16, kind="Internal").ap()

    es = ExitStack()
    uid = [0]
    with es:
        S = Sched(nc, es)
        pe, act, dve, pool, sp = S.pe, S.act, S.dve, S.pool, S.sp

        def sb(name, shape, dt):
            return es.enter_context(nc.sbuf_tensor(name, list(shape), dt))

        xT = sb("xT", [128, 8, NT], F32)
        XB = [Buf() for _ in range(NTT)]
        ps = es.enter_context(nc.psum_tensor("ps", [128, 8, 512], F32))
        PB = [Buf() for _ in range(8)]
        cst = sb("cst", [128, 4, 128], BF16)
        CB = Buf()
        ident, antiI, ones, bones = cst[:, 0, :], cst[:, 1, :], cst[:, 2, :], cst[:, 3, :]
        g32 = sb("g32", [128, L * 24], F32)
        qkgs = sb("qkgs", [128, L * 6], F32)
        dnrm = sb("dnrm", [128, L], F32)
        lamt = sb("lamt", [128, L * 256], F32)
        neglam = sb("neglam", [128, L], F32)
        widx = sb("widx", [128, NT // 128 * 8], F32)
        WIB = Buf()
        GB = Buf()

        S.dma(pool, cst[:], consts.rearrange("k p n -> p k n"), writes=[CB])
        epsb = sb("epsb", [128, 3], F32)
        ci = {1024.0 * EPS: 0, 64.0 * EPS: 1, 128.0 * EPS: 2}
        for cval, cidx in ci.items():
            S.op(dve, lambda h, cval=cval, cidx=cidx: h.memset(epsb[:, cidx:cidx + 1], cval), writes=[CB])
        for tt in range(NTT):
            S.dma(sp, xT[:, :, tt * 512:(tt + 1) * 512],
                  xT_in.rearrange("(c p) n -> p c n", p=128)[:, :, tt * 512:(tt + 1) * 512], writes=[XB[tt]])
        S.dma(sp, g32[:], normsT, writes=[GB])
        S.dma(sp, qkgs[:], qkg, writes=[GB])
        S.dma(sp, dnrm[:], dnorm, writes=[GB])
        S.dma(sp, lamt[:], dlam, writes=[GB])
        S.op(dve, lambda h: h.tensor_scalar(out=g32[:], in0=g32[:], scalar1=32.0, scalar2=None, op0=ALU.mult),
             reads=[GB], writes=[GB])
        for l in range(L):
            for i in range(3):
                c = l * 6 + i * 2 + 1
                S.op(dve, lambda h, c=c: h.tensor_scalar(out=qkgs[:, c:c + 1], in0=qkgs[:, c:c + 1], scalar1=8.0,
                                                         scalar2=None, op0=ALU.mult), reads=[GB], writes=[GB])
        if has_b:
            ltmp = sb("ltmp", [128, 64], F32)
            lsum = sb("lsum", [128, 4], F32)
            LB = Buf()
            for l in range(L):
                lam_init = 0.8 - 0.6 * math.exp(-0.3 * (l + lbase))
                for k in range(2):
                    a0 = l * 256 + k * 128
                    S.op(dve, lambda h, a0=a0: h.tensor_tensor(out=ltmp[:], in0=lamt[:, a0:a0 + 64],
                                                               in1=lamt[:, a0 + 64:a0 + 128], op=ALU.mult),
                         reads=[GB], writes=[LB])
                    S.op(dve, lambda h, k=k: h.tensor_reduce(out=lsum[:, k:k + 1], in_=ltmp[:], axis=AX.X, op=ALU.add),
                         reads=[LB], writes=[LB])
                    S.op(act, lambda h, k=k: h.activation(out=lsum[:, 2 + k:3 + k], in_=lsum[:, k:k + 1], func=AF.Exp),
                         reads=[LB], writes=[LB])
                S.op(dve, lambda h, l=l, li=lam_init: h.scalar_tensor_tensor(
                    out=neglam[:, l:l + 1], in0=lsum[:, 3:4], scalar=-li, in1=lsum[:, 2:3], op0=ALU.add,
                    op1=ALU.subtract), reads=[LB], writes=[GB])
                S.op(dve, lambda h, l=l, li=lam_init: h.tensor_scalar(
                    out=dnrm[:, l:l + 1], in0=dnrm[:, l:l + 1], scalar1=(1.0 - li) * math.sqrt(128.0), scalar2=None,
                    op0=ALU.mult), reads=[GB], writes=[GB])

        bank_rr = {"i": 0}

        def mm(out, lhsT, rhs, start, stop, reads, writes):
            S.op(pe, lambda h: h.matmul(out, lhsT, rhs, start=start, stop=stop), reads=reads, writes=writes)

        def rsq(out, in_, c, reads, wbuf):
            S.op(act, lambda h: h.activation(out=out, in_=in_, func=AF.Sqrt, bias=epsb[0:out.shape[0], ci[c]:ci[c] + 1], scale=1.0),
                 reads=list(reads) + [CB], writes=[wbuf])
            S.op(dve, lambda h: h.reciprocal(out=out, in_=out), reads=[wbuf], writes=[wbuf])

        def emit_rms(gcol0, tgs, hT, HB, tok0, sq, SQB, rstd, RB, banks):
            for n, tg in enumerate(tgs):
                b = banks[n % len(banks)]
                tok = slice(tg * 512, tg * 512 + 512)
                lt = slice((tg - tok0) * 512, (tg - tok0) * 512 + 512)
                for c in range(8):
                    k = c % 2
                    S.op(act, lambda h, c=c, k=k: h.activation(out=sq[:, k, :], in_=xT[:, c, tok], func=AF.Square),
                         reads=[XB[tg]], writes=[SQB[k]])
                    mm(ps[:, b, :], ones, sq[:, k, :], c == 0, c == 7, [SQB[k], CB], [PB[b]])
                rsq(rstd[:], ps[:, b, :], 1024.0 * EPS, [PB[b]], RB)
                for c in range(8):
                    S.op(dve, lambda h, c=c: h.scalar_tensor_tensor(
                        out=hT[:, c, lt], in0=xT[:, c, tok], scalar=g32[:, gcol0 + c:gcol0 + c + 1], in1=rstd[:],
                        op0=ALU.mult, op1=ALU.mult), reads=[XB[tg], RB, GB], writes=[HB[tg - tok0]])

        def emit_ffn(l, which):
            with ExitStack() as fs:
                def fsb(name, shape, dt):
                    uid[0] += 1
                    return fs.enter_context(nc.sbuf_tensor(f"{name}_{uid[0]}", list(shape), dt))
                NH = 1024 if NT >= 1024 else NT
                ntt = NH // 512
                hT = fsb("f_hT", [128, 8, NH], BF16)
                aT = fsb("f_aT", [128, NF, NH], BF16)
                sq = fsb("f_sq", [128, 2, 512], BF16)
                rstd = fsb("f_rstd", [128, 512], F32)
                sg = fsb("f_sg", [128, 2, 512], F32)
                wg = fsb("f_wg", [128, 3, 8, 128], BF16)
                wu = fsb("f_wu", [128, 3, 8, 128], BF16)
                wd = fsb("f_wd", [128, 2, NF, 128], BF16)
                HB = [Buf() for _ in range(ntt)]
                AB = [[Buf() for _ in range(ntt)] for _ in range(NF)]
                SQB = [Buf(), Buf()]
                RB = Buf()
                SGB = [Buf(), Buf()]
                WGB = [Buf() for _ in range(3)]
                WUB = [Buf() for _ in range(3)]
                WDB = [Buf(), Buf()]
                wgv = w_gate[which][l].rearrange("(c p) n -> p c n", p=128)
                wuv = w_up[which][l].rearrange("(c p) n -> p c n", p=128)
                wdv = w_down[which][l].rearrange("(j p) n -> p j n", p=128)
                gcol0 = l * 24 + (0 if which == 0 else 16)
                nsg = 0
                for th in range(NT // NH):
                    tgs = [th * ntt + i for i in range(ntt)]
                    emit_rms(gcol0, tgs, hT, HB, th * ntt, sq, SQB, rstd, RB, [6, 7])
                    for j in range(NF):
                        k3 = j % 3
                        S.dma(pool, wg[:, k3], wgv[:, :, j * 128:(j + 1) * 128], writes=[WGB[k3]])
                        S.dma(pool, wu[:, k3], wuv[:, :, j * 128:(j + 1) * 128], writes=[WUB[k3]])
                        for tt in range(ntt):
                            bg = (j * ntt + tt) % 2
                            bu = 2 + (j * ntt + tt) % 2
                            tl = slice(tt * 512, tt * 512 + 512)
                            for c in range(8):
                                mm(ps[:, bg, :], wg[:, k3, c, :], hT[:, c, tl], c == 0, c == 7, [WGB[k3], HB[tt]], [PB[bg]])
                            for c in range(8):
                                mm(ps[:, bu, :], wu[:, k3, c, :], hT[:, c, tl], c == 0, c == 7, [WUB[k3], HB[tt]], [PB[bu]])
                            k = nsg % 2
                            nsg += 1
                            S.op(act, lambda h, k=k, bg=bg: h.activation(out=sg[:, k, :], in_=ps[:, bg, :], func=AF.Silu),
                                 reads=[PB[bg]], writes=[SGB[k]])
                            S.op(dve, lambda h, k=k, bu=bu, j=j, tl=tl: h.tensor_tensor(
                                out=aT[:, j, tl], in0=sg[:, k, :], in1=ps[:, bu, :], op=ALU.mult),
                                reads=[SGB[k], PB[bu]], writes=[AB[j][tt]])
                    for oc in range(8):
                        k2 = oc % 2
                        S.dma(pool, wd[:, k2], wdv[:, :, oc * 128:(oc + 1) * 128], writes=[WDB[k2]])
                        for tt in range(ntt):
                            bo = 4 + (oc * ntt + tt) % 2
                            tg = th * ntt + tt
                            tl = slice(tt * 512, tt * 512 + 512)
                            tok = slice(tg * 512, tg * 512 + 512)
                            for j in range(NF):
                                mm(ps[:, bo, :], wd[:, k2, j, :], aT[:, j, tl], j == 0, j == NF - 1,
                                   [WDB[k2], AB[j][tt]], [PB[bo]])
                            S.op(dve, lambda h, bo=bo, oc=oc, tok=tok: h.scalar_tensor_tensor(
                                out=xT[:, oc, tok], in0=ps[:, bo, :], scalar=0.5, in1=xT[:, oc, tok], op0=ALU.mult,
                                op1=ALU.add), reads=[PB[bo], XB[tg]], writes=[XB[tg]])
                S.barrier()

        def emit_pre(l):
            with ExitStack() as fs:
                def fsb(name, shape, dt):
                    uid[0] += 1
                    return fs.enter_context(nc.sbuf_tensor(f"{name}_{uid[0]}", list(shape), dt))
                hT = fsb("p_hT", [128, 8, NT], BF16)
                sq = fsb("p_sq", [128, 2, 512], BF16)
                rstd = fsb("p_rstd", [128, 512], F32)
                wt = fsb("p_wt", [128, 3, 8, 128], BF16)
                st = fsb("p_st", [128, 2, NT], BF16)
                wv = fsb("p_wv", [128, 8, VW + 8], BF16)
                vst = fsb("p_vst", [128, 2, VW], BF16)
                HB = [Buf() for _ in range(NTT)]
                SQB = [Buf(), Buf()]
                RB = Buf()
                WTB = [Buf() for _ in range(3)]
                STB = [Buf(), Buf()]
                WVB = Buf()
                VSB = [Buf(), Buf()]
                winv = w_in[l].rearrange("(c p) n -> p c n", p=128)
                emit_rms(l * 24 + 8, list(range(NTT)), hT, HB, 0, sq, SQB, rstd, RB, [6, 7])
                for tt in range(NTT):
                    S.dma(sp, hT_s.rearrange("(c p) n -> p c n", p=128)[:, :, tt * 512:(tt + 1) * 512],
                          hT[:, :, tt * 512:(tt + 1) * 512], reads=[HB[tt]])
                S.dma(pool, wv[:, :, 0:768], winv[:, :, 1536:2304], writes=[WVB])
                S.dma(pool, wv[:, :, 768:1280], winv[:, :, 3328:3840], writes=[WVB])
                S.dma(pool, wv[:, :, 1280:1344], winv[:, :, 4160:4224], writes=[WVB])
                S.dma(pool, wv[:, :, 1344:1352], winv[:, :, 4800:4808], writes=[WVB])
                qc = l * 6
                chunks = []
                for i in range(6):
                    chunks.append(("n", i * 128, None, qc + 0, QT_s, i))
                for i in range(6):
                    chunks.append(("n", 768 + i * 128, None, qc + 1, KT_l, i))
                for i in range(4):
                    chunks.append(("n", 2304 + i * 128, None, qc + 2, QT_s, 6 + i))
                for i in range(4):
                    chunks.append(("n", 2816 + i * 128, None, qc + 3, KT_l, 6 + i))
                for i in range(2):
                    chunks.append(("n", 3840 + i * 128, None, qc + 4, QT_s, 10 + i))
                chunks.append(("ck", 4096, 4736, qc + 5, KT_l, 10))
                for i in range(4):
                    chunks.append(("p", 4224 + i * 128, None, None, QT_s, 12 + i))
                for ci, (kind, c0, c1, gcol, dst, dchunk) in enumerate(chunks):
                    k3 = ci % 3
                    if kind == "ck":
                        S.dma(pool, wt[:, k3, :, 0:64], winv[:, :, c0:c0 + 64], writes=[WTB[k3]])
                        S.dma(pool, wt[:, k3, :, 64:128], winv[:, :, c1:c1 + 64], writes=[WTB[k3]])
                    else:
                        S.dma(pool, wt[:, k3], winv[:, :, c0:c0 + 128], writes=[WTB[k3]])
                    ks = ci % 2
                    for tt in range(NTT):
                        b = (ci * NTT + tt) % 2
                        tl = slice(tt * 512, tt * 512 + 512)
                        for c in range(8):
                            mm(ps[:, b, :], wt[:, k3, c, :], hT[:, c, tl], c == 0, c == 7, [WTB[k3], HB[tt]], [PB[b]])
                        if kind == "p":
                            S.op(act, lambda h, b=b, ks=ks, tl=tl: h.activation(out=st[:, ks, tl], in_=ps[:, b, :],
                                                                                func=AF.Copy, scale=0.125),
                                 reads=[PB[b]], writes=[STB[ks]])
                            continue
                        np_ = 64 if kind == "ck" else 128
                        kq = (ci * NTT + tt) % 2
                        b2 = 2 + (ci * NTT + tt) % 2
                        S.op(act, lambda h, b=b, kq=kq, np_=np_: h.activation(out=sq[0:np_, kq, :], in_=ps[0:np_, b, :],
                                                                               func=AF.Square),
                             reads=[PB[b]], writes=[SQB[kq]])
                        mm(ps[0:np_, b2, :], bones[0:np_, 0:np_], sq[0:np_, kq, :], True, True, [SQB[kq], CB], [PB[b2]])
                        rsq(rstd[0:np_, :], ps[0:np_, b2, :], 64.0 * EPS, [PB[b2]], RB)
                        S.op(dve, lambda h, b=b, ks=ks, tl=tl, np_=np_, gcol=gcol: h.scalar_tensor_tensor(
                            out=st[0:np_, ks, tl], in0=ps[0:np_, b, :], scalar=qkgs[0:np_, gcol:gcol + 1],
                            in1=rstd[0:np_, :], op0=ALU.mult, op1=ALU.mult), reads=[PB[b], RB, GB], writes=[STB[ks]])
                        if kind == "ck":
                            S.op(act, lambda h, b=b, ks=ks, tl=tl: h.activation(
                                out=st[64:128, ks, tl], in_=ps[64:128, b, :], func=AF.Copy), reads=[PB[b]],
                                writes=[STB[ks]])
                    S.dma(sp, dst[dchunk * 128:(dchunk + 1) * 128, :], st[:, ks, :], reads=[STB[ks]])
                for t128 in range(NT // 128):
                    tl = slice(t128 * 128, t128 * 128 + 128)
                    kv = t128 % 2
                    groups = [(0, 512), (512, 768), (768, 1280), (1280, 1352)]
                    for gi, (a0, a1) in enumerate(groups):
                        b = 4 + (t128 * 4 + gi) % 4
                        n = a1 - a0
                        for c in range(8):
                            mm(ps[:, b, 0:n], hT[:, c, tl], wv[:, c, a0:a1], c == 0, c == 7, [HB[t128 // 4], WVB], [PB[b]])
                        if gi < 3:
                            eng = act if gi % 2 == 0 else dve
                            if eng is act:
                                S.op(act, lambda h, b=b, n=n, a0=a0, a1=a1, kv=kv: h.activation(
                                    out=vst[:, kv, a0:a1], in_=ps[:, b, 0:n], func=AF.Copy), reads=[PB[b]], writes=[VSB[kv]])
                            else:
                                S.op(dve, lambda h, b=b, n=n, a0=a0, a1=a1, kv=kv: h.tensor_copy(
                                    out=vst[:, kv, a0:a1], in_=ps[:, b, 0:n]), reads=[PB[b]], writes=[VSB[kv]])
                        else:
                            S.op(act, lambda h, b=b, kv=kv: h.activation(
                                out=vst[:, kv, 1280:1344], in_=ps[:, b, 0:64], func=AF.Copy), reads=[PB[b]], writes=[VSB[kv]])
                            S.op(dve, lambda h, b=b, t128=t128: h.tensor_scalar(
                                out=widx[:, t128 * 8:t128 * 8 + 8], in0=ps[:, b, 64:72], scalar1=8.0 ** -0.5,
                                scalar2=None, op0=ALU.mult), reads=[PB[b]], writes=[WIB])
                    S.dma(sp, V_l[tl, :], vst[:, kv, :], reads=[VSB[kv]])
                if not fused:
                    S.dma(sp, widx_o, widx[:], reads=[WIB])
                S.barrier()

        def emit_vec():
            with ExitStack() as fs:
                def fsb(name, shape, dt):
                    uid[0] += 1
                    return fs.enter_context(nc.sbuf_tensor(f"{name}_{uid[0]}", list(shape), dt))
                tbl = fsb("v_tbl", [33, 20], F32)
                oh = fsb("v_oh", [33, 2, 512], F32)
                vs = fsb("v_vs", [20, 2, 512], BF16)
                TB = Buf()
                OB = [Buf(), Buf()]
                VB = [Buf(), Buf()]
                S.op(dve, lambda h: h.memset(tbl[:], -BIG), writes=[TB])
                S.dma(sp, tbl[0:32, :], rel_bias, writes=[TB])
                n = 0
                for kp in range(NKIND * 2):
                    for c0 in range(0, LV, 512):
                        cw = min(512, LV - c0)
                        k = n % 2
                        n += 1
                        S.dma(sp, oh[:, k, 0:cw], onehot[kp, :, c0:c0 + cw], writes=[OB[k]])
                        b = k
                        mm(ps[0:20, b, 0:cw], tbl[:], oh[:, k, 0:cw], True, True, [TB, OB[k]], [PB[b]])
                        S.op(act, lambda h, b=b, k=k, cw=cw: h.activation(out=vs[:, k, 0:cw], in_=ps[0:20, b, 0:cw],
                                                                          func=AF.Copy), reads=[PB[b]], writes=[VB[k]])
                        S.dma(sp, vec_s.rearrange("(k h) n -> k h n", h=20)[kp, :, c0:c0 + cw], vs[:, k, 0:cw],
                              reads=[VB[k]])
                S.barrier()

        def strip_src(kind, par, head, width):
            row = (kind * 2 + par) * 20 + head
            return bass.AP(tensor=vec_s.tensor, offset=row * LV, ap=[[1, 128], [1, width]])

        def kcol(kt):
            gs = kt // 4
            r, li = gpos(gs)
            return r * NT + li * 512 + (kt % 4) * 128

        def vtile(kt):
            gs = kt // 4
            r, li = gpos(gs)
            return li * 8 + r * 4 + kt % 4

        def emit_att(l, oT_a, oT_b, OAB, OBB):
            with ExitStack() as fs:
                def fsb(name, shape, dt):
                    uid[0] += 1
                    return fs.enter_context(nc.sbuf_tensor(f"{name}_{uid[0]}", list(shape), dt))
                KT = fsb("a_KT", [64, 2, 2 * NT], BF16)
                QTt = fsb("a_QT", [64, 2, NT], BF16)
                Vt = fsb("a_V", [128, 2 * NT // 128, 128], BF16)
                G = fsb("a_G", [128, 2, WD], BF16)
                Pt = fsb("a_P", [128, 3, 512], BF16)
                ev = fsb("a_ev", [128, 4, 512], F32)
                sqb = fsb("a_sq", [128, 512], BF16)
                KB = [Buf(), Buf()]
                QB = [Buf(), Buf()]
                VB = Buf()
                GBf = Buf()
                PtB = [Buf() for _ in range(3)]
                EB = [Buf() for _ in range(4)]
                SQ = Buf()
                KTf = KT_f.rearrange("(g r c p) n -> g c p r n", g=4, r=2, c=3, p=128)
                QTi = QT_si.rearrange("(c p) n -> c p n", p=128)
                Vf = V_f.rearrange("(t p) c -> p t c", p=128)
                npt = [0]

                def attend(m, terms, nmap, dv):
                    first = {0: True, 1: True}
                    total = {0: 0, 1: 0}
                    for (mp, ksl, qsl, kts, c0f) in terms:
                        total[mp] += len(kts)
                    cnt = {0: 0, 1: 0}
                    for (mp, ksl, qsl, kts, c0f) in terms:
                        for kt in kts:
                            k = npt[0] % 3
                            npt[0] += 1
                            b = k
                            kc = kcol(kt)
                            mm(ps[:, b, :], KT[:, ksl, kc:kc + 128], QTt[:, qsl, m * 512:(m + 1) * 512], True, False,
                               [KB[ksl], QB[qsl]], [PB[b]])
                            c0 = c0f(kt)
                            mm(ps[:, b, :], antiI, G[:, m % 2, c0:c0 + 512], False, True, [GBf, CB], [PB[b]])
                            S.op(act, lambda h, b=b, k=k: h.activation(out=Pt[:, k, :], in_=ps[:, b, :], func=AF.Exp),
                                 reads=[PB[b]], writes=[PtB[k]])
                            cnt[mp] += 1
                            st_, sp_ = cnt[mp] == 1, cnt[mp] == total[mp]
                            mm(ps[0:dv, 3 + mp, :], Vt[:, vtile(kt), 0:dv], Pt[:, k, :], st_, sp_, [VB, PtB[k]], [PB[3 + mp]])
                            mm(ps[0:dv, 5 + mp, :], ones[:, 0:dv], Pt[:, k, :], st_, sp_, [CB, PtB[k]], [PB[5 + mp]])

                for s in range(4):
                    for m in range(NM):
                        gsN = 2 * m + 1
                        terms = []
                        first = True
                        ngrp = 0
                        kt_lists = []
                        for g, (w, d) in enumerate(DIL):
                            kts = [kt for kt in range((gsN + 1) * 4)
                                   if -384 <= gsN * 512 - 128 * kt <= w + 639]
                            kt_lists.append(kts)
                        tot = sum(len(k) for k in kt_lists)
                        cnt = 0
                        for g, (w, d) in enumerate(DIL):
                            head = 4 * g + s
                            ch, hf_ = head // 2, head % 2
                            sl = (s * 3 * NM + m * 3 + g) % 2
                            S.dma(sp, KT[:, sl, :].rearrange("p (r n) -> p r n", r=2), KTf[ch // 3, ch % 3, hf_ * 64:hf_ * 64 + 64],
                                  writes=[KB[sl]])
                            S.dma(sp, QTt[:, sl, :], QTi[ch, hf_ * 64:hf_ * 64 + 64, :], writes=[QB[sl]])
                            if m == 0 or True:
                                S.dma(sp, Vt[:, :, 0:64], Vf[:, :, head * 64:head * 64 + 64], writes=[VB])
                            wdt = min(WD, w + 639 + 384 + 512 + 128)
                            for par in range(2):
                                S.dma(sp, G[:, par, 0:wdt], strip_src(g, par, head, wdt), writes=[GBf])
                            for kt in kt_lists[g]:
                                k = npt[0] % 3
                                npt[0] += 1
                                b = k
                                kc = kcol(kt)
                                mm(ps[:, b, :], KT[:, sl, kc:kc + 128], QTt[:, sl, m * 512:(m + 1) * 512], True, False,
                                   [KB[sl], QB[sl]], [PB[b]])
                                c0 = gsN * 512 - 128 * kt + 384
                                mm(ps[:, b, :], antiI, G[:, m % 2, c0:c0 + 512], False, True, [GBf, CB], [PB[b]])
                                S.op(act, lambda h, b=b, k=k: h.activation(out=Pt[:, k, :], in_=ps[:, b, :], func=AF.Exp),
                                     reads=[PB[b]], writes=[PtB[k]])
                                cnt += 1
                                mm(ps[0:64, 3, :], Vt[:, vtile(kt), 0:64], Pt[:, k, :], cnt == 1, cnt == tot,
                                   [VB, PtB[k]], [PB[3]])
                                mm(ps[0:64, 5, :], ones[:, 0:64], Pt[:, k, :], cnt == 1, cnt == tot, [CB, PtB[k]], [PB[5]])
                        S.op(dve, lambda h: h.reciprocal(out=ev[0:64, 0, :], in_=ps[0:64, 5, :]), reads=[PB[5]], writes=[EB[0]])
                        S.op(dve, lambda h, s=s, m=m: h.tensor_tensor(out=oT_a[:, s, m * 512:(m + 1) * 512],
                                                                       in0=ps[0:64, 3, :], in1=ev[0:64, 0, :], op=ALU.mult),
                             reads=[PB[3], EB[0]], writes=[OAB])
                for hb in range(4):
                    ch, hf_ = hb // 2, hb % 2
                    for mp in range(2):
                        S.dma(sp, KT[:, mp, :].rearrange("p (r n) -> p r n", r=2),
                              KTf[(6 + 2 * mp + ch) // 3, (6 + 2 * mp + ch) % 3, hf_ * 64:hf_ * 64 + 64], writes=[KB[mp]])
                        S.dma(sp, QTt[:, mp, :], QTi[6 + 2 * mp + ch, hf_ * 64:hf_ * 64 + 64, :], writes=[QB[mp]])
                    S.dma(sp, Vt[:], Vf[:, :, 768 + hb * 128:768 + hb * 128 + 128], writes=[VB])
                    for par in range(2):
                        S.dma(sp, G[:, par, :], strip_src(3, par, 12 + hb, WD), writes=[GBf])
                    for m in range(NM):
                        gsN = 2 * m + 1
                        nkt = (gsN + 1) * 4
                        for kt in range(nkt):
                            D0 = gsN * 512 - 128 * kt
                            c0 = D0 + 384 if D0 < 2176 else 2560
                            kc = kcol(kt)
                            for mp in range(2):
                                k = npt[0] % 3
                                npt[0] += 1
                                b = k
                                mm(ps[:, b, :], KT[:, mp, kc:kc + 128], QTt[:, mp, m * 512:(m + 1) * 512], True, False,
                                   [KB[mp], QB[mp]], [PB[b]])
                                mm(ps[:, b, :], antiI, G[:, m % 2, c0:c0 + 512], False, True, [GBf, CB], [PB[b]])
                                S.op(act, lambda h, b=b, k=k: h.activation(out=Pt[:, k, :], in_=ps[:, b, :], func=AF.Exp),
                                     reads=[PB[b]], writes=[PtB[k]])
                                mm(ps[:, 3 + mp, :], Vt[:, vtile(kt), :], Pt[:, k, :], kt == 0, kt == nkt - 1,
                                   [VB, PtB[k]], [PB[3 + mp]])
                                mm(ps[:, 5 + mp, :], ones, Pt[:, k, :], kt == 0, kt == nkt - 1, [CB, PtB[k]], [PB[5 + mp]])
                        for mp in range(2):
                            S.op(dve, lambda h, mp=mp: h.reciprocal(out=ev[:, mp, :], in_=ps[:, 5 + mp, :]),
                                 reads=[PB[5 + mp]], writes=[EB[mp]])
                            S.op(dve, lambda h, mp=mp: h.tensor_tensor(out=ev[:, mp, :], in0=ps[:, 3 + mp, :],
                                                                       in1=ev[:, mp, :], op=ALU.mult),
                                 reads=[PB[3 + mp], EB[mp]], writes=[EB[mp]])
                        S.op(dve, lambda h: h.scalar_tensor_tensor(out=ev[:, 2, :], in0=ev[:, 1, :],
                                                                   scalar=neglam[:, l:l + 1], in1=ev[:, 0, :],
                                                                   op0=ALU.mult, op1=ALU.add),
                             reads=[EB[0], EB[1], GB], writes=[EB[2]])
                        S.op(act, lambda h: h.activation(out=sqb[:], in_=ev[:, 2, :], func=AF.Square), reads=[EB[2]],
                             writes=[SQ])
                        mm(ps[:, 7, :], ones, sqb[:], True, True, [SQ, CB], [PB[7]])
                        rsq(ev[:, 3, :], ps[:, 7, :], 128.0 * EPS, [PB[7]], EB[3])
                        S.op(dve, lambda h, hb=hb, m=m: h.scalar_tensor_tensor(
                            out=oT_b[:, hb, m * 512:(m + 1) * 512], in0=ev[:, 2, :], scalar=dnrm[:, l:l + 1],
                            in1=ev[:, 3, :], op0=ALU.mult, op1=ALU.mult), reads=[EB[2], EB[3], GB], writes=[OBB])
                S.barrier()

        WC = 3072

        def emit_dsa(l, oT_c, OCB):
            with ExitStack() as fs:
                def fsb(name, shape, dt):
                    uid[0] += 1
                    return fs.enter_context(nc.sbuf_tensor(f"{name}_{uid[0]}", list(shape), dt))
                KK = fsb("c_KK", [128, 2 * NT], BF16)
                QI = fsb("c_QI", [128, 8, 512], BF16)
                QC = fsb("c_QC", [64, 4, 512], BF16)
                Vc = fsb("c_V", [128, 2 * NT // 128, 64], BF16)
                G = fsb("c_G", [128, 4, WC], BF16)
                cmt = fsb("c_cm", [128, 2, 1024], F32)
                cmb = fsb("c_cmb", [128, 2, 1024], BF16)
                sc = fsb("c_sc", [128, T], F32)
                rl = fsb("c_rl", [128, 2, 512], F32)
                mx = fsb("c_mx", [128, 8], F32)
                mneg = fsb("c_mneg", [128, T], BF16)
                Pt = fsb("c_P", [128, 2, 512], BF16)
                ev = fsb("c_ev", [64, 512], F32)
                B_ld = Buf()
                QLB = Buf()
                GLB = Buf()
                SCB = Buf()
                RLB = [Buf(), Buf()]
                MXB = Buf()
                MNB = Buf()
                PtB = [Buf(), Buf()]
                EVB = Buf()
                KTf = KT_f.rearrange("(g r c p) n -> g c p r n", g=4, r=2, c=3, p=128)
                QTi = QT_si.rearrange("(c p) n -> c p n", p=128)
                Vf = V_f.rearrange("(t p) c -> p t c", p=128)
                S.dma(sp, KK[:, :].rearrange("p (r n) -> p r n", r=2), KTf[3, 1], writes=[B_ld])
                S.dma(sp, Vc[:], Vf[:, :, 1280:1344], writes=[B_ld])
                S.dma(sp, cmt[:], cm_in.rearrange("k p n -> p k n"), writes=[B_ld])
                S.op(dve, lambda h: h.tensor_scalar(out=cmb[:], in0=cmt[:], scalar1=BIG / 1.0e30, scalar2=None,
                                                    op0=ALU.mult), reads=[B_ld], writes=[B_ld])
                for m in range(NM):
                    gsN = 2 * m + 1
                    nkeys = (gsN + 1) * 512
                    nch = nkeys // 512
                    ms = slice(m * 512, m * 512 + 512)
                    for i in range(8):
                        S.dma(sp, QI[64:128, i, :], QTi[12 + i // 2, (i % 2) * 64:(i % 2) * 64 + 64, ms], writes=[QLB])
                    for i in range(4):
                        S.dma(sp, QC[:, i, :], QTi[10 + i // 2, (i % 2) * 64:(i % 2) * 64 + 64, ms], writes=[QLB])
                        S.dma(sp, G[:, i, :], strip_src(3, m % 2, 16 + i, WC), writes=[GLB])
                    for qb in range(4):
                        ql = slice(qb * 128, qb * 128 + 128)
                        q0 = m * 512 + qb * 128
                        t128 = q0 // 128
                        nr = 0
                        for kc in range(nch):
                            r, li = gpos(kc)
                            col = r * NT + li * 512
                            for hh in range(8):
                                b = (kc * 8 + hh) % 2
                                mm(ps[:, b, :], QI[64:128, hh, ql], KK[64:128, col:col + 512],
                                   True, True, [B_ld, QLB], [PB[b]])
                                k = nr % 2
                                nr += 1
                                S.op(act, lambda h, b=b, k=k: h.activation(out=rl[:, k, :], in_=ps[:, b, :], func=AF.Relu),
                                     reads=[PB[b]], writes=[RLB[k]])
                                dst = sc[:, kc * 512:(kc + 1) * 512]
                                wcol = widx[:, t128 * 8 + hh:t128 * 8 + hh + 1]
                                if hh == 0:
                                    S.op(pool, lambda h, k=k, dst=dst, wcol=wcol: h.tensor_scalar(
                                        out=dst, in0=rl[:, k, :], scalar1=wcol, scalar2=None, op0=ALU.mult),
                                        reads=[RLB[k], WIB, MNB], writes=[SCB])
                                else:
                                    S.op(pool, lambda h, k=k, wcol=wcol: h.tensor_scalar(
                                        out=rl[:, k, :], in0=rl[:, k, :], scalar1=wcol, scalar2=None, op0=ALU.mult),
                                        reads=[RLB[k], WIB], writes=[RLB[k]])
                                    S.op(pool, lambda h, k=k, dst=dst: h.tensor_tensor(
                                        out=dst, in0=dst, in1=rl[:, k, :], op=ALU.add),
                                        reads=[RLB[k]], writes=[SCB])
                        lo = gsN * 512 + 128 * qb - 512
                        hi = nkeys
                        S.op(dve, lambda h, lo=lo, hi=hi, m=m: h.tensor_tensor(
                            out=sc[:, lo:hi], in0=sc[:, lo:hi], in1=cmt[:, m % 2, 0:hi - lo], op=ALU.add),
                            reads=[SCB, B_ld], writes=[SCB])
                        for it in range(32):
                            S.op(dve, lambda h: h.max(out=mx[:], in_=sc[:, 0:nkeys]), reads=[SCB], writes=[MXB])
                            S.op(dve, lambda h: h.match_replace(out=sc[:, 0:nkeys], in_to_replace=mx[:],
                                                                in_values=sc[:, 0:nkeys], imm_value=-3.0e38),
                                 reads=[SCB, MXB], writes=[SCB])
                        S.op(dve, lambda h: h.tensor_scalar(out=mneg[:, 0:nkeys], in0=sc[:, 0:nkeys],
                                                            scalar1=-1.0e38, scalar2=-BIG, op0=ALU.is_gt,
                                                            op1=ALU.mult), reads=[SCB], writes=[MNB])
                        S.op(dve, lambda h, lo=lo, hi=hi, m=m: h.tensor_tensor(
                            out=mneg[:, lo:hi], in0=mneg[:, lo:hi], in1=cmb[:, m % 2, 0:hi - lo], op=ALU.add),
                            reads=[MNB, B_ld], writes=[MNB])
                        nkt = nkeys // 128
                        for kt in range(nkt):
                            kcg = kcol(kt)
                            D0 = gsN * 512 - 128 * kt
                            c0 = (D0 + 384 if D0 < 2176 else 2560) + 128 * qb
                            b = 2 + kt % 2
                            k = kt % 2
                            for hc in range(4):
                                o = ps[:, b, hc * 128:(hc + 1) * 128]
                                mm(o, KK[0:64, kcg:kcg + 128], QC[:, hc, ql], True, False, [B_ld, QLB], [PB[b]])
                                mm(o, antiI, G[:, hc, c0:c0 + 128], False, False, [GLB, CB], [PB[b]])
                                mm(o, mneg[:, kt * 128:(kt + 1) * 128], ident, False, True, [MNB, CB], [PB[b]])
                            S.op(act, lambda h, b=b, k=k: h.activation(out=Pt[:, k, :], in_=ps[:, b, :], func=AF.Exp),
                                 reads=[PB[b]], writes=[PtB[k]])
                            for hc in range(4):
                                mm(ps[0:64, 4, hc * 128:(hc + 1) * 128], Vc[:, vtile(kt), :],
                                   Pt[:, k, hc * 128:(hc + 1) * 128], kt == 0 and hc == 0, kt == nkt - 1,
                                   [B_ld, PtB[k]], [PB[4]])
                            mm(ps[0:64, 5, :], ones[:, 0:64], Pt[:, k, :], kt == 0, kt == nkt - 1, [CB, PtB[k]], [PB[5]])
                        S.op(dve, lambda h: h.reciprocal(out=ev[:], in_=ps[0:64, 5, :]), reads=[PB[5]], writes=[EVB])
                        for hc in range(4):
                            S.op(dve, lambda h, hc=hc, q0=q0: h.tensor_tensor(
                                out=oT_c[:, hc, q0:q0 + 128], in0=ps[0:64, 4, hc * 128:(hc + 1) * 128],
                                in1=ev[:, hc * 128:(hc + 1) * 128], op=ALU.mult), reads=[PB[4], EVB], writes=[OCB])
                S.barrier()

        def emit_post(l, oT_a, oT_b, oT_c, OAB, OBB, OCB):
            with ExitStack() as fs:
                def fsb(name, shape, dt):
                    uid[0] += 1
                    return fs.enter_context(nc.sbuf_tensor(f"{name}_{uid[0]}", list(shape), dt))
                hT = fsb("o_hT", [128, 1, 8, 512], BF16)
                yT = fsb("o_yT", [128, 8, 512], BF16)
                wga = fsb("o_wg", [128, 3, 8, 128], BF16)
                wa = fsb("o_wa", [64, 4, D], BF16)
                wb = fsb("o_wb", [128, 4, D], BF16)
                wc = fsb("o_wc", [64, 4, D], BF16)
                wo = fsb("o_wo", [128, 8, D], BF16)
                sg = fsb("o_sg", [128, 2, 512], F32)
                tmp = fsb("o_tmp", [128, 2, 512], F32)
                yacc = fsb("o_yacc", [128, 512], F32)
                HB = [Buf(), Buf()]
                YB = Buf()
                WGB = [Buf() for _ in range(3)]
                WB = Buf()
                SGB = [Buf(), Buf()]
                TMB = [Buf(), Buf()]
                YAB = Buf()
                winv = w_in[l].rearrange("(c p) n -> p c n", p=128)
                S.dma(pool, wa[:], w_br[0][l].rearrange("(h p) n -> p h n", p=64), writes=[WB])
                S.dma(pool, wb[:], w_br[1][l].rearrange("(h p) n -> p h n", p=128), writes=[WB])
                S.dma(pool, wc[:], w_br[2][l].rearrange("(h p) n -> p h n", p=64), writes=[WB])
                S.dma(pool, wo[:], w_out[l].rearrange("(c p) n -> p c n", p=128), writes=[WB])
                nw = 0
                ns = 0
                for tt in range(NTT):
                    tok = slice(tt * 512, tt * 512 + 512)
                    kh = 0
                    S.dma(sp, hT[:, kh], hT_si.rearrange("(c p) n -> p c n", p=128)[:, :, tok], writes=[HB[kh]])
                    for oc in range(8):
                        for i in range(3):
                            k3 = nw % 3
                            nw += 1
                            g0 = 4808 + i * 1024 + oc * 128
                            S.dma(pool, wga[:, k3], winv[:, :, g0:g0 + 128], writes=[WGB[k3]])
                            bg = (oc * 3 + i) % 2
                            bb = 2 + (oc * 3 + i) % 2
                            for c in range(8):
                                mm(ps[:, bg, :], wga[:, k3, c, :], hT[:, kh, c, :], c == 0, c == 7, [WGB[k3], HB[kh]], [PB[bg]])
                            ocs = slice(oc * 128, oc * 128 + 128)
                            if i == 0:
                                for hh in range(4):
                                    mm(ps[:, bb, :], wa[:, hh, ocs], oT_a[:, hh, tok], hh == 0, hh == 3, [WB, OAB], [PB[bb]])
                            elif i == 1:
                                for hh in range(4):
                                    mm(ps[:, bb, :], wb[:, hh, ocs], oT_b[:, hh, tok], hh == 0, hh == 3, [WB, OBB], [PB[bb]])
                            else:
                                for hh in range(4):
                                    mm(ps[:, bb, :], wc[:, hh, ocs], oT_c[:, hh, tok], hh == 0, hh == 3, [WB, OCB], [PB[bb]])
                            k = ns % 2
                            ns += 1
                            S.op(act, lambda h, k=k, bg=bg: h.activation(out=sg[:, k, :], in_=ps[:, bg, :], func=AF.Sigmoid),
                                 reads=[PB[bg]], writes=[SGB[k]])
                            if i == 0:
                                S.op(dve, lambda h, k=k, bb=bb: h.tensor_tensor(out=yacc[:], in0=sg[:, k, :], in1=ps[:, bb, :],
                                                                               op=ALU.mult), reads=[SGB[k], PB[bb]], writes=[YAB])
                            else:
                                S.op(dve, lambda h, k=k, bb=bb: h.tensor_tensor(out=tmp[:, k, :], in0=sg[:, k, :],
                                                                               in1=ps[:, bb, :], op=ALU.mult),
                                     reads=[SGB[k], PB[bb]], writes=[TMB[k]])
                                if i == 1:
                                    S.op(pool, lambda h, k=k: h.tensor_tensor(out=yacc[:], in0=yacc[:], in1=tmp[:, k, :],
                                                                             op=ALU.add), reads=[YAB, TMB[k]], writes=[YAB])
                                else:
                                    S.op(pool, lambda h, k=k, oc=oc: h.tensor_tensor(out=yT[:, oc, :], in0=yacc[:],
                                                                                    in1=tmp[:, k, :], op=ALU.add),
                                         reads=[YAB, TMB[k]], writes=[YB])
                    for oc in range(8):
                        bo = 4 + oc % 2
                        for c in range(8):
                            mm(ps[:, bo, :], wo[:, c, oc * 128:(oc + 1) * 128], yT[:, c, :], c == 0, c == 7, [WB, YB], [PB[bo]])
                        S.op(dve, lambda h, bo=bo, oc=oc, tok=tok: h.tensor_tensor(out=xT[:, oc, tok], in0=ps[:, bo, :],
                                                                                  in1=xT[:, oc, tok], op=ALU.add),
                             reads=[PB[bo], XB[tt]], writes=[XB[tt]])
                S.barrier()

        S.barrier()
        if has_b:
            emit_vec()
        for (ph, l) in parts:
            if fused:
                KT_l, V_l, KT_f, V_f = KT_l2[l % 2], V_l2[l % 2], KT_f2[l % 2], V_f2[l % 2]
            if ph == "a":
                emit_ffn(l, 0)
                emit_pre(l)
                if fused:
                    S.drain(pool)
                    groups = [[2 * i, 2 * i + 1] for i in range(ncores // 2)]
                    pieces = [(KT_l[g * 384:(g + 1) * 384, :], KT_f[g * 768:(g + 1) * 768, :]) for g in range(4)]
                    pieces += [(V_l[g * 512:(g + 1) * 512, :], V_f[g * 1024:(g + 1) * 1024, :]) for g in range(NM)]
                    for (src_, dst_) in pieces:
                        ins = nc.gpsimd.collective_compute("AllGather", ALU.bypass, replica_groups=groups,
                                                           ins=[src_.opt()], outs=[dst_.opt()])
                        S.cc_val += 1
                        ins.then_inc(S.cc_sem)
                    S.barrier()
            elif ph == "b":
                if not fused and (ph, l) == parts[0]:
                    S.dma(sp, widx[:], widx_i, writes=[WIB])
                with ExitStack() as bs:
                    oT_c = bs.enter_context(nc.sbuf_tensor(f"oT_c{l}", [64, 4, NT], BF16))
                    OAB, OBB, OCB = Buf(), Buf(), Buf()
                    if "c" in MIX:
                        emit_dsa(l, oT_c, OCB)
                    oT_a = bs.enter_context(nc.sbuf_tensor(f"oT_a{l}", [64, 4, NT], BF16))
                    oT_b = bs.enter_context(nc.sbuf_tensor(f"oT_b{l}", [128, 4, NT], BF16))
                    emit_att(l, oT_a, oT_b, OAB, OBB)
                    emit_post(l, oT_a, oT_b, oT_c, OAB, OBB, OCB)
                emit_ffn(l, 1)
        for tt in range(NTT):
            S.dma(sp, xT_out.rearrange("(c p) n -> p c n", p=128)[:, :, tt * 512:(tt + 1) * 512],
                  xT[:, :, tt * 512:(tt + 1) * 512], reads=[XB[tt]])
        S.barrier()
    nc._n_ins = S.n_ins
    return nc


def rel_bucket_np(dist):
    n = np.maximum(dist, 0)
    nf = np.maximum(n, 1).astype(np.float32)
    large = 16 + (np.log(nf / np.float32(16)) / np.float32(math.log(2048 / 16)) * np.float32(16)).astype(np.int32)
    large = np.minimum(large, 31)
    return np.where(n < 16, n, large)


def make_onehot(e_par):
    oh = np.zeros((NKIND * 2, 33, LV), np.float32)
    v = np.arange(LV)
    for kind in range(NKIND):
        for p in range(2):
            dist = v - VOFF - 512 * e_par[p]
            if kind < 3:
                w, d = DIL[kind]
                valid = (dist >= 0) & (dist <= w) & (dist % d == 0)
            else:
                valid = dist >= 0
            bk = rel_bucket_np(dist)
            o = oh[kind * 2 + p]
            o[bk[valid], v[valid]] = 1.0
            o[32, v[~valid]] = 1.0
    return oh


def make_cm(e_par):
    cm = np.zeros((2, 128, 1024), np.float32)
    u = np.arange(1024)[None, :]
    qi = np.arange(128)[:, None]
    for p in range(2):
        cm[p] = np.where(u - 512 + 512 * e_par[p] <= qi, 0.0, -1.0e30)
    return cm


def local_superblocks(hf, ns):
    return [gs for gs in range(ns) if (gs % 4 in (0, 3)) == (hf == 0)]


_PROG = {}


def _get_prog(T, L, parts, fused, lbase=0, ncores=8):
    key = (T, L, tuple(parts), fused, lbase, ncores)
    if key not in _PROG:
        _PROG[key] = build(T, L, list(parts), fused, lbase, ncores)
    return _PROG[key]


A_KEYS = ("ffn1_w_gate", "ffn1_w_up", "ffn1_w_down", "w_in")
B_KEYS = ("ffn2_w_gate", "ffn2_w_up", "ffn2_w_down", "w_in", "w_branch_a", "w_branch_b", "w_branch_c", "w_out")


def run_model(inputs, T, L, B, fused=True):
    x = np.asarray(inputs["x"], np.float32)
    NT = T // 2
    NS = T // 512
    ncores = 2 * B
    consts = np.zeros((4, 128, 128), np.float32)
    consts[0] = np.eye(128)
    consts[1] = np.eye(128)[::-1]
    consts[2] = 1.0
    consts[3, :64, :64] = 1.0
    consts[3, 64:, 64:] = 1.0
    norms = np.stack([inputs["ffn1_norm"], inputs["mix_norm"], inputs["ffn2_norm"]], 1)
    normsT = np.ascontiguousarray(norms.reshape(L, 3, 8, 128).transpose(3, 0, 1, 2).reshape(128, L * 24)).astype(np.float32)
    qg = np.asarray(inputs["qk_gain"], np.float32).reshape(L * 6, 64)
    qkg = np.ascontiguousarray(np.concatenate([qg, qg], 1).T)
    dnorm = np.ascontiguousarray(np.asarray(inputs["diff_out_norm"], np.float32).T)
    dlam = np.ascontiguousarray(np.broadcast_to(np.asarray(inputs["diff_lambda"], np.float32).reshape(1, L * 256), (128, L * 256)))
    shared = {"consts": consts, "normsT": normsT, "qkg": qkg, "dnorm": dnorm, "dlam": dlam,
              "rel_bias": np.asarray(inputs["rel_bias"], np.float32)}
    for k in ("ffn1_w_gate", "ffn1_w_up", "ffn1_w_down", "ffn2_w_gate", "ffn2_w_up", "ffn2_w_down", "w_in",
              "w_branch_a", "w_branch_b", "w_branch_c", "w_out"):
        shared[k] = np.asarray(inputs[k], np.float32)
    percore = []
    for c in range(ncores):
        b, hf = c // 2, c % 2
        sbs = local_superblocks(hf, NS)
        xs = np.concatenate([x[b, gs * 512:(gs + 1) * 512] for gs in sbs], 0)
        e_par = [1, 0] if hf == 0 else [0, 1]
        percore.append({"xT_in": np.ascontiguousarray(xs.T), "onehot": make_onehot(e_par), "cm": make_cm(e_par)})
    cores = list(range(ncores))
    if fused:
        parts = []
        for l in range(L):
            parts += [("a", l), ("b", l)]
        nc = _get_prog(T, L, parts, True, 0, ncores)
        res = run_bass_kernel_spmd(nc, [dict(shared, **pc) for pc in percore], core_ids=cores)
        outs = [r["xT_out"] for r in res.results]
    else:
        state = [pc["xT_in"] for pc in percore]
        small = {k: shared[k] for k in ("consts",)}
        for l in range(L):
            sm = dict(small)
            sm["normsT"] = np.ascontiguousarray(normsT[:, l * 24:(l + 1) * 24])
            sm["qkg"] = np.ascontiguousarray(qkg[:, l * 6:(l + 1) * 6])
            sm["dnorm"] = np.ascontiguousarray(dnorm[:, l:l + 1])
            sm["dlam"] = np.ascontiguousarray(dlam[:, l * 256:(l + 1) * 256])
            sa = dict(sm)
            for k in A_KEYS:
                sa[k] = shared[k][l:l + 1]
            nca = _get_prog(T, 1, [("a", 0)], False, 0)
            res = run_bass_kernel_spmd(nca, [dict(sa, xT_in=state[i]) for i in range(ncores)], core_ids=cores)
            ra = res.results
            sbm = dict(sm)
            for k in B_KEYS:
                sbm[k] = shared[k][l:l + 1]
            sbm["rel_bias"] = shared["rel_bias"]
            ncb = _get_prog(T, 1, [("b", 0)], False, l)
            maps = []
            for i, pc in enumerate(percore):
                p0 = (i // 2) * 2
                m = dict(sbm, onehot=pc["onehot"], cm=pc["cm"])
                m["xT_in"] = ra[i]["xT_out"]
                m["hT_i"] = ra[i]["hT_o"]
                m["QT_i"] = ra[i]["QT_o"]
                m["widx_i"] = ra[i]["widx_o"]
                k0, k1 = np.asarray(ra[p0]["KT_o"]), np.asarray(ra[p0 + 1]["KT_o"])
                m["KT_f"] = np.concatenate([np.concatenate([k0[g * 384:(g + 1) * 384], k1[g * 384:(g + 1) * 384]], 0)
                                            for g in range(4)], 0)
                v0, v1 = np.asarray(ra[p0]["V_o"]), np.asarray(ra[p0 + 1]["V_o"])
                m["V_f"] = np.concatenate([np.concatenate([v0[g * 512:(g + 1) * 512], v1[g * 512:(g + 1) * 512]], 0)
                                           for g in range(NT // 512)], 0)
                maps.append(m)
            res = run_bass_kernel_spmd(ncb, maps, core_ids=cores)
            state = [r["xT_out"] for r in res.results]
        outs = state
    out = np.zeros((B, T, D), np.float32)
    for c in range(ncores):
        b, hf = c // 2, c % 2
        sbs = local_superblocks(hf, NS)
        xo = np.asarray(outs[c]).T
        for i, gs in enumerate(sbs):
            out[b, gs * 512:(gs + 1) * 512] = xo[i * 512:(i + 1) * 512]
    return out


FUSED = True


def kernel(**inputs):
    x = np.asarray(inputs["x"])
    B, T, _ = x.shape
    L = np.asarray(inputs["w_in"]).shape[0]
    return run_model(inputs, T, L, B, fused=FUSED)
```

```python
import math
from contextlib import ExitStack
import numpy as np
import concourse.bass as bass
import concourse.mybir as mybir
from concourse.bass_utils import run_bass_kernel_spmd

F32 = mybir.dt.float32
BF16 = mybir.dt.bfloat16
AF = mybir.ActivationFunctionType
ALU = mybir.AluOpType
AX = mybir.AxisListType

D = 1024
DFF = 2816
NF = DFF // 128
NIN = 7880
EPS = 1e-6
BIG = 30000.0
LV = 3840
WD = 3712
VOFF = 511
NKIND = 4
DIL = ((128, 1), (512, 4), (2048, 16))
R_DMA = 8
MIX = "abc"


class Buf:
    __slots__ = ("w", "r")

    def __init__(self):
        self.w = None
        self.r = {}


class Eng:
    def __init__(self, name, h, sem, same):
        self.name = name
        self.h = h
        self.sem = sem
        self.cnt = 0
        self.seen = {}
        self.same = same


class Sched:
    def __init__(self, nc, es):
        self.nc = nc
        mk = lambda n: es.enter_context(nc.semaphore(n))
        self.pe = Eng("pe", nc.tensor, mk("s_pe"), False)
        self.act = Eng("act", nc.scalar, mk("s_act"), True)
        self.dve = Eng("dve", nc.vector, mk("s_dve"), True)
        self.pool = Eng("pool", nc.gpsimd, mk("s_pool"), True)
        self.sp = Eng("sp", nc.sync, mk("s_sp"), False)
        self.engs = [self.pe, self.act, self.dve, self.pool, self.sp]
        self.dq = {}
        for e in (self.sp, self.pool):
            self.dq[e.name] = {"sems": [mk(f"d_{e.name}{i}") for i in range(R_DMA)], "vals": [0] * R_DMA, "i": 0}
        self.n_ins = 0
        self.cc_sem = mk("cc_sem")
        self.cc_val = 0

    def _deps(self, reads, writes):
        d = {}

        def add(t):
            if t is None:
                return
            k, s, v = t
            if k not in d or d[k][1] < v:
                d[k] = (s, v)

        for b in reads:
            add(b.w)
        for b in writes:
            add(b.w)
            for t in b.r.values():
                add(t)
        return d

    def _filter(self, eng, deps):
        out = []
        for key, (sem, val) in deps.items():
            if key == eng.name and not eng.same:
                continue
            if eng.seen.get(key, 0) >= val:
                continue
            eng.seen[key] = val
            out.append((key, sem, val))
        out.sort(key=lambda t: 0 if t[0] == eng.name else 1)
        return out

    def _emit(self, eng, fn, waits):
        for (_, sem, val) in waits[1:]:
            eng.h.wait_ge(sem, val)
        ins = fn(eng.h)
        if waits:
            ins._wait_ge(waits[0][1], waits[0][2])
        self.n_ins += 1 + max(0, len(waits) - 1)
        return ins

    def op(self, eng, fn, reads=(), writes=()):
        waits = self._filter(eng, self._deps(reads, writes))
        ins = self._emit(eng, fn, waits)
        ins.then_inc(eng.sem, 1)
        eng.cnt += 1
        tok = (eng.name, eng.sem, eng.cnt)
        for b in reads:
            b.r[eng.name] = tok
        for b in writes:
            b.w = tok
            b.r = {}
        return tok

    def dma(self, eng, out, in_, reads=(), writes=(), **kw):
        q = self.dq[eng.name]
        i = q["i"]
        q["i"] = (i + 1) % R_DMA
        sem = q["sems"][i]
        key = f"d_{eng.name}{i}"
        deps = self._deps(reads, writes)
        if q["vals"][i] > 0:
            deps[key] = (sem, q["vals"][i])
        waits = self._filter(eng, deps)
        ins = self._emit(eng, lambda h: h.dma_start(out=out, in_=in_, **kw), waits)
        q["vals"][i] += 16
        ins.then_inc(sem, 16)
        tok = (key, sem, q["vals"][i])
        for b in reads:
            b.r[key] = tok
        for b in writes:
            b.w = tok
            b.r = {}
        return tok

    def drain(self, eng):
        for e in self.engs:
            if e.cnt > 0 and eng.seen.get(e.name, 0) < e.cnt and e is not eng:
                eng.h.wait_ge(e.sem, e.cnt)
                eng.seen[e.name] = e.cnt
        if self.cc_val > 0 and eng.seen.get("cc", 0) < self.cc_val:
            eng.h.wait_ge(self.cc_sem, self.cc_val)
            eng.seen["cc"] = self.cc_val
        for qn, q in self.dq.items():
            for i in range(R_DMA):
                key = f"d_{qn}{i}"
                if q["vals"][i] > 0 and eng.seen.get(key, 0) < q["vals"][i]:
                    eng.h.wait_ge(q["sems"][i], q["vals"][i])
                    eng.seen[key] = q["vals"][i]

    def barrier(self):
        for e in self.engs:
            self.drain(e)


def gpos(gs):
    r = 0 if gs % 4 in (0, 3) else 1
    li = gs // 2
    return r, li


def build(T, L, parts, fused, lbase=0, ncores=8):
    NT = T // 2
    NS = T // 512
    NM = NS // 2
    NTT = NT // 512
    nc = bass.Bass("TRN2", target_bir_lowering=False)

    def din(name, shape, dt=F32):
        return nc.dram_tensor(name, list(shape), dt, kind="ExternalInput").ap()

    def dout(name, shape, dt=F32):
        return nc.dram_tensor(name, list(shape), dt, kind="ExternalOutput").ap()

    def dscr(name, shape, dt, io):
        if fused:
            return nc.dram_tensor(name, list(shape), dt, kind="Internal").ap()
        return nc.dram_tensor(name, list(shape), dt, kind="ExternalInput" if io == "in" else "ExternalOutput").ap()

    has_a = any(p[0] == "a" for p in parts)
    has_b = any(p[0] == "b" for p in parts)
    first_is_b = parts[0][0] == "b"
    last_is_a = parts[-1][0] == "a"

    xT_in = din("xT_in", [D, NT])
    xT_out = dout("xT_out", [D, NT])
    consts = din("consts", [4, 128, 128])
    normsT = din("normsT", [128, L * 24])
    qkg = din("qkg", [128, L * 6])
    dnorm = din("dnorm", [128, L])
    dlam = din("dlam", [128, L * 256])
    w_gate = [din("ffn1_w_gate", [L, D, DFF]) if has_a else None, din("ffn2_w_gate", [L, D, DFF]) if has_b else None]
    w_up = [din("ffn1_w_up", [L, D, DFF]) if has_a else None, din("ffn2_w_up", [L, D, DFF]) if has_b else None]
    w_down = [din("ffn1_w_down", [L, DFF, D]) if has_a else None, din("ffn2_w_down", [L, DFF, D]) if has_b else None]
    w_in = din("w_in", [L, D, NIN])
    if has_b:
        w_br = [din("w_branch_a", [L, 256, D]), din("w_branch_b", [L, 512, D]), din("w_branch_c", [L, 256, D])]
        w_out = din("w_out", [L, D, D])
        rel_bias = din("rel_bias", [32, 20])
        onehot = din("onehot", [NKIND * 2, 33, LV])
        cm_in = din("cm", [2, 128, 1024])

    NQC = 16
    NKC = 12
    VW = 1344
    if fused:
        hT_s = dscr("hT_s", [8 * 128, NT], BF16, None)
        QT_s = dscr("QT_s", [NQC * 128, NT], BF16, None)
        KT_l2 = [dscr(f"KT_l{i}", [NKC * 128, NT], BF16, None) for i in range(2)]
        V_l2 = [dscr(f"V_l{i}", [NT, VW], BF16, None) for i in range(2)]
        KT_f2 = [dscr(f"KT_f{i}", [2 * NKC * 128, NT], BF16, None) for i in range(2)]
        V_f2 = [dscr(f"V_f{i}", [2 * NT, VW], BF16, None) for i in range(2)]
        KT_l, V_l, KT_f, V_f = KT_l2[0], V_l2[0], KT_f2[0], V_f2[0]
        hT_si = hT_s
        QT_si = QT_s
    else:
        if last_is_a:
            hT_s = dscr("hT_o", [8 * 128, NT], BF16, "out")
            QT_s = dscr("QT_o", [NQC * 128, NT], BF16, "out")
            KT_l = dscr("KT_o", [NKC * 128, NT], BF16, "out")
            V_l = dscr("V_o", [NT, VW], BF16, "out")
            widx_o = dscr("widx_o", [128, NT // 128 * 8], F32, "out")
        if first_is_b:
            hT_si = dscr("hT_i", [8 * 128, NT], BF16, "in")
            QT_si = dscr("QT_i", [NQC * 128, NT], BF16, "in")
            KT_f = dscr("KT_f", [2 * NKC * 128, NT], BF16, "in")
            V_f = dscr("V_f", [2 * NT, VW], BF16, "in")
            widx_i = dscr("widx_i", [128, NT // 128 * 8], F32, "in")
    vec_s = nc.dram_tensor("vec_s", [NKIND * 2 * 20, LV], BF16, kind="Internal").ap()

    es = ExitStack()
    uid = [0]
    with es:
        S = Sched(nc, es)
        pe, act, dve, pool, sp = S.pe, S.act, S.dve, S.pool, S.sp

        def sb(name, shape, dt):
            return es.enter_context(nc.sbuf_tensor(name, list(shape), dt))

        xT = sb("xT", [128, 8, NT], F32)
        XB = [Buf() for _ in range(NTT)]
        ps = es.enter_context(nc.psum_tensor("ps", [128, 8, 512], F32))
        PB = [Buf() for _ in range(8)]
        cst = sb("cst", [128, 4, 128], BF16)
        CB = Buf()
        ident, antiI, ones, bones = cst[:, 0, :], cst[:, 1, :], cst[:, 2, :], cst[:, 3, :]
        g32 = sb("g32", [128, L * 24], F32)
        qkgs = sb("qkgs", [128, L * 6], F32)
        dnrm = sb("dnrm", [128, L], F32)
        lamt = sb("lamt", [128, L * 256], F32)
        neglam = sb("neglam", [128, L], F32)
        widx = sb("widx", [128, NT // 128 * 8], F32)
        WIB = Buf()
        GB = Buf()

        S.dma(pool, cst[:], consts.rearrange("k p n -> p k n"), writes=[CB])
        epsb = sb("epsb", [128, 3], F32)
        ci = {1024.0 * EPS: 0, 64.0 * EPS: 1, 128.0 * EPS: 2}
        for cval, cidx in ci.items():
            S.op(dve, lambda h, cval=cval, cidx=cidx: h.memset(epsb[:, cidx:cidx + 1], cval), writes=[CB])
        for tt in range(NTT):
            S.dma(sp, xT[:, :, tt * 512:(tt + 1) * 512],
                  xT_in.rearrange("(c p) n -> p c n", p=128)[:, :, tt * 512:(tt + 1) * 512], writes=[XB[tt]])
        S.dma(sp, g32[:], normsT, writes=[GB])
        S.dma(sp, qkgs[:], qkg, writes=[GB])
        S.dma(sp, dnrm[:], dnorm, writes=[GB])
        S.dma(sp, lamt[:], dlam, writes=[GB])
        S.op(dve, lambda h: h.tensor_scalar(out=g32[:], in0=g32[:], scalar1=32.0, scalar2=None, op0=ALU.mult),
             reads=[GB], writes=[GB])
        for l in range(L):
            for i in range(3):
                c = l * 6 + i * 2 + 1
                S.op(dve, lambda h, c=c: h.tensor_scalar(out=qkgs[:, c:c + 1], in0=qkgs[:, c:c + 1], scalar1=8.0,
                                                         scalar2=None, op0=ALU.mult), reads=[GB], writes=[GB])
        if has_b:
            ltmp = sb("ltmp", [128, 64], F32)
            lsum = sb("lsum", [128, 4], F32)
            LB = Buf()
            for l in range(L):
                lam_init = 0.8 - 0.6 * math.exp(-0.3 * (l + lbase))
                for k in range(2):
                    a0 = l * 256 + k * 128
                    S.op(dve, lambda h, a0=a0: h.tensor_tensor(out=ltmp[:], in0=lamt[:, a0:a0 + 64],
                                                               in1=lamt[:, a0 + 64:a0 + 128], op=ALU.mult),
                         reads=[GB], writes=[LB])
                    S.op(dve, lambda h, k=k: h.tensor_reduce(out=lsum[:, k:k + 1], in_=ltmp[:], axis=AX.X, op=ALU.add),
                         reads=[LB], writes=[LB])
                    S.op(act, lambda h, k=k: h.activation(out=lsum[:, 2 + k:3 + k], in_=lsum[:, k:k + 1], func=AF.Exp),
                         reads=[LB], writes=[LB])
                S.op(dve, lambda h, l=l, li=lam_init: h.scalar_tensor_tensor(
                    out=neglam[:, l:l + 1], in0=lsum[:, 3:4], scalar=-li, in1=lsum[:, 2:3], op0=ALU.add,
                    op1=ALU.subtract), reads=[LB], writes=[GB])
                S.op(dve, lambda h, l=l, li=lam_init: h.tensor_scalar(
                    out=dnrm[:, l:l + 1], in0=dnrm[:, l:l + 1], scalar1=(1.0 - li) * math.sqrt(128.0), scalar2=None,
                    op0=ALU.mult), reads=[GB], writes=[GB])

        bank_rr = {"i": 0}

        def mm(out, lhsT, rhs, start, stop, reads, writes):
            S.op(pe, lambda h: h.matmul(out, lhsT, rhs, start=start, stop=stop), reads=reads, writes=writes)

        def rsq(out, in_, c, reads, wbuf):
            S.op(act, lambda h: h.activation(out=out, in_=in_, func=AF.Sqrt, bias=epsb[0:out.shape[0], ci[c]:ci[c] + 1], scale=1.0),
                 reads=list(reads) + [CB], writes=[wbuf])
            S.op(dve, lambda h: h.reciprocal(out=out, in_=out), reads=[wbuf], writes=[wbuf])

        def emit_rms(gcol0, tgs, hT, HB, tok0, sq, SQB, rstd, RB, banks):
            for n, tg in enumerate(tgs):
                b = banks[n % len(banks)]
                tok = slice(tg * 512, tg * 512 + 512)
                lt = slice((tg - tok0) * 512, (tg - tok0) * 512 + 512)
                for c in range(8):
                    k = c % 2
                    S.op(act, lambda h, c=c, k=k: h.activation(out=sq[:, k, :], in_=xT[:, c, tok], func=AF.Square),
                         reads=[XB[tg]], writes=[SQB[k]])
                    mm(ps[:, b, :], ones, sq[:, k, :], c == 0, c == 7, [SQB[k], CB], [PB[b]])
                rsq(rstd[:], ps[:, b, :], 1024.0 * EPS, [PB[b]], RB)
                for c in range(8):
                    S.op(dve, lambda h, c=c: h.scalar_tensor_tensor(
                        out=hT[:, c, lt], in0=xT[:, c, tok], scalar=g32[:, gcol0 + c:gcol0 + c + 1], in1=rstd[:],
                        op0=ALU.mult, op1=ALU.mult), reads=[XB[tg], RB, GB], writes=[HB[tg - tok0]])

        def emit_ffn(l, which):
            with ExitStack() as fs:
                def fsb(name, shape, dt):
                    uid[0] += 1
                    return fs.enter_context(nc.sbuf_tensor(f"{name}_{uid[0]}", list(shape), dt))
                NH = 1024 if NT >= 1024 else NT
                ntt = NH // 512
                hT = fsb("f_hT", [128, 8, NH], BF16)
                aT = fsb("f_aT", [128, NF, NH], BF16)
                sq = fsb("f_sq", [128, 2, 512], BF16)
                rstd = fsb("f_rstd", [128, 512], F32)
                sg = fsb("f_sg", [128, 2, 512], F32)
                wg = fsb("f_wg", [128, 3, 8, 128], BF16)
                wu = fsb("f_wu", [128, 3, 8, 128], BF16)
                wd = fsb("f_wd", [128, 2, NF, 128], BF16)
                HB = [Buf() for _ in range(ntt)]
                AB = [[Buf() for _ in range(ntt)] for _ in range(NF)]
                SQB = [Buf(), Buf()]
                RB = Buf()
                SGB = [Buf(), Buf()]
                WGB = [Buf() for _ in range(3)]
                WUB = [Buf() for _ in range(3)]
                WDB = [Buf(), Buf()]
                wgv = w_gate[which][l].rearrange("(c p) n -> p c n", p=128)
                wuv = w_up[which][l].rearrange("(c p) n -> p c n", p=128)
                wdv = w_down[which][l].rearrange("(j p) n -> p j n", p=128)
                gcol0 = l * 24 + (0 if which == 0 else 16)
                nsg = 0
                for th in range(NT // NH):
                    tgs = [th * ntt + i for i in range(ntt)]
                    emit_rms(gcol0, tgs, hT, HB, th * ntt, sq, SQB, rstd, RB, [6, 7])
                    for j in range(NF):
                        k3 = j % 3
                        S.dma(pool, wg[:, k3], wgv[:, :, j * 128:(j + 1) * 128], writes=[WGB[k3]])
                        S.dma(pool, wu[:, k3], wuv[:, :, j * 128:(j + 1) * 128], writes=[WUB[k3]])
                        for tt in range(ntt):
                            bg = (j * ntt + tt) % 2
                            bu = 2 + (j * ntt + tt) % 2
                            tl = slice(tt * 512, tt * 512 + 512)
                            for c in range(8):
                                mm(ps[:, bg, :], wg[:, k3, c, :], hT[:, c, tl], c == 0, c == 7, [WGB[k3], HB[tt]], [PB[bg]])
                            for c in range(8):
                                mm(ps[:, bu, :], wu[:, k3, c, :], hT[:, c, tl], c == 0, c == 7, [WUB[k3], HB[tt]], [PB[bu]])
                            k = nsg % 2
                            nsg += 1
                            S.op(act, lambda h, k=k, bg=bg: h.activation(out=sg[:, k, :], in_=ps[:, bg, :], func=AF.Silu),
                                 reads=[PB[bg]], writes=[SGB[k]])
                            S.op(dve, lambda h, k=k, bu=bu, j=j, tl=tl: h.tensor_tensor(
                                out=aT[:, j, tl], in0=sg[:, k, :], in1=ps[:, bu, :], op=ALU.mult),
                                reads=[SGB[k], PB[bu]], writes=[AB[j][tt]])
                    for oc in range(8):
                        k2 = oc % 2
                        S.dma(pool, wd[:, k2], wdv[:, :, oc * 128:(oc + 1) * 128], writes=[WDB[k2]])
                        for tt in range(ntt):
                            bo = 4 + (oc * ntt + tt) % 2
                            tg = th * ntt + tt
                            tl = slice(tt * 512, tt * 512 + 512)
                            tok = slice(tg * 512, tg * 512 + 512)
                            for j in range(NF):
                                mm(ps[:, bo, :], wd[:, k2, j, :], aT[:, j, tl], j == 0, j == NF - 1,
                                   [WDB[k2], AB[j][tt]], [PB[bo]])
                            S.op(dve, lambda h, bo=bo, oc=oc, tok=tok: h.scalar_tensor_tensor(
                                out=xT[:, oc, tok], in0=ps[:, bo, :], scalar=0.5, in1=xT[:, oc, tok], op0=ALU.mult,
                                op1=ALU.add), reads=[PB[bo], XB[tg]], writes=[XB[tg]])
                S.barrier()

        def emit_pre(l):
            with ExitStack() as fs:
                def fsb(name, shape, dt):
                    uid[0] += 1
                    return fs.enter_context(nc.sbuf_tensor(f"{name}_{uid[0]}", list(shape), dt))
                hT = fsb("p_hT", [128, 8, NT], BF16)
                sq = fsb("p_sq", [128, 2, 512], BF16)
                rstd = fsb("p_rstd", [128, 512], F32)
                wt = fsb("p_wt", [128, 3, 8, 128], BF16)
                st = fsb("p_st", [128, 2, NT], BF16)
                wv = fsb("p_wv", [128, 8, VW + 8], BF16)
                vst = fsb("p_vst", [128, 2, VW], BF16)
                HB = [Buf() for _ in range(NTT)]
                SQB = [Buf(), Buf()]
                RB = Buf()
                WTB = [Buf() for _ in range(3)]
                STB = [Buf(), Buf()]
                WVB = Buf()
                VSB = [Buf(), Buf()]
                winv = w_in[l].rearrange("(c p) n -> p c n", p=128)
                emit_rms(l * 24 + 8, list(range(NTT)), hT, HB, 0, sq, SQB, rstd, RB, [6, 7])
                for tt in range(NTT):
                    S.dma(sp, hT_s.rearrange("(c p) n -> p c n", p=128)[:, :, tt * 512:(tt + 1) * 512],
                          hT[:, :, tt * 512:(tt + 1) * 512], reads=[HB[tt]])
                S.dma(pool, wv[:, :, 0:768], winv[:, :, 1536:2304], writes=[WVB])
                S.dma(pool, wv[:, :, 768:1280], winv[:, :, 3328:3840], writes=[WVB])
                S.dma(pool, wv[:, :, 1280:1344], winv[:, :, 4160:4224], writes=[WVB])
                S.dma(pool, wv[:, :, 1344:1352], winv[:, :, 4800:4808], writes=[WVB])
                qc = l * 6
                chunks = []
                for i in range(6):
                    chunks.append(("n", i * 128, None, qc + 0, QT_s, i))
                for i in range(6):
                    chunks.append(("n", 768 + i * 128, None, qc + 1, KT_l, i))
                for i in range(4):
                    chunks.append(("n", 2304 + i * 128, None, qc + 2, QT_s, 6 + i))
                for i in range(4):
                    chunks.append(("n", 2816 + i * 128, None, qc + 3, KT_l, 6 + i))
                for i in range(2):
                    chunks.append(("n", 3840 + i * 128, None, qc + 4, QT_s, 10 + i))
                chunks.append(("ck", 4096, 4736, qc + 5, KT_l, 10))
                for i in range(4):
                    chunks.append(("p", 4224 + i * 128, None, None, QT_s, 12 + i))
                for ci, (kind, c0, c1, gcol, dst, dchunk) in enumerate(chunks):
                    k3 = ci % 3
                    if kind == "ck":
                        S.dma(pool, wt[:, k3, :, 0:64], winv[:, :, c0:c0 + 64], writes=[WTB[k3]])
                        S.dma(pool, wt[:, k3, :, 64:128], winv[:, :, c1:c1 + 64], writes=[WTB[k3]])
                    else:
                        S.dma(pool, wt[:, k3], winv[:, :, c0:c0 + 128], writes=[WTB[k3]])
                    ks = ci % 2
                    for tt in range(NTT):
                        b = (ci * NTT + tt) % 2
                        tl = slice(tt * 512, tt * 512 + 512)
                        for c in range(8):
                            mm(ps[:, b, :], wt[:, k3, c, :], hT[:, c, tl], c == 0, c == 7, [WTB[k3], HB[tt]], [PB[b]])
                        if kind == "p":
                            S.op(act, lambda h, b=b, ks=ks, tl=tl: h.activation(out=st[:, ks, tl], in_=ps[:, b, :],
                                                                                func=AF.Copy, scale=0.125),
                                 reads=[PB[b]], writes=[STB[ks]])
                            continue
                        np_ = 64 if kind == "ck" else 128
                        kq = (ci * NTT + tt) % 2
                        b2 = 2 + (ci * NTT + tt) % 2
                        S.op(act, lambda h, b=b, kq=kq, np_=np_: h.activation(out=sq[0:np_, kq, :], in_=ps[0:np_, b, :],
                                                                               func=AF.Square),
                             reads=[PB[b]], writes=[SQB[kq]])
                        mm(ps[0:np_, b2, :], bones[0:np_, 0:np_], sq[0:np_, kq, :], True, True, [SQB[kq], CB], [PB[b2]])
                        rsq(rstd[0:np_, :], ps[0:np_, b2, :], 64.0 * EPS, [PB[b2]], RB)
                        S.op(dve, lambda h, b=b, ks=ks, tl=tl, np_=np_, gcol=gcol: h.scalar_tensor_tensor(
                            out=st[0:np_, ks, tl], in0=ps[0:np_, b, :], scalar=qkgs[0:np_, gcol:gcol + 1],
                            in1=rstd[0:np_, :], op0=ALU.mult, op1=ALU.mult), reads=[PB[b], RB, GB], writes=[STB[ks]])
                        if kind == "ck":
                            S.op(act, lambda h, b=b, ks=ks, tl=tl: h.activation(
                                out=st[64:128, ks, tl], in_=ps[64:128, b, :], func=AF.Copy), reads=[PB[b]],
                                writes=[STB[ks]])
                    S.dma(sp, dst[dchunk * 128:(dchunk + 1) * 128, :], st[:, ks, :], reads=[STB[ks]])
                for t128 in range(NT // 128):
                    tl = slice(t128 * 128, t128 * 128 + 128)
                    kv = t128 % 2
                    groups = [(0, 512), (512, 768), (768, 1280), (1280, 1352)]
                    for gi, (a0, a1) in enumerate(groups):
                        b = 4 + (t128 * 4 + gi) % 4
                        n = a1 - a0
                        for c in range(8):
                            mm(ps[:, b, 0:n], hT[:, c, tl], wv[:, c, a0:a1], c == 0, c == 7, [HB[t128 // 4], WVB], [PB[b]])
                        if gi < 3:
                            eng = act if gi % 2 == 0 else dve
                            if eng is act:
                                S.op(act, lambda h, b=b, n=n, a0=a0, a1=a1, kv=kv: h.activation(
                                    out=vst[:, kv, a0:a1], in_=ps[:, b, 0:n], func=AF.Copy), reads=[PB[b]], writes=[VSB[kv]])
                            else:
                                S.op(dve, lambda h, b=b, n=n, a0=a0, a1=a1, kv=kv: h.tensor_copy(
                                    out=vst[:, kv, a0:a1], in_=ps[:, b, 0:n]), reads=[PB[b]], writes=[VSB[kv]])
                        else:
                            S.op(act, lambda h, b=b, kv=kv: h.activation(
                                out=vst[:, kv, 1280:1344], in_=ps[:, b, 0:64], func=AF.Copy), reads=[PB[b]], writes=[VSB[kv]])
                            S.op(dve, lambda h, b=b, t128=t128: h.tensor_scalar(
                                out=widx[:, t128 * 8:t128 * 8 + 8], in0=ps[:, b, 64:72], scalar1=8.0 ** -0.5,
                                scalar2=None, op0=ALU.mult), reads=[PB[b]], writes=[WIB])
                    S.dma(sp, V_l[tl, :], vst[:, kv, :], reads=[VSB[kv]])
                if not fused:
                    S.dma(sp, widx_o, widx[:], reads=[WIB])
                S.barrier()

        def emit_vec():
            with ExitStack() as fs:
                def fsb(name, shape, dt):
                    uid[0] += 1
                    return fs.enter_context(nc.sbuf_tensor(f"{name}_{uid[0]}", list(shape), dt))
                tbl = fsb("v_tbl", [33, 20], F32)
                oh = fsb("v_oh", [33, 2, 512], F32)
                vs = fsb("v_vs", [20, 2, 512], BF16)
                TB = Buf()
                OB = [Buf(), Buf()]
                VB = [Buf(), Buf()]
                S.op(dve, lambda h: h.memset(tbl[:], -BIG), writes=[TB])
                S.dma(sp, tbl[0:32, :], rel_bias, writes=[TB])
                n = 0
                for kp in range(NKIND * 2):
                    for c0 in range(0, LV, 512):
                        cw = min(512, LV - c0)
                        k = n % 2
                        n += 1
                        S.dma(sp, oh[:, k, 0:cw], onehot[kp, :, c0:c0 + cw], writes=[OB[k]])
                        b = k
                        mm(ps[0:20, b, 0:cw], tbl[:], oh[:, k, 0:cw], True, True, [TB, OB[k]], [PB[b]])
                        S.op(act, lambda h, b=b, k=k, cw=cw: h.activation(out=vs[:, k, 0:cw], in_=ps[0:20, b, 0:cw],
                                                                          func=AF.Copy), reads=[PB[b]], writes=[VB[k]])
                        S.dma(sp, vec_s.rearrange("(k h) n -> k h n", h=20)[kp, :, c0:c0 + cw], vs[:, k, 0:cw],
                              reads=[VB[k]])
                S.barrier()

        def strip_src(kind, par, head, width):
            row = (kind * 2 + par) * 20 + head
            return bass.AP(tensor=vec_s.tensor, offset=row * LV, ap=[[1, 128], [1, width]])

        def kcol(kt):
            gs = kt // 4
            r, li = gpos(gs)
            return r * NT + li * 512 + (kt % 4) * 128

        def vtile(kt):
            gs = kt // 4
            r, li = gpos(gs)
            return li * 8 + r * 4 + kt % 4

        def emit_att(l, oT_a, oT_b, OAB, OBB):
            with ExitStack() as fs:
                def fsb(name, shape, dt):
                    uid[0] += 1
                    return fs.enter_context(nc.sbuf_tensor(f"{name}_{uid[0]}", list(shape), dt))
                KT = fsb("a_KT", [64, 2, 2 * NT], BF16)
                QTt = fsb("a_QT", [64, 2, NT], BF16)
                Vt = fsb("a_V", [128, 2 * NT // 128, 128], BF16)
                G = fsb("a_G", [128, 2, WD], BF16)
                Pt = fsb("a_P", [128, 3, 512], BF16)
                ev = fsb("a_ev", [128, 4, 512], F32)
                sqb = fsb("a_sq", [128, 512], BF16)
                KB = [Buf(), Buf()]
                QB = [Buf(), Buf()]
                VB = Buf()
                GBf = Buf()
                PtB = [Buf() for _ in range(3)]
                EB = [Buf() for _ in range(4)]
                SQ = Buf()
                KTf = KT_f.rearrange("(g r c p) n -> g c p r n", g=4, r=2, c=3, p=128)
                QTi = QT_si.rearrange("(c p) n -> c p n", p=128)
                Vf = V_f.rearrange("(t p) c -> p t c", p=128)
                npt = [0]

                def attend(m, terms, nmap, dv):
                    first = {0: True, 1: True}
                    total = {0: 0, 1: 0}
                    for (mp, ksl, qsl, kts, c0f) in terms:
                        total[mp] += len(kts)
                    cnt = {0: 0, 1: 0}
                    for (mp, ksl, qsl, kts, c0f) in terms:
                        for kt in kts:
                            k = npt[0] % 3
                            npt[0] += 1
                            b = k
                            kc = kcol(kt)
                            mm(ps[:, b, :], KT[:, ksl, kc:kc + 128], QTt[:, qsl, m * 512:(m + 1) * 512], True, False,
                               [KB[ksl], QB[qsl]], [PB[b]])
                            c0 = c0f(kt)
                            mm(ps[:, b, :], antiI, G[:, m % 2, c0:c0 + 512], False, True, [GBf, CB], [PB[b]])
                            S.op(act, lambda h, b=b, k=k: h.activation(out=Pt[:, k, :], in_=ps[:, b, :], func=AF.Exp),
                                 reads=[PB[b]], writes=[PtB[k]])
                            cnt[mp] += 1
                            st_, sp_ = cnt[mp] == 1, cnt[mp] == total[mp]
                            mm(ps[0:dv, 3 + mp, :], Vt[:, vtile(kt), 0:dv], Pt[:, k, :], st_, sp_, [VB, PtB[k]], [PB[3 + mp]])
                            mm(ps[0:dv, 5 + mp, :], ones[:, 0:dv], Pt[:, k, :], st_, sp_, [CB, PtB[k]], [PB[5 + mp]])

                for s in range(4):
                    for m in range(NM):
                        gsN = 2 * m + 1
                        terms = []
                        first = True
                        ngrp = 0
                        kt_lists = []
                        for g, (w, d) in enumerate(DIL):
                            kts = [kt for kt in range((gsN + 1) * 4)
                                   if -384 <= gsN * 512 - 128 * kt <= w + 639]
                            kt_lists.append(kts)
                        tot = sum(len(k) for k in kt_lists)
                        cnt = 0
                        for g, (w, d) in enumerate(DIL):
                            head = 4 * g + s
                            ch, hf_ = head // 2, head % 2
                            sl = (s * 3 * NM + m * 3 + g) % 2
                            S.dma(sp, KT[:, sl, :].rearrange("p (r n) -> p r n", r=2), KTf[ch // 3, ch % 3, hf_ * 64:hf_ * 64 + 64],
                                  writes=[KB[sl]])
                            S.dma(sp, QTt[:, sl, :], QTi[ch, hf_ * 64:hf_ * 64 + 64, :], writes=[QB[sl]])
                            if m == 0 or True:
                                S.dma(sp, Vt[:, :, 0:64], Vf[:, :, head * 64:head * 64 + 64], writes=[VB])
                            wdt = min(WD, w + 639 + 384 + 512 + 128)
                            for par in range(2):
                                S.dma(sp, G[:, par, 0:wdt], strip_src(g, par, head, wdt), writes=[GBf])
                            for kt in kt_lists[g]:
                                k = npt[0] % 3
                                npt[0] += 1
                                b = k
                                kc = kcol(kt)
                                mm(ps[:, b, :], KT[:, sl, kc:kc + 128], QTt[:, sl, m * 512:(m + 1) * 512], True, False,
                                   [KB[sl], QB[sl]], [PB[b]])
                                c0 = gsN * 512 - 128 * kt + 384
                                mm(ps[:, b, :], antiI, G[:, m % 2, c0:c0 + 512], False, True, [GBf, CB], [PB[b]])
                                S.op(act, lambda h, b=b, k=k: h.activation(out=Pt[:, k, :], in_=ps[:, b, :], func=AF.Exp),
                                     reads=[PB[b]], writes=[PtB[k]])
                                cnt += 1
                                mm(ps[0:64, 3, :], Vt[:, vtile(kt), 0:64], Pt[:, k, :], cnt == 1, cnt == tot,
                                   [VB, PtB[k]], [PB[3]])
                                mm(ps[0:64, 5, :], ones[:, 0:64], Pt[:, k, :], cnt == 1, cnt == tot, [CB, PtB[k]], [PB[5]])
                        S.op(dve, lambda h: h.reciprocal(out=ev[0:64, 0, :], in_=ps[0:64, 5, :]), reads=[PB[5]], writes=[EB[0]])
                        S.op(dve, lambda h, s=s, m=m: h.tensor_tensor(out=oT_a[:, s, m * 512:(m + 1) * 512],
                                                                       in0=ps[0:64, 3, :], in1=ev[0:64, 0, :], op=ALU.mult),
                             reads=[PB[3], EB[0]], writes=[OAB])
                for hb in range(4):
                    ch, hf_ = hb // 2, hb % 2
                    for mp in range(2):
                        S.dma(sp, KT[:, mp, :].rearrange("p (r n) -> p r n", r=2),
                              KTf[(6 + 2 * mp + ch) // 3, (6 + 2 * mp + ch) % 3, hf_ * 64:hf_ * 64 + 64], writes=[KB[mp]])
                        S.dma(sp, QTt[:, mp, :], QTi[6 + 2 * mp + ch, hf_ * 64:hf_ * 64 + 64, :], writes=[QB[mp]])
                    S.dma(sp, Vt[:], Vf[:, :, 768 + hb * 128:768 + hb * 128 + 128], writes=[VB])
                    for par in range(2):
                        S.dma(sp, G[:, par, :], strip_src(3, par, 12 + hb, WD), writes=[GBf])
                    for m in range(NM):
                        gsN = 2 * m + 1
                        nkt = (gsN + 1) * 4
                        for kt in range(nkt):
                            D0 = gsN * 512 - 128 * kt
                            c0 = D0 + 384 if D0 < 2176 else 2560
                            kc = kcol(kt)
                            for mp in range(2):
                                k = npt[0] % 3
                                npt[0] += 1
                                b = k
                                mm(ps[:, b, :], KT[:, mp, kc:kc + 128], QTt[:, mp, m * 512:(m + 1) * 512], True, False,
                                   [KB[mp], QB[mp]], [PB[b]])
                                mm(ps[:, b, :], antiI, G[:, m % 2, c0:c0 + 512], False, True, [GBf, CB], [PB[b]])
                                S.op(act, lambda h, b=b, k=k: h.activation(out=Pt[:, k, :], in_=ps[:, b, :], func=AF.Exp),
                                     reads=[PB[b]], writes=[PtB[k]])
                                mm(ps[:, 3 + mp, :], Vt[:, vtile(kt), :], Pt[:, k, :], kt == 0, kt == nkt - 1,
                                   [VB, PtB[k]], [PB[3 + mp]])
                                mm(ps[:, 5 + mp, :], ones, Pt[:, k, :], kt == 0, kt == nkt - 1, [CB, PtB[k]], [PB[5 + mp]])
                        for mp in range(2):
                            S.op(dve, lambda h, mp=mp: h.reciprocal(out=ev[:, mp, :], in_=ps[:, 5 + mp, :]),
                                 reads=[PB[5 + mp]], writes=[EB[mp]])
                            S.op(dve, lambda h, mp=mp: h.tensor_tensor(out=ev[:, mp, :], in0=ps[:, 3 + mp, :],
                                                                       in1=ev[:, mp, :], op=ALU.mult),
                                 reads=[PB[3 + mp], EB[mp]], writes=[EB[mp]])
                        S.op(dve, lambda h: h.scalar_tensor_tensor(out=ev[:, 2, :], in0=ev[:, 1, :],
                                                                   scalar=neglam[:, l:l + 1], in1=ev[:, 0, :],
                                                                   op0=ALU.mult, op1=ALU.add),
                             reads=[EB[0], EB[1], GB], writes=[EB[2]])
                        S.op(act, lambda h: h.activation(out=sqb[:], in_=ev[:, 2, :], func=AF.Square), reads=[EB[2]],
                             writes=[SQ])
                        mm(ps[:, 7, :], ones, sqb[:], True, True, [SQ, CB], [PB[7]])
                        rsq(ev[:, 3, :], ps[:, 7, :], 128.0 * EPS, [PB[7]], EB[3])
                        S.op(dve, lambda h, hb=hb, m=m: h.scalar_tensor_tensor(
                            out=oT_b[:, hb, m * 512:(m + 1) * 512], in0=ev[:, 2, :], scalar=dnrm[:, l:l + 1],
                            in1=ev[:, 3, :], op0=ALU.mult, op1=ALU.mult), reads=[EB[2], EB[3], GB], writes=[OBB])
                S.barrier()

        WC = 3072

        def emit_dsa(l, oT_c, OCB):
            with ExitStack() as fs:
                def fsb(name, shape, dt):
                    uid[0] += 1
                    return fs.enter_context(nc.sbuf_tensor(f"{name}_{uid[0]}", list(shape), dt))
                KK = fsb("c_KK", [128, 2 * NT], BF16)
                QQ = fsb("c_QQ", [128, 8, 512], BF16)
                Vc = fsb("c_V", [128, 2 * NT // 128, 64], BF16)
                G = fsb("c_G", [128, 4, WC], BF16)
                cmb = fsb("c_cmb", [128, 2, 1024], BF16)
                sc = fsb("c_sc", [128, 2, T], F32)
                rlb = fsb("c_rl", [128, 3, 512], BF16)
                dg = fsb("c_dg", [128, 2, 8, 128], BF16)
                mx = fsb("c_mx", [128, 8], F32)
                mneg = fsb("c_mneg", [128, T], BF16)
                Pt = fsb("c_P", [128, 2, 512], BF16)
                ev = fsb("c_ev", [64, 512], F32)
                B_ld = Buf()
                QLB = Buf()
                GLB = Buf()
                SCB = [Buf(), Buf()]
                RLB = [Buf(), Buf(), Buf()]
                DGB = [Buf(), Buf()]
                MXB = Buf()
                MNB = Buf()
                PtB = [Buf(), Buf()]
                EVB = Buf()
                KTf = KT_f.rearrange("(g r c p) n -> g c p r n", g=4, r=2, c=3, p=128)
                QTi = QT_si.rearrange("(c p) n -> c p n", p=128)
                Vf = V_f.rearrange("(t p) c -> p t c", p=128)
                S.dma(sp, KK[:, :].rearrange("p (r n) -> p r n", r=2), KTf[3, 1], writes=[B_ld])
                S.dma(sp, Vc[:], Vf[:, :, 1280:1344], writes=[B_ld])
                S.dma(pool, cmb[:], cm_in.rearrange("k p n -> p k n"), writes=[B_ld])
                nq = 0
                nr = 0
                nacc = 0
                for m in range(NM):
                    gsN = 2 * m + 1
                    nkeys = (gsN + 1) * 512
                    nch = nkeys // 512
                    ms = slice(m * 512, m * 512 + 512)
                    for i in range(8):
                        S.dma(sp, QQ[64:128, i, :], QTi[12 + i // 2, (i % 2) * 64:(i % 2) * 64 + 64, ms], writes=[QLB])
                    for i in range(4):
                        S.dma(sp, QQ[0:64, i, :], QTi[10 + i // 2, (i % 2) * 64:(i % 2) * 64 + 64, ms], writes=[QLB])
                        S.dma(sp, G[:, i, :], strip_src(3, m % 2, 16 + i, WC), writes=[GLB])
                    for qb in range(4):
                        ql = slice(qb * 128, qb * 128 + 128)
                        q0 = m * 512 + qb * 128
                        t128 = q0 // 128
                        ks = nq % 2
                        nq += 1
                        for hh in range(8):
                            wcol = widx[:, t128 * 8 + hh:t128 * 8 + hh + 1]
                            S.op(pool, lambda h, ks=ks, hh=hh, wcol=wcol: h.tensor_scalar(
                                out=dg[:, ks, hh, :], in0=ident, scalar1=wcol, scalar2=None, op0=ALU.mult),
                                reads=[CB, WIB], writes=[DGB[ks]])
                        for kc in range(nch):
                            r, li = gpos(kc)
                            col = r * NT + li * 512
                            ab = 6 + nacc % 2
                            nacc += 1
                            for hh in range(8):
                                b = nr % 2
                                k = nr % 3
                                nr += 1
                                mm(ps[:, b, :], QQ[64:128, hh, ql], KK[64:128, col:col + 512],
                                   True, True, [B_ld, QLB], [PB[b]])
                                S.op(act, lambda h, b=b, k=k: h.activation(out=rlb[:, k, :], in_=ps[:, b, :], func=AF.Relu),
                                     reads=[PB[b]], writes=[RLB[k]])
                                mm(ps[:, ab, :], dg[:, ks, hh, :], rlb[:, k, :], hh == 0, hh == 7, [DGB[ks], RLB[k]], [PB[ab]])
                            S.op(act, lambda h, ab=ab, ks=ks, kc=kc: h.activation(
                                out=sc[:, ks, kc * 512:(kc + 1) * 512], in_=ps[:, ab, :], func=AF.Copy),
                                reads=[PB[ab]], writes=[SCB[ks]])
                        lo = gsN * 512 + 128 * qb - 512
                        hi = nkeys
                        S.op(dve, lambda h, ks=ks, lo=lo, hi=hi, m=m: h.tensor_tensor(
                            out=sc[:, ks, lo:hi], in0=sc[:, ks, lo:hi], in1=cmb[:, m % 2, 0:hi - lo], op=ALU.add),
                            reads=[SCB[ks], B_ld], writes=[SCB[ks]])
                        for it in range(32):
                            S.op(dve, lambda h, ks=ks: h.max(out=mx[:], in_=sc[:, ks, 0:nkeys]), reads=[SCB[ks]], writes=[MXB])
                            S.op(dve, lambda h, ks=ks: h.match_replace(out=sc[:, ks, 0:nkeys], in_to_replace=mx[:],
                                                                       in_values=sc[:, ks, 0:nkeys], imm_value=-3.0e38),
                                 reads=[SCB[ks], MXB], writes=[SCB[ks]])
                        S.op(dve, lambda h, ks=ks: h.tensor_scalar(out=mneg[:, 0:nkeys], in0=sc[:, ks, 0:nkeys],
                                                                   scalar1=-1.0e38, scalar2=-BIG, op0=ALU.is_gt,
                                                                   op1=ALU.mult), reads=[SCB[ks]], writes=[MNB])
                        S.op(dve, lambda h, lo=lo, hi=hi, m=m: h.tensor_tensor(
                            out=mneg[:, lo:hi], in0=mneg[:, lo:hi], in1=cmb[:, m % 2, 0:hi - lo], op=ALU.add),
                            reads=[MNB, B_ld], writes=[MNB])
                        nkt = nkeys // 128
                        for kt in range(nkt):
                            kcg = kcol(kt)
                            D0 = gsN * 512 - 128 * kt
                            c0 = (D0 + 384 if D0 < 2176 else 2560) + 128 * qb
                            b = 2 + kt % 2
                            k = kt % 2
                            for hc in range(4):
                                o = ps[:, b, hc * 128:(hc + 1) * 128]
                                mm(o, KK[0:64, kcg:kcg + 128], QQ[0:64, hc, ql], True, False, [B_ld, QLB], [PB[b]])
                                mm(o, antiI, G[:, hc, c0:c0 + 128], False, False, [GLB, CB], [PB[b]])
                                mm(o, mneg[:, kt * 128:(kt + 1) * 128], ident, False, True, [MNB, CB], [PB[b]])
                            S.op(act, lambda h, b=b, k=k: h.activation(out=Pt[:, k, :], in_=ps[:, b, :], func=AF.Exp),
                                 reads=[PB[b]], writes=[PtB[k]])
                            for hc in range(4):
                                mm(ps[0:64, 4, hc * 128:(hc + 1) * 128], Vc[:, vtile(kt), :],
                                   Pt[:, k, hc * 128:(hc + 1) * 128], kt == 0 and hc == 0, kt == nkt - 1,
                                   [B_ld, PtB[k]], [PB[4]])
                            mm(ps[0:64, 5, :], ones[:, 0:64], Pt[:, k, :], kt == 0, kt == nkt - 1, [CB, PtB[k]], [PB[5]])
                        S.op(dve, lambda h: h.reciprocal(out=ev[:], in_=ps[0:64, 5, :]), reads=[PB[5]], writes=[EVB])
                        for hc in range(4):
                            S.op(dve, lambda h, hc=hc, q0=q0: h.tensor_tensor(
                                out=oT_c[:, hc, q0:q0 + 128], in0=ps[0:64, 4, hc * 128:(hc + 1) * 128],
                                in1=ev[:, hc * 128:(hc + 1) * 128], op=ALU.mult), reads=[PB[4], EVB], writes=[OCB])
                S.barrier()

        def emit_post(l, oT_a, oT_b, oT_c, OAB, OBB, OCB):
            with ExitStack() as fs:
                def fsb(name, shape, dt):
                    uid[0] += 1
                    return fs.enter_context(nc.sbuf_tensor(f"{name}_{uid[0]}", list(shape), dt))
                hT = fsb("o_hT", [128, 1, 8, 512], BF16)
                yT = fsb("o_yT", [128, 8, 512], BF16)
                wga = fsb("o_wg", [128, 3, 8, 128], BF16)
                wa = fsb("o_wa", [64, 4, D], BF16)
                wb = fsb("o_wb", [128, 4, D], BF16)
                wc = fsb("o_wc", [64, 4, D], BF16)
                wo = fsb("o_wo", [128, 8, D], BF16)
                sg = fsb("o_sg", [128, 2, 512], F32)
                tmp = fsb("o_tmp", [128, 2, 512], F32)
                yacc = fsb("o_yacc", [128, 512], F32)
                HB = [Buf(), Buf()]
                YB = Buf()
                WGB = [Buf() for _ in range(3)]
                WB = Buf()
                SGB = [Buf(), Buf()]
                TMB = [Buf(), Buf()]
                YAB = Buf()
                winv = w_in[l].rearrange("(c p) n -> p c n", p=128)
                S.dma(pool, wa[:], w_br[0][l].rearrange("(h p) n -> p h n", p=64), writes=[WB])
                S.dma(pool, wb[:], w_br[1][l].rearrange("(h p) n -> p h n", p=128), writes=[WB])
                S.dma(pool, wc[:], w_br[2][l].rearrange("(h p) n -> p h n", p=64), writes=[WB])
                S.dma(pool, wo[:], w_out[l].rearrange("(c p) n -> p c n", p=128), writes=[WB])
                nw = 0
                ns = 0
                for tt in range(NTT):
                    tok = slice(tt * 512, tt * 512 + 512)
                    kh = 0
                    S.dma(sp, hT[:, kh], hT_si.rearrange("(c p) n -> p c n", p=128)[:, :, tok], writes=[HB[kh]])
                    for oc in range(8):
                        for i in range(3):
                            k3 = nw % 3
                            nw += 1
                            g0 = 4808 + i * 1024 + oc * 128
                            S.dma(pool, wga[:, k3], winv[:, :, g0:g0 + 128], writes=[WGB[k3]])
                            bg = (oc * 3 + i) % 2
                            bb = 2 + (oc * 3 + i) % 2
                            for c in range(8):
                                mm(ps[:, bg, :], wga[:, k3, c, :], hT[:, kh, c, :], c == 0, c == 7, [WGB[k3], HB[kh]], [PB[bg]])
                            ocs = slice(oc * 128, oc * 128 + 128)
                            if i == 0:
                                for hh in range(4):
                                    mm(ps[:, bb, :], wa[:, hh, ocs], oT_a[:, hh, tok], hh == 0, hh == 3, [WB, OAB], [PB[bb]])
                            elif i == 1:
                                for hh in range(4):
                                    mm(ps[:, bb, :], wb[:, hh, ocs], oT_b[:, hh, tok], hh == 0, hh == 3, [WB, OBB], [PB[bb]])
                            else:
                                for hh in range(4):
                                    mm(ps[:, bb, :], wc[:, hh, ocs], oT_c[:, hh, tok], hh == 0, hh == 3, [WB, OCB], [PB[bb]])
                            k = ns % 2
                            ns += 1
                            S.op(act, lambda h, k=k, bg=bg: h.activation(out=sg[:, k, :], in_=ps[:, bg, :], func=AF.Sigmoid),
                                 reads=[PB[bg]], writes=[SGB[k]])
                            if i == 0:
                                S.op(dve, lambda h, k=k, bb=bb: h.tensor_tensor(out=yacc[:], in0=sg[:, k, :], in1=ps[:, bb, :],
                                                                               op=ALU.mult), reads=[SGB[k], PB[bb]], writes=[YAB])
                            else:
                                S.op(dve, lambda h, k=k, bb=bb: h.tensor_tensor(out=tmp[:, k, :], in0=sg[:, k, :],
                                                                               in1=ps[:, bb, :], op=ALU.mult),
                                     reads=[SGB[k], PB[bb]], writes=[TMB[k]])
                                if i == 1:
                                    S.op(pool, lambda h, k=k: h.tensor_tensor(out=yacc[:], in0=yacc[:], in1=tmp[:, k, :],
                                                                             op=ALU.add), reads=[YAB, TMB[k]], writes=[YAB])
                                else:
                                    S.op(pool, lambda h, k=k, oc=oc: h.tensor_tensor(out=yT[:, oc, :], in0=yacc[:],
                                                                                    in1=tmp[:, k, :], op=ALU.add),
                                         reads=[YAB, TMB[k]], writes=[YB])
                    for oc in range(8):
                        bo = 4 + oc % 2
                        for c in range(8):
                            mm(ps[:, bo, :], wo[:, c, oc * 128:(oc + 1) * 128], yT[:, c, :], c == 0, c == 7, [WB, YB], [PB[bo]])
                        S.op(dve, lambda h, bo=bo, oc=oc, tok=tok: h.tensor_tensor(out=xT[:, oc, tok], in0=ps[:, bo, :],
                                                                                  in1=xT[:, oc, tok], op=ALU.add),
                             reads=[PB[bo], XB[tt]], writes=[XB[tt]])
                S.barrier()

        S.barrier()
        if has_b:
            emit_vec()
        for (ph, l) in parts:
            if fused:
                KT_l, V_l, KT_f, V_f = KT_l2[l % 2], V_l2[l % 2], KT_f2[l % 2], V_f2[l % 2]
            if ph == "a":
                emit_ffn(l, 0)
                emit_pre(l)
                if fused:
                    S.drain(pool)
                    groups = [[2 * i, 2 * i + 1] for i in range(ncores // 2)]
                    pieces = [(KT_l[g * 384:(g + 1) * 384, :], KT_f[g * 768:(g + 1) * 768, :]) for g in range(4)]
                    pieces += [(V_l[g * 512:(g + 1) * 512, :], V_f[g * 1024:(g + 1) * 1024, :]) for g in range(NM)]
                    for (src_, dst_) in pieces:
                        ins = nc.gpsimd.collective_compute("AllGather", ALU.bypass, replica_groups=groups,
                                                           ins=[src_.opt()], outs=[dst_.opt()])
                        S.cc_val += 1
                        ins.then_inc(S.cc_sem)
                    S.barrier()
            elif ph == "b":
                if not fused and (ph, l) == parts[0]:
                    S.dma(sp, widx[:], widx_i, writes=[WIB])
                with ExitStack() as bs:
                    oT_c = bs.enter_context(nc.sbuf_tensor(f"oT_c{l}", [64, 4, NT], BF16))
                    OAB, OBB, OCB = Buf(), Buf(), Buf()
                    if "c" in MIX:
                        emit_dsa(l, oT_c, OCB)
                    oT_a = bs.enter_context(nc.sbuf_tensor(f"oT_a{l}", [64, 4, NT], BF16))
                    oT_b = bs.enter_context(nc.sbuf_tensor(f"oT_b{l}", [128, 4, NT], BF16))
                    emit_att(l, oT_a, oT_b, OAB, OBB)
                    emit_post(l, oT_a, oT_b, oT_c, OAB, OBB, OCB)
                emit_ffn(l, 1)
        for tt in range(NTT):
            S.dma(sp, xT_out.rearrange("(c p) n -> p c n", p=128)[:, :, tt * 512:(tt + 1) * 512],
                  xT[:, :, tt * 512:(tt + 1) * 512], reads=[XB[tt]])
        S.barrier()
    nc._n_ins = S.n_ins
    return nc


def rel_bucket_np(dist):
    n = np.maximum(dist, 0)
    nf = np.maximum(n, 1).astype(np.float32)
    large = 16 + (np.log(nf / np.float32(16)) / np.float32(math.log(2048 / 16)) * np.float32(16)).astype(np.int32)
    large = np.minimum(large, 31)
    return np.where(n < 16, n, large)


def make_onehot(e_par):
    oh = np.zeros((NKIND * 2, 33, LV), np.float32)
    v = np.arange(LV)
    for kind in range(NKIND):
        for p in range(2):
            dist = v - VOFF - 512 * e_par[p]
            if kind < 3:
                w, d = DIL[kind]
                valid = (dist >= 0) & (dist <= w) & (dist % d == 0)
            else:
                valid = dist >= 0
            bk = rel_bucket_np(dist)
            o = oh[kind * 2 + p]
            o[bk[valid], v[valid]] = 1.0
            o[32, v[~valid]] = 1.0
    return oh


def make_cm(e_par):
    cm = np.zeros((2, 128, 1024), np.float32)
    u = np.arange(1024)[None, :]
    qi = np.arange(128)[:, None]
    for p in range(2):
        cm[p] = np.where(u - 512 + 512 * e_par[p] <= qi, 0.0, -BIG)
    return cm


def local_superblocks(hf, ns):
    return [gs for gs in range(ns) if (gs % 4 in (0, 3)) == (hf == 0)]


_PROG = {}


def _get_prog(T, L, parts, fused, lbase=0, ncores=8):
    key = (T, L, tuple(parts), fused, lbase, ncores)
    if key not in _PROG:
        _PROG[key] = build(T, L, list(parts), fused, lbase, ncores)
    return _PROG[key]


A_KEYS = ("ffn1_w_gate", "ffn1_w_up", "ffn1_w_down", "w_in")
B_KEYS = ("ffn2_w_gate", "ffn2_w_up", "ffn2_w_down", "w_in", "w_branch_a", "w_branch_b", "w_branch_c", "w_out")


def run_model(inputs, T, L, B, fused=True):
    x = np.asarray(inputs["x"], np.float32)
    NT = T // 2
    NS = T // 512
    ncores = 2 * B
    consts = np.zeros((4, 128, 128), np.float32)
    consts[0] = np.eye(128)
    consts[1] = np.eye(128)[::-1]
    consts[2] = 1.0
    consts[3, :64, :64] = 1.0
    consts[3, 64:, 64:] = 1.0
    norms = np.stack([inputs["ffn1_norm"], inputs["mix_norm"], inputs["ffn2_norm"]], 1)
    normsT = np.ascontiguousarray(norms.reshape(L, 3, 8, 128).transpose(3, 0, 1, 2).reshape(128, L * 24)).astype(np.float32)
    qg = np.asarray(inputs["qk_gain"], np.float32).reshape(L * 6, 64)
    qkg = np.ascontiguousarray(np.concatenate([qg, qg], 1).T)
    dnorm = np.ascontiguousarray(np.asarray(inputs["diff_out_norm"], np.float32).T)
    dlam = np.ascontiguousarray(np.broadcast_to(np.asarray(inputs["diff_lambda"], np.float32).reshape(1, L * 256), (128, L * 256)))
    shared = {"consts": consts, "normsT": normsT, "qkg": qkg, "dnorm": dnorm, "dlam": dlam,
              "rel_bias": np.asarray(inputs["rel_bias"], np.float32)}
    for k in ("ffn1_w_gate", "ffn1_w_up", "ffn1_w_down", "ffn2_w_gate", "ffn2_w_up", "ffn2_w_down", "w_in",
              "w_branch_a", "w_branch_b", "w_branch_c", "w_out"):
        shared[k] = np.asarray(inputs[k], np.float32)
    percore = []
    for c in range(ncores):
        b, hf = c // 2, c % 2
        sbs = local_superblocks(hf, NS)
        xs = np.concatenate([x[b, gs * 512:(gs + 1) * 512] for gs in sbs], 0)
        e_par = [1, 0] if hf == 0 else [0, 1]
        percore.append({"xT_in": np.ascontiguousarray(xs.T), "onehot": make_onehot(e_par), "cm": make_cm(e_par)})
    cores = list(range(ncores))
    if fused:
        parts = []
        for l in range(L):
            parts += [("a", l), ("b", l)]
        nc = _get_prog(T, L, parts, True, 0, ncores)
        res = run_bass_kernel_spmd(nc, [dict(shared, **pc) for pc in percore], core_ids=cores)
        outs = [r["xT_out"] for r in res.results]
    else:
        state = [pc["xT_in"] for pc in percore]
        small = {k: shared[k] for k in ("consts",)}
        for l in range(L):
            sm = dict(small)
            sm["normsT"] = np.ascontiguousarray(normsT[:, l * 24:(l + 1) * 24])
            sm["qkg"] = np.ascontiguousarray(qkg[:, l * 6:(l + 1) * 6])
            sm["dnorm"] = np.ascontiguousarray(dnorm[:, l:l + 1])
            sm["dlam"] = np.ascontiguousarray(dlam[:, l * 256:(l + 1) * 256])
            sa = dict(sm)
            for k in A_KEYS:
                sa[k] = shared[k][l:l + 1]
            nca = _get_prog(T, 1, [("a", 0)], False, 0)
            res = run_bass_kernel_spmd(nca, [dict(sa, xT_in=state[i]) for i in range(ncores)], core_ids=cores)
            ra = res.results
            sbm = dict(sm)
            for k in B_KEYS:
                sbm[k] = shared[k][l:l + 1]
            sbm["rel_bias"] = shared["rel_bias"]
            ncb = _get_prog(T, 1, [("b", 0)], False, l)
            maps = []
            for i, pc in enumerate(percore):
                p0 = (i // 2) * 2
                m = dict(sbm, onehot=pc["onehot"], cm=pc["cm"])
                m["xT_in"] = ra[i]["xT_out"]
                m["hT_i"] = ra[i]["hT_o"]
                m["QT_i"] = ra[i]["QT_o"]
                m["widx_i"] = ra[i]["widx_o"]
                k0, k1 = np.asarray(ra[p0]["KT_o"]), np.asarray(ra[p0 + 1]["KT_o"])
                m["KT_f"] = np.concatenate([np.concatenate([k0[g * 384:(g + 1) * 384], k1[g * 384:(g + 1) * 384]], 0)
                                            for g in range(4)], 0)
                v0, v1 = np.asarray(ra[p0]["V_o"]), np.asarray(ra[p0 + 1]["V_o"])
                m["V_f"] = np.concatenate([np.concatenate([v0[g * 512:(g + 1) * 512], v1[g * 512:(g + 1) * 512]], 0)
                                           for g in range(NT // 512)], 0)
                maps.append(m)
            res = run_bass_kernel_spmd(ncb, maps, core_ids=cores)
            state = [r["xT_out"] for r in res.results]
        outs = state
    out = np.zeros((B, T, D), np.float32)
    for c in range(ncores):
        b, hf = c // 2, c % 2
        sbs = local_superblocks(hf, NS)
        xo = np.asarray(outs[c]).T
        for i, gs in enumerate(sbs):
            out[b, gs * 512:(gs + 1) * 512] = xo[i * 512:(i + 1) * 512]
    return out


FUSED = True


def kernel(**inputs):
    x = np.asarray(inputs["x"])
    B, T, _ = x.shape
    L = np.asarray(inputs["w_in"]).shape[0]
    return run_model(inputs, T, L, B, fused=FUSED)
```

```python
import math
from contextlib import ExitStack
import numpy as np
import concourse.bass as bass
import concourse.mybir as mybir
from concourse.bass_utils import run_bass_kernel_spmd

F32 = mybir.dt.float32
BF16 = mybir.dt.bfloat16
AF = mybir.ActivationFunctionType
ALU = mybir.AluOpType
AX = mybir.AxisListType

D = 1024
DFF = 2816
NF = DFF // 128
NIN = 7880
EPS = 1e-6
BIG = 30000.0
LV = 3840
WD = 3712
VOFF = 511
NKIND = 4
DIL = ((128, 1), (512, 4), (2048, 16))
R_DMA = 8
MIX = "abc"


class Buf:
    __slots__ = ("w", "r")

    def __init__(self):
        self.w = None
        self.r = {}


class Eng:
    def __init__(self, name, h, sem, same):
        self.name = name
        self.h = h
        self.sem = sem
        self.cnt = 0
        self.seen = {}
        self.same = same


class Sched:
    def __init__(self, nc, es):
        self.nc = nc
        mk = lambda n: es.enter_context(nc.semaphore(n))
        self.pe = Eng("pe", nc.tensor, mk("s_pe"), False)
        self.act = Eng("act", nc.scalar, mk("s_act"), True)
        self.dve = Eng("dve", nc.vector, mk("s_dve"), True)
        self.pool = Eng("pool", nc.gpsimd, mk("s_pool"), True)
        self.sp = Eng("sp", nc.sync, mk("s_sp"), False)
        self.engs = [self.pe, self.act, self.dve, self.pool, self.sp]
        self.dq = {}
        for e in (self.sp, self.pool):
            self.dq[e.name] = {"sems": [mk(f"d_{e.name}{i}") for i in range(R_DMA)], "vals": [0] * R_DMA, "i": 0}
        self.n_ins = 0
        self.cc_sem = mk("cc_sem")
        self.cc_val = 0

    def _deps(self, reads, writes):
        d = {}

        def add(t):
            if t is None:
                return
            k, s, v = t
            if k not in d or d[k][1] < v:
                d[k] = (s, v)

        for b in reads:
            add(b.w)
        for b in writes:
            add(b.w)
            for t in b.r.values():
                add(t)
        return d

    def _filter(self, eng, deps):
        out = []
        for key, (sem, val) in deps.items():
            if key == eng.name and not eng.same:
                continue
            if eng.seen.get(key, 0) >= val:
                continue
            eng.seen[key] = val
            out.append((key, sem, val))
        out.sort(key=lambda t: 0 if t[0] == eng.name else 1)
        return out

    def _emit(self, eng, fn, waits):
        for (_, sem, val) in waits[1:]:
            eng.h.wait_ge(sem, val)
        ins = fn(eng.h)
        if waits:
            ins._wait_ge(waits[0][1], waits[0][2])
        self.n_ins += 1 + max(0, len(waits) - 1)
        return ins

    def op(self, eng, fn, reads=(), writes=()):
        waits = self._filter(eng, self._deps(reads, writes))
        ins = self._emit(eng, fn, waits)
        ins.then_inc(eng.sem, 1)
        eng.cnt += 1
        tok = (eng.name, eng.sem, eng.cnt)
        for b in reads:
            b.r[eng.name] = tok
        for b in writes:
            b.w = tok
            b.r = {}
        return tok

    def dma(self, eng, out, in_, reads=(), writes=(), **kw):
        q = self.dq[eng.name]
        i = q["i"]
        q["i"] = (i + 1) % R_DMA
        sem = q["sems"][i]
        key = f"d_{eng.name}{i}"
        deps = self._deps(reads, writes)
        if q["vals"][i] > 0:
            deps[key] = (sem, q["vals"][i])
        waits = self._filter(eng, deps)
        ins = self._emit(eng, lambda h: h.dma_start(out=out, in_=in_, **kw), waits)
        q["vals"][i] += 16
        ins.then_inc(sem, 16)
        tok = (key, sem, q["vals"][i])
        for b in reads:
            b.r[key] = tok
        for b in writes:
            b.w = tok
            b.r = {}
        return tok

    def drain(self, eng):
        for e in self.engs:
            if e.cnt > 0 and eng.seen.get(e.name, 0) < e.cnt and e is not eng:
                eng.h.wait_ge(e.sem, e.cnt)
                eng.seen[e.name] = e.cnt
        if self.cc_val > 0 and eng.seen.get("cc", 0) < self.cc_val:
            eng.h.wait_ge(self.cc_sem, self.cc_val)
            eng.seen["cc"] = self.cc_val
        for qn, q in self.dq.items():
            for i in range(R_DMA):
                key = f"d_{qn}{i}"
                if q["vals"][i] > 0 and eng.seen.get(key, 0) < q["vals"][i]:
                    eng.h.wait_ge(q["sems"][i], q["vals"][i])
                    eng.seen[key] = q["vals"][i]

    def barrier(self):
        for e in self.engs:
            self.drain(e)


def gpos(gs):
    r = 0 if gs % 4 in (0, 3) else 1
    li = gs // 2
    return r, li


def build(T, L, parts, fused, lbase=0, ncores=8):
    NT = T // 2
    NS = T // 512
    NM = NS // 2
    NTT = NT // 512
    nc = bass.Bass("TRN2", target_bir_lowering=False)

    def din(name, shape, dt=F32):
        return nc.dram_tensor(name, list(shape), dt, kind="ExternalInput").ap()

    def dout(name, shape, dt=F32):
        return nc.dram_tensor(name, list(shape), dt, kind="ExternalOutput").ap()

    def dscr(name, shape, dt, io):
        if fused:
            return nc.dram_tensor(name, list(shape), dt, kind="Internal").ap()
        return nc.dram_tensor(name, list(shape), dt, kind="ExternalInput" if io == "in" else "ExternalOutput").ap()

    has_a = any(p[0] == "a" for p in parts)
    has_b = any(p[0] == "b" for p in parts)
    first_is_b = parts[0][0] == "b"
    last_is_a = parts[-1][0] == "a"

    xT_in = din("xT_in", [D, NT])
    xT_out = dout("xT_out", [D, NT])
    consts = din("consts", [4, 128, 128])
    normsT = din("normsT", [128, L * 24])
    qkg = din("qkg", [128, L * 6])
    dnorm = din("dnorm", [128, L])
    dlam = din("dlam", [128, L * 256])
    w_gate = [din("ffn1_w_gate", [L, D, DFF]) if has_a else None, din("ffn2_w_gate", [L, D, DFF]) if has_b else None]
    w_up = [din("ffn1_w_up", [L, D, DFF]) if has_a else None, din("ffn2_w_up", [L, D, DFF]) if has_b else None]
    w_down = [din("ffn1_w_down", [L, DFF, D]) if has_a else None, din("ffn2_w_down", [L, DFF, D]) if has_b else None]
    w_in = din("w_in", [L, D, NIN])
    if has_b:
        w_br = [din("w_branch_a", [L, 256, D]), din("w_branch_b", [L, 512, D]), din("w_branch_c", [L, 256, D])]
        w_out = din("w_out", [L, D, D])
        rel_bias = din("rel_bias", [32, 20])
        onehot = din("onehot", [NKIND * 2, 33, LV])
        cm_in = din("cm", [2, 128, 1024])

    NQC = 16
    NKC = 12
    VW = 1344
    if fused:
        hT_s = dscr("hT_s", [8 * 128, NT], BF16, None)
        QT_s = dscr("QT_s", [NQC * 128, NT], BF16, None)
        KT_l2 = [dscr(f"KT_l{i}", [NKC * 128, NT], BF16, None) for i in range(2)]
        V_l2 = [dscr(f"V_l{i}", [NT, VW], BF16, None) for i in range(2)]
        KT_f2 = [dscr(f"KT_f{i}", [2 * NKC * 128, NT], BF16, None) for i in range(2)]
        V_f2 = [dscr(f"V_f{i}", [2 * NT, VW], BF16, None) for i in range(2)]
        KT_l, V_l, KT_f, V_f = KT_l2[0], V_l2[0], KT_f2[0], V_f2[0]
        hT_si = hT_s
        QT_si = QT_s
    else:
        if last_is_a:
            hT_s = dscr("hT_o", [8 * 128, NT], BF16, "out")
            QT_s = dscr("QT_o", [NQC * 128, NT], BF16, "out")
            KT_l = dscr("KT_o", [NKC * 128, NT], BF16, "out")
            V_l = dscr("V_o", [NT, VW], BF16, "out")
            widx_o = dscr("widx_o", [128, NT // 128 * 8], F32, "out")
        if first_is_b:
            hT_si = dscr("hT_i", [8 * 128, NT], BF16, "in")
            QT_si = dscr("QT_i", [NQC * 128, NT], BF16, "in")
            KT_f = dscr("KT_f", [2 * NKC * 128, NT], BF16, "in")
            V_f = dscr("V_f", [2 * NT, VW], BF16, "in")
            widx_i = dscr("widx_i", [128, NT // 128 * 8], F32, "in")
    vec_s = nc.dram_tensor("vec_s", [NKIND * 2 * 20, LV], BF16, kind="Internal").ap()

    es = ExitStack()
    uid = [0]
    with es:
        S = Sched(nc, es)
        pe, act, dve, pool, sp = S.pe, S.act, S.dve, S.pool, S.sp

        def sb(name, shape, dt):
            return es.enter_context(nc.sbuf_tensor(name, list(shape), dt))

        xT = sb("xT", [128, 8, NT], F32)
        XB = [Buf() for _ in range(NTT)]
        ps = es.enter_context(nc.psum_tensor("ps", [128, 8, 512], F32))
        PB = [Buf() for _ in range(8)]
        cst = sb("cst", [128, 4, 128], BF16)
        CB = Buf()
        ident, antiI, ones, bones = cst[:, 0, :], cst[:, 1, :], cst[:, 2, :], cst[:, 3, :]
        g32 = sb("g32", [128, L * 24], F32)
        qkgs = sb("qkgs", [128, L * 6], F32)
        dnrm = sb("dnrm", [128, L], F32)
        lamt = sb("lamt", [128, L * 256], F32)
        neglam = sb("neglam", [128, L], F32)
        widx = sb("widx", [128, NT // 128 * 8], F32)
        WIB = Buf()
        GB = Buf()

        S.dma(pool, cst[:], consts.rearrange("k p n -> p k n"), writes=[CB])
        epsb = sb("epsb", [128, 3], F32)
        ci = {1024.0 * EPS: 0, 64.0 * EPS: 1, 128.0 * EPS: 2}
        for cval, cidx in ci.items():
            S.op(dve, lambda h, cval=cval, cidx=cidx: h.memset(epsb[:, cidx:cidx + 1], cval), writes=[CB])
        for tt in range(NTT):
            S.dma(sp, xT[:, :, tt * 512:(tt + 1) * 512],
                  xT_in.rearrange("(c p) n -> p c n", p=128)[:, :, tt * 512:(tt + 1) * 512], writes=[XB[tt]])
        S.dma(sp, g32[:], normsT, writes=[GB])
        S.dma(sp, qkgs[:], qkg, writes=[GB])
        S.dma(sp, dnrm[:], dnorm, writes=[GB])
        S.dma(sp, lamt[:], dlam, writes=[GB])
        S.op(dve, lambda h: h.tensor_scalar(out=g32[:], in0=g32[:], scalar1=32.0, scalar2=None, op0=ALU.mult),
             reads=[GB], writes=[GB])
        for l in range(L):
            for i in range(3):
                c = l * 6 + i * 2 + 1
                S.op(dve, lambda h, c=c: h.tensor_scalar(out=qkgs[:, c:c + 1], in0=qkgs[:, c:c + 1], scalar1=8.0,
                                                         scalar2=None, op0=ALU.mult), reads=[GB], writes=[GB])
        if has_b:
            ltmp = sb("ltmp", [128, 64], F32)
            lsum = sb("lsum", [128, 4], F32)
            LB = Buf()
            for l in range(L):
                lam_init = 0.8 - 0.6 * math.exp(-0.3 * (l + lbase))
                for k in range(2):
                    a0 = l * 256 + k * 128
                    S.op(dve, lambda h, a0=a0: h.tensor_tensor(out=ltmp[:], in0=lamt[:, a0:a0 + 64],
                                                               in1=lamt[:, a0 + 64:a0 + 128], op=ALU.mult),
                         reads=[GB], writes=[LB])
                    S.op(dve, lambda h, k=k: h.tensor_reduce(out=lsum[:, k:k + 1], in_=ltmp[:], axis=AX.X, op=ALU.add),
                         reads=[LB], writes=[LB])
                    S.op(act, lambda h, k=k: h.activation(out=lsum[:, 2 + k:3 + k], in_=lsum[:, k:k + 1], func=AF.Exp),
                         reads=[LB], writes=[LB])
                S.op(dve, lambda h, l=l, li=lam_init: h.scalar_tensor_tensor(
                    out=neglam[:, l:l + 1], in0=lsum[:, 3:4], scalar=-li, in1=lsum[:, 2:3], op0=ALU.add,
                    op1=ALU.subtract), reads=[LB], writes=[GB])
                S.op(dve, lambda h, l=l, li=lam_init: h.tensor_scalar(
                    out=dnrm[:, l:l + 1], in0=dnrm[:, l:l + 1], scalar1=(1.0 - li) * math.sqrt(128.0), scalar2=None,
                    op0=ALU.mult), reads=[GB], writes=[GB])

        bank_rr = {"i": 0}

        def mm(out, lhsT, rhs, start, stop, reads, writes):
            S.op(pe, lambda h: h.matmul(out, lhsT, rhs, start=start, stop=stop), reads=reads, writes=writes)

        def rsq(out, in_, c, reads, wbuf):
            S.op(act, lambda h: h.activation(out=out, in_=in_, func=AF.Sqrt, bias=epsb[0:out.shape[0], ci[c]:ci[c] + 1], scale=1.0),
                 reads=list(reads) + [CB], writes=[wbuf])
            S.op(dve, lambda h: h.reciprocal(out=out, in_=out), reads=[wbuf], writes=[wbuf])

        def emit_rms(gcol0, tgs, hT, HB, tok0, sq, SQB, rstd, RB, banks):
            for n, tg in enumerate(tgs):
                b = banks[n % len(banks)]
                tok = slice(tg * 512, tg * 512 + 512)
                lt = slice((tg - tok0) * 512, (tg - tok0) * 512 + 512)
                for c in range(8):
                    k = c % 2
                    S.op(act, lambda h, c=c, k=k: h.activation(out=sq[:, k, :], in_=xT[:, c, tok], func=AF.Square),
                         reads=[XB[tg]], writes=[SQB[k]])
                    mm(ps[:, b, :], ones, sq[:, k, :], c == 0, c == 7, [SQB[k], CB], [PB[b]])
                rsq(rstd[:], ps[:, b, :], 1024.0 * EPS, [PB[b]], RB)
                for c in range(8):
                    S.op(dve, lambda h, c=c: h.scalar_tensor_tensor(
                        out=hT[:, c, lt], in0=xT[:, c, tok], scalar=g32[:, gcol0 + c:gcol0 + c + 1], in1=rstd[:],
                        op0=ALU.mult, op1=ALU.mult), reads=[XB[tg], RB, GB], writes=[HB[tg - tok0]])

        def emit_ffn(l, which):
            with ExitStack() as fs:
                def fsb(name, shape, dt):
                    uid[0] += 1
                    return fs.enter_context(nc.sbuf_tensor(f"{name}_{uid[0]}", list(shape), dt))
                NH = 1024 if NT >= 1024 else NT
                ntt = NH // 512
                hT = fsb("f_hT", [128, 8, NH], BF16)
                aT = fsb("f_aT", [128, NF, NH], BF16)
                sq = fsb("f_sq", [128, 2, 512], BF16)
                rstd = fsb("f_rstd", [128, 512], F32)
                sg = fsb("f_sg", [128, 2, 512], F32)
                wg = fsb("f_wg", [128, 3, 8, 128], BF16)
                wu = fsb("f_wu", [128, 3, 8, 128], BF16)
                wd = fsb("f_wd", [128, 2, NF, 128], BF16)
                HB = [Buf() for _ in range(ntt)]
                AB = [[Buf() for _ in range(ntt)] for _ in range(NF)]
                SQB = [Buf(), Buf()]
                RB = Buf()
                SGB = [Buf(), Buf()]
                WGB = [Buf() for _ in range(3)]
                WUB = [Buf() for _ in range(3)]
                WDB = [Buf(), Buf()]
                wgv = w_gate[which][l].rearrange("(c p) n -> p c n", p=128)
                wuv = w_up[which][l].rearrange("(c p) n -> p c n", p=128)
                wdv = w_down[which][l].rearrange("(j p) n -> p j n", p=128)
                gcol0 = l * 24 + (0 if which == 0 else 16)
                nsg = 0
                for th in range(NT // NH):
                    tgs = [th * ntt + i for i in range(ntt)]
                    emit_rms(gcol0, tgs, hT, HB, th * ntt, sq, SQB, rstd, RB, [6, 7])
                    for j in range(NF):
                        k3 = j % 3
                        S.dma(pool, wg[:, k3], wgv[:, :, j * 128:(j + 1) * 128], writes=[WGB[k3]])
                        S.dma(pool, wu[:, k3], wuv[:, :, j * 128:(j + 1) * 128], writes=[WUB[k3]])
                        for tt in range(ntt):
                            bg = (j * ntt + tt) % 2
                            bu = 2 + (j * ntt + tt) % 2
                            tl = slice(tt * 512, tt * 512 + 512)
                            for c in range(8):
                                mm(ps[:, bg, :], wg[:, k3, c, :], hT[:, c, tl], c == 0, c == 7, [WGB[k3], HB[tt]], [PB[bg]])
                            for c in range(8):
                                mm(ps[:, bu, :], wu[:, k3, c, :], hT[:, c, tl], c == 0, c == 7, [WUB[k3], HB[tt]], [PB[bu]])
                            k = nsg % 2
                            nsg += 1
                            S.op(act, lambda h, k=k, bg=bg: h.activation(out=sg[:, k, :], in_=ps[:, bg, :], func=AF.Silu),
                                 reads=[PB[bg]], writes=[SGB[k]])
                            S.op(dve, lambda h, k=k, bu=bu, j=j, tl=tl: h.tensor_tensor(
                                out=aT[:, j, tl], in0=sg[:, k, :], in1=ps[:, bu, :], op=ALU.mult),
                                reads=[SGB[k], PB[bu]], writes=[AB[j][tt]])
                    for oc in range(8):
                        k2 = oc % 2
                        S.dma(pool, wd[:, k2], wdv[:, :, oc * 128:(oc + 1) * 128], writes=[WDB[k2]])
                        for tt in range(ntt):
                            bo = 4 + (oc * ntt + tt) % 2
                            tg = th * ntt + tt
                            tl = slice(tt * 512, tt * 512 + 512)
                            tok = slice(tg * 512, tg * 512 + 512)
                            for j in range(NF):
                                mm(ps[:, bo, :], wd[:, k2, j, :], aT[:, j, tl], j == 0, j == NF - 1,
                                   [WDB[k2], AB[j][tt]], [PB[bo]])
                            S.op(dve, lambda h, bo=bo, oc=oc, tok=tok: h.scalar_tensor_tensor(
                                out=xT[:, oc, tok], in0=ps[:, bo, :], scalar=0.5, in1=xT[:, oc, tok], op0=ALU.mult,
                                op1=ALU.add), reads=[PB[bo], XB[tg]], writes=[XB[tg]])
                S.barrier()

        def emit_pre(l):
            with ExitStack() as fs:
                def fsb(name, shape, dt):
                    uid[0] += 1
                    return fs.enter_context(nc.sbuf_tensor(f"{name}_{uid[0]}", list(shape), dt))
                hT = fsb("p_hT", [128, 8, NT], BF16)
                sq = fsb("p_sq", [128, 2, 512], BF16)
                rstd = fsb("p_rstd", [128, 512], F32)
                wt = fsb("p_wt", [128, 3, 8, 128], BF16)
                st = fsb("p_st", [128, 2, NT], BF16)
                wv = fsb("p_wv", [128, 8, VW + 8], BF16)
                vst = fsb("p_vst", [128, 2, VW], BF16)
                HB = [Buf() for _ in range(NTT)]
                SQB = [Buf(), Buf()]
                RB = Buf()
                WTB = [Buf() for _ in range(3)]
                STB = [Buf(), Buf()]
                WVB = Buf()
                VSB = [Buf(), Buf()]
                winv = w_in[l].rearrange("(c p) n -> p c n", p=128)
                emit_rms(l * 24 + 8, list(range(NTT)), hT, HB, 0, sq, SQB, rstd, RB, [6, 7])
                for tt in range(NTT):
                    S.dma(sp, hT_s.rearrange("(c p) n -> p c n", p=128)[:, :, tt * 512:(tt + 1) * 512],
                          hT[:, :, tt * 512:(tt + 1) * 512], reads=[HB[tt]])
                S.dma(pool, wv[:, :, 0:768], winv[:, :, 1536:2304], writes=[WVB])
                S.dma(pool, wv[:, :, 768:1280], winv[:, :, 3328:3840], writes=[WVB])
                S.dma(pool, wv[:, :, 1280:1344], winv[:, :, 4160:4224], writes=[WVB])
                S.dma(pool, wv[:, :, 1344:1352], winv[:, :, 4800:4808], writes=[WVB])
                qc = l * 6
                chunks = []
                for i in range(6):
                    chunks.append(("n", i * 128, None, qc + 0, QT_s, i))
                for i in range(6):
                    chunks.append(("n", 768 + i * 128, None, qc + 1, KT_l, i))
                for i in range(4):
                    chunks.append(("n", 2304 + i * 128, None, qc + 2, QT_s, 6 + i))
                for i in range(4):
                    chunks.append(("n", 2816 + i * 128, None, qc + 3, KT_l, 6 + i))
                for i in range(2):
                    chunks.append(("n", 3840 + i * 128, None, qc + 4, QT_s, 10 + i))
                chunks.append(("ck", 4096, 4736, qc + 5, KT_l, 10))
                for i in range(4):
                    chunks.append(("p", 4224 + i * 128, None, None, QT_s, 12 + i))
                for ci, (kind, c0, c1, gcol, dst, dchunk) in enumerate(chunks):
                    k3 = ci % 3
                    if kind == "ck":
                        S.dma(pool, wt[:, k3, :, 0:64], winv[:, :, c0:c0 + 64], writes=[WTB[k3]])
                        S.dma(pool, wt[:, k3, :, 64:128], winv[:, :, c1:c1 + 64], writes=[WTB[k3]])
                    else:
                        S.dma(pool, wt[:, k3], winv[:, :, c0:c0 + 128], writes=[WTB[k3]])
                    ks = ci % 2
                    for tt in range(NTT):
                        b = (ci * NTT + tt) % 2
                        tl = slice(tt * 512, tt * 512 + 512)
                        for c in range(8):
                            mm(ps[:, b, :], wt[:, k3, c, :], hT[:, c, tl], c == 0, c == 7, [WTB[k3], HB[tt]], [PB[b]])
                        if kind == "p":
                            S.op(act, lambda h, b=b, ks=ks, tl=tl: h.activation(out=st[:, ks, tl], in_=ps[:, b, :],
                                                                                func=AF.Copy, scale=0.125),
                                 reads=[PB[b]], writes=[STB[ks]])
                            continue
                        np_ = 64 if kind == "ck" else 128
                        kq = (ci * NTT + tt) % 2
                        b2 = 2 + (ci * NTT + tt) % 2
                        S.op(act, lambda h, b=b, kq=kq, np_=np_: h.activation(out=sq[0:np_, kq, :], in_=ps[0:np_, b, :],
                                                                               func=AF.Square),
                             reads=[PB[b]], writes=[SQB[kq]])
                        mm(ps[0:np_, b2, :], bones[0:np_, 0:np_], sq[0:np_, kq, :], True, True, [SQB[kq], CB], [PB[b2]])
                        rsq(rstd[0:np_, :], ps[0:np_, b2, :], 64.0 * EPS, [PB[b2]], RB)
                        S.op(dve, lambda h, b=b, ks=ks, tl=tl, np_=np_, gcol=gcol: h.scalar_tensor_tensor(
                            out=st[0:np_, ks, tl], in0=ps[0:np_, b, :], scalar=qkgs[0:np_, gcol:gcol + 1],
                            in1=rstd[0:np_, :], op0=ALU.mult, op1=ALU.mult), reads=[PB[b], RB, GB], writes=[STB[ks]])
                        if kind == "ck":
                            S.op(act, lambda h, b=b, ks=ks, tl=tl: h.activation(
                                out=st[64:128, ks, tl], in_=ps[64:128, b, :], func=AF.Copy), reads=[PB[b]],
                                writes=[STB[ks]])
                    S.dma(sp, dst[dchunk * 128:(dchunk + 1) * 128, :], st[:, ks, :], reads=[STB[ks]])
                for t128 in range(NT // 128):
                    tl = slice(t128 * 128, t128 * 128 + 128)
                    kv = t128 % 2
                    groups = [(0, 512), (512, 768), (768, 1280), (1280, 1352)]
                    for gi, (a0, a1) in enumerate(groups):
                        b = 4 + (t128 * 4 + gi) % 4
                        n = a1 - a0
                        for c in range(8):
                            mm(ps[:, b, 0:n], hT[:, c, tl], wv[:, c, a0:a1], c == 0, c == 7, [HB[t128 // 4], WVB], [PB[b]])
                        if gi < 3:
                            eng = act if gi % 2 == 0 else dve
                            if eng is act:
                                S.op(act, lambda h, b=b, n=n, a0=a0, a1=a1, kv=kv: h.activation(
                                    out=vst[:, kv, a0:a1], in_=ps[:, b, 0:n], func=AF.Copy), reads=[PB[b]], writes=[VSB[kv]])
                            else:
                                S.op(dve, lambda h, b=b, n=n, a0=a0, a1=a1, kv=kv: h.tensor_copy(
                                    out=vst[:, kv, a0:a1], in_=ps[:, b, 0:n]), reads=[PB[b]], writes=[VSB[kv]])
                        else:
                            S.op(act, lambda h, b=b, kv=kv: h.activation(
                                out=vst[:, kv, 1280:1344], in_=ps[:, b, 0:64], func=AF.Copy), reads=[PB[b]], writes=[VSB[kv]])
                            S.op(dve, lambda h, b=b, t128=t128: h.tensor_scalar(
                                out=widx[:, t128 * 8:t128 * 8 + 8], in0=ps[:, b, 64:72], scalar1=8.0 ** -0.5,
                                scalar2=None, op0=ALU.mult), reads=[PB[b]], writes=[WIB])
                    S.dma(sp, V_l[tl, :], vst[:, kv, :], reads=[VSB[kv]])
                if not fused:
                    S.dma(sp, widx_o, widx[:], reads=[WIB])
                S.barrier()

        def emit_vec():
            with ExitStack() as fs:
                def fsb(name, shape, dt):
                    uid[0] += 1
                    return fs.enter_context(nc.sbuf_tensor(f"{name}_{uid[0]}", list(shape), dt))
                tbl = fsb("v_tbl", [33, 20], F32)
                oh = fsb("v_oh", [33, 2, 512], F32)
                vs = fsb("v_vs", [20, 2, 512], BF16)
                TB = Buf()
                OB = [Buf(), Buf()]
                VB = [Buf(), Buf()]
                S.op(dve, lambda h: h.memset(tbl[:], -BIG), writes=[TB])
                S.dma(sp, tbl[0:32, :], rel_bias, writes=[TB])
                n = 0
                for kp in range(NKIND * 2):
                    for c0 in range(0, LV, 512):
                        cw = min(512, LV - c0)
                        k = n % 2
                        n += 1
                        S.dma(sp, oh[:, k, 0:cw], onehot[kp, :, c0:c0 + cw], writes=[OB[k]])
                        b = k
                        mm(ps[0:20, b, 0:cw], tbl[:], oh[:, k, 0:cw], True, True, [TB, OB[k]], [PB[b]])
                        S.op(act, lambda h, b=b, k=k, cw=cw: h.activation(out=vs[:, k, 0:cw], in_=ps[0:20, b, 0:cw],
                                                                          func=AF.Copy), reads=[PB[b]], writes=[VB[k]])
                        S.dma(sp, vec_s.rearrange("(k h) n -> k h n", h=20)[kp, :, c0:c0 + cw], vs[:, k, 0:cw],
                              reads=[VB[k]])
                S.barrier()

        def strip_src(kind, par, head, width):
            row = (kind * 2 + par) * 20 + head
            return bass.AP(tensor=vec_s.tensor, offset=row * LV, ap=[[1, 128], [1, width]])

        def kcol(kt):
            gs = kt // 4
            r, li = gpos(gs)
            return r * NT + li * 512 + (kt % 4) * 128

        def vtile(kt):
            gs = kt // 4
            r, li = gpos(gs)
            return li * 8 + r * 4 + kt % 4

        def emit_att(l, oT_a, oT_b, OAB, OBB):
            with ExitStack() as fs:
                def fsb(name, shape, dt):
                    uid[0] += 1
                    return fs.enter_context(nc.sbuf_tensor(f"{name}_{uid[0]}", list(shape), dt))
                KT = fsb("a_KT", [64, 2, 2 * NT], BF16)
                QTt = fsb("a_QT", [64, 2, NT], BF16)
                Vt = fsb("a_V", [128, 2 * NT // 128, 128], BF16)
                G = fsb("a_G", [128, 2, WD], BF16)
                Pt = fsb("a_P", [128, 3, 512], BF16)
                ev = fsb("a_ev", [128, 4, 512], F32)
                sqb = fsb("a_sq", [128, 512], BF16)
                KB = [Buf(), Buf()]
                QB = [Buf(), Buf()]
                VB = Buf()
                GBf = Buf()
                PtB = [Buf() for _ in range(3)]
                EB = [Buf() for _ in range(4)]
                SQ = Buf()
                KTf = KT_f.rearrange("(g r c p) n -> g c p r n", g=4, r=2, c=3, p=128)
                QTi = QT_si.rearrange("(c p) n -> c p n", p=128)
                Vf = V_f.rearrange("(t p) c -> p t c", p=128)
                npt = [0]

                def attend(m, terms, nmap, dv):
                    first = {0: True, 1: True}
                    total = {0: 0, 1: 0}
                    for (mp, ksl, qsl, kts, c0f) in terms:
                        total[mp] += len(kts)
                    cnt = {0: 0, 1: 0}
                    for (mp, ksl, qsl, kts, c0f) in terms:
                        for kt in kts:
                            k = npt[0] % 3
                            npt[0] += 1
                            b = k
                            kc = kcol(kt)
                            mm(ps[:, b, :], KT[:, ksl, kc:kc + 128], QTt[:, qsl, m * 512:(m + 1) * 512], True, False,
                               [KB[ksl], QB[qsl]], [PB[b]])
                            c0 = c0f(kt)
                            mm(ps[:, b, :], antiI, G[:, m % 2, c0:c0 + 512], False, True, [GBf, CB], [PB[b]])
                            S.op(act, lambda h, b=b, k=k: h.activation(out=Pt[:, k, :], in_=ps[:, b, :], func=AF.Exp),
                                 reads=[PB[b]], writes=[PtB[k]])
                            cnt[mp] += 1
                            st_, sp_ = cnt[mp] == 1, cnt[mp] == total[mp]
                            mm(ps[0:dv, 3 + mp, :], Vt[:, vtile(kt), 0:dv], Pt[:, k, :], st_, sp_, [VB, PtB[k]], [PB[3 + mp]])
                            mm(ps[0:dv, 5 + mp, :], ones[:, 0:dv], Pt[:, k, :], st_, sp_, [CB, PtB[k]], [PB[5 + mp]])

                for s in range(4):
                    for m in range(NM):
                        gsN = 2 * m + 1
                        terms = []
                        first = True
                        ngrp = 0
                        kt_lists = []
                        for g, (w, d) in enumerate(DIL):
                            kts = [kt for kt in range((gsN + 1) * 4)
                                   if -384 <= gsN * 512 - 128 * kt <= w + 639]
                            kt_lists.append(kts)
                        tot = sum(len(k) for k in kt_lists)
                        cnt = 0
                        for g, (w, d) in enumerate(DIL):
                            head = 4 * g + s
                            ch, hf_ = head // 2, head % 2
                            sl = (s * 3 * NM + m * 3 + g) % 2
                            S.dma(sp, KT[:, sl, :].rearrange("p (r n) -> p r n", r=2), KTf[ch // 3, ch % 3, hf_ * 64:hf_ * 64 + 64],
                                  writes=[KB[sl]])
                            S.dma(sp, QTt[:, sl, :], QTi[ch, hf_ * 64:hf_ * 64 + 64, :], writes=[QB[sl]])
                            if m == 0 or True:
                                S.dma(sp, Vt[:, :, 0:64], Vf[:, :, head * 64:head * 64 + 64], writes=[VB])
                            wdt = min(WD, w + 639 + 384 + 512 + 128)
                            S.dma(sp, G[:, m % 2, 0:wdt], strip_src(g, m % 2, head, wdt), writes=[GBf])
                            pend = None
                            for kt in kt_lists[g]:
                                k = npt[0] % 3
                                npt[0] += 1
                                b = k
                                kc = kcol(kt)
                                mm(ps[:, b, :], KT[:, sl, kc:kc + 128], QTt[:, sl, m * 512:(m + 1) * 512], True, False,
                                   [KB[sl], QB[sl]], [PB[b]])
                                c0 = gsN * 512 - 128 * kt + 384
                                mm(ps[:, b, :], antiI, G[:, m % 2, c0:c0 + 512], False, True, [GBf, CB], [PB[b]])
                                S.op(act, lambda h, b=b, k=k: h.activation(out=Pt[:, k, :], in_=ps[:, b, :], func=AF.Exp),
                                     reads=[PB[b]], writes=[PtB[k]])
                                if pend is not None:
                                    pend()
                                cnt += 1

                                def pend(kt=kt, k=k, st_=(cnt == 1), sp_=(cnt == tot)):
                                    mm(ps[0:64, 3, :], Vt[:, vtile(kt), 0:64], Pt[:, k, :], st_, sp_, [VB, PtB[k]], [PB[3]])
                                    mm(ps[0:64, 5, :], ones[:, 0:64], Pt[:, k, :], st_, sp_, [CB, PtB[k]], [PB[5]])
                            if pend is not None:
                                pend()
                        S.op(dve, lambda h: h.reciprocal(out=ev[0:64, 0, :], in_=ps[0:64, 5, :]), reads=[PB[5]], writes=[EB[0]])
                        S.op(dve, lambda h, s=s, m=m: h.tensor_tensor(out=oT_a[:, s, m * 512:(m + 1) * 512],
                                                                       in0=ps[0:64, 3, :], in1=ev[0:64, 0, :], op=ALU.mult),
                             reads=[PB[3], EB[0]], writes=[OAB])
                for hb in range(4):
                    ch, hf_ = hb // 2, hb % 2
                    for mp in range(2):
                        S.dma(sp, KT[:, mp, :].rearrange("p (r n) -> p r n", r=2),
                              KTf[(6 + 2 * mp + ch) // 3, (6 + 2 * mp + ch) % 3, hf_ * 64:hf_ * 64 + 64], writes=[KB[mp]])
                        S.dma(sp, QTt[:, mp, :], QTi[6 + 2 * mp + ch, hf_ * 64:hf_ * 64 + 64, :], writes=[QB[mp]])
                    S.dma(sp, Vt[:], Vf[:, :, 768 + hb * 128:768 + hb * 128 + 128], writes=[VB])
                    for par in range(2):
                        S.dma(sp, G[:, par, :], strip_src(3, par, 12 + hb, WD), writes=[GBf])
                    for m in range(NM):
                        gsN = 2 * m + 1
                        nkt = (gsN + 1) * 4
                        pend = None
                        for kt in range(nkt):
                            D0 = gsN * 512 - 128 * kt
                            c0 = D0 + 384 if D0 < 2176 else 2560
                            kc = kcol(kt)
                            for mp in range(2):
                                k = npt[0] % 3
                                npt[0] += 1
                                b = k
                                mm(ps[:, b, :], KT[:, mp, kc:kc + 128], QTt[:, mp, m * 512:(m + 1) * 512], True, False,
                                   [KB[mp], QB[mp]], [PB[b]])
                                mm(ps[:, b, :], antiI, G[:, m % 2, c0:c0 + 512], False, True, [GBf, CB], [PB[b]])
                                S.op(act, lambda h, b=b, k=k: h.activation(out=Pt[:, k, :], in_=ps[:, b, :], func=AF.Exp),
                                     reads=[PB[b]], writes=[PtB[k]])
                                if pend is not None:
                                    pend()

                                def pend(kt=kt, k=k, mp=mp, nkt=nkt):
                                    mm(ps[:, 3 + mp, :], Vt[:, vtile(kt), :], Pt[:, k, :], kt == 0, kt == nkt - 1,
                                       [VB, PtB[k]], [PB[3 + mp]])
                                    mm(ps[:, 5 + mp, :], ones, Pt[:, k, :], kt == 0, kt == nkt - 1, [CB, PtB[k]], [PB[5 + mp]])
                        if pend is not None:
                            pend()
                        for mp in range(2):
                            S.op(dve, lambda h, mp=mp: h.reciprocal(out=ev[:, mp, :], in_=ps[:, 5 + mp, :]),
                                 reads=[PB[5 + mp]], writes=[EB[mp]])
                            S.op(dve, lambda h, mp=mp: h.tensor_tensor(out=ev[:, mp, :], in0=ps[:, 3 + mp, :],
                                                                       in1=ev[:, mp, :], op=ALU.mult),
                                 reads=[PB[3 + mp], EB[mp]], writes=[EB[mp]])
                        S.op(dve, lambda h: h.scalar_tensor_tensor(out=ev[:, 2, :], in0=ev[:, 1, :],
                                                                   scalar=neglam[:, l:l + 1], in1=ev[:, 0, :],
                                                                   op0=ALU.mult, op1=ALU.add),
                             reads=[EB[0], EB[1], GB], writes=[EB[2]])
                        S.op(act, lambda h: h.activation(out=sqb[:], in_=ev[:, 2, :], func=AF.Square), reads=[EB[2]],
                             writes=[SQ])
                        mm(ps[:, 7, :], ones, sqb[:], True, True, [SQ, CB], [PB[7]])
                        rsq(ev[:, 3, :], ps[:, 7, :], 128.0 * EPS, [PB[7]], EB[3])
                        S.op(dve, lambda h, hb=hb, m=m: h.scalar_tensor_tensor(
                            out=oT_b[:, hb, m * 512:(m + 1) * 512], in0=ev[:, 2, :], scalar=dnrm[:, l:l + 1],
                            in1=ev[:, 3, :], op0=ALU.mult, op1=ALU.mult), reads=[EB[2], EB[3], GB], writes=[OBB])
                S.barrier()

        WC = 3072

        def emit_dsa(l, oT_c, OCB):
            with ExitStack() as fs:
                def fsb(name, shape, dt):
                    uid[0] += 1
                    return fs.enter_context(nc.sbuf_tensor(f"{name}_{uid[0]}", list(shape), dt))
                KK = fsb("c_KK", [128, 2 * NT], BF16)
                QQ = fsb("c_QQ", [128, 8, 512], BF16)
                Vc = fsb("c_V", [128, 2 * NT // 128, 64], BF16)
                G = fsb("c_G", [128, 4, WC], BF16)
                cmb = fsb("c_cmb", [128, 2, 1024], BF16)
                sc = fsb("c_sc", [128, 2, T], F32)
                rlb = fsb("c_rl", [128, 3, 512], BF16)
                dg = fsb("c_dg", [128, 2, 8, 128], BF16)
                mx = fsb("c_mx", [128, 8], F32)
                mneg = fsb("c_mneg", [128, T], BF16)
                Pt = fsb("c_P", [128, 2, 512], BF16)
                ev = fsb("c_ev", [64, 512], F32)
                B_ld = Buf()
                QLB = Buf()
                GLB = Buf()
                SCB = [Buf(), Buf()]
                RLB = [Buf(), Buf(), Buf()]
                DGB = [Buf(), Buf()]
                MXB = Buf()
                MNB = Buf()
                PtB = [Buf(), Buf()]
                EVB = Buf()
                KTf = KT_f.rearrange("(g r c p) n -> g c p r n", g=4, r=2, c=3, p=128)
                QTi = QT_si.rearrange("(c p) n -> c p n", p=128)
                Vf = V_f.rearrange("(t p) c -> p t c", p=128)
                S.dma(sp, KK[:, :].rearrange("p (r n) -> p r n", r=2), KTf[3, 1], writes=[B_ld])
                S.dma(sp, Vc[:], Vf[:, :, 1280:1344], writes=[B_ld])
                S.dma(pool, cmb[:], cm_in.rearrange("k p n -> p k n"), writes=[B_ld])
                nq = 0
                nr = 0
                nacc = 0
                for m in range(NM):
                    gsN = 2 * m + 1
                    nkeys = (gsN + 1) * 512
                    nch = nkeys // 512
                    ms = slice(m * 512, m * 512 + 512)
                    for i in range(8):
                        S.dma(sp, QQ[64:128, i, :], QTi[12 + i // 2, (i % 2) * 64:(i % 2) * 64 + 64, ms], writes=[QLB])
                    for i in range(4):
                        S.dma(sp, QQ[0:64, i, :], QTi[10 + i // 2, (i % 2) * 64:(i % 2) * 64 + 64, ms], writes=[QLB])
                        S.dma(sp, G[:, i, :], strip_src(3, m % 2, 16 + i, WC), writes=[GLB])
                    for qb in range(4):
                        ql = slice(qb * 128, qb * 128 + 128)
                        q0 = m * 512 + qb * 128
                        t128 = q0 // 128
                        ks = nq % 2
                        nq += 1
                        for hh in range(8):
                            wcol = widx[:, t128 * 8 + hh:t128 * 8 + hh + 1]
                            S.op(pool, lambda h, ks=ks, hh=hh, wcol=wcol: h.tensor_scalar(
                                out=dg[:, ks, hh, :], in0=ident, scalar1=wcol, scalar2=None, op0=ALU.mult),
                                reads=[CB, WIB], writes=[DGB[ks]])
                        for kc in range(nch):
                            r, li = gpos(kc)
                            col = r * NT + li * 512
                            ab = 6 + nacc % 2
                            nacc += 1
                            pend = None
                            for hh in range(8):
                                b = nr % 2
                                k = nr % 3
                                nr += 1
                                mm(ps[:, b, :], QQ[64:128, hh, ql], KK[64:128, col:col + 512],
                                   True, True, [B_ld, QLB], [PB[b]])
                                S.op(act, lambda h, b=b, k=k: h.activation(out=rlb[:, k, :], in_=ps[:, b, :], func=AF.Relu),
                                     reads=[PB[b]], writes=[RLB[k]])
                                if pend is not None:
                                    pend()

                                def pend(hh=hh, k=k, ab=ab, ks=ks):
                                    mm(ps[:, ab, :], dg[:, ks, hh, :], rlb[:, k, :], hh == 0, hh == 7, [DGB[ks], RLB[k]], [PB[ab]])
                            pend()
                            S.op(act, lambda h, ab=ab, ks=ks, kc=kc: h.activation(
                                out=sc[:, ks, kc * 512:(kc + 1) * 512], in_=ps[:, ab, :], func=AF.Copy),
                                reads=[PB[ab]], writes=[SCB[ks]])
                        lo = gsN * 512 + 128 * qb - 512
                        hi = nkeys
                        S.op(dve, lambda h, ks=ks, lo=lo, hi=hi, m=m: h.tensor_tensor(
                            out=sc[:, ks, lo:hi], in0=sc[:, ks, lo:hi], in1=cmb[:, m % 2, 0:hi - lo], op=ALU.add),
                            reads=[SCB[ks], B_ld], writes=[SCB[ks]])
                        for it in range(32):
                            S.op(dve, lambda h, ks=ks: h.max(out=mx[:], in_=sc[:, ks, 0:nkeys]), reads=[SCB[ks]], writes=[MXB])
                            S.op(dve, lambda h, ks=ks: h.match_replace(out=sc[:, ks, 0:nkeys], in_to_replace=mx[:],
                                                                       in_values=sc[:, ks, 0:nkeys], imm_value=-3.0e38),
                                 reads=[SCB[ks], MXB], writes=[SCB[ks]])
                        S.op(dve, lambda h, ks=ks: h.tensor_scalar(out=mneg[:, 0:nkeys], in0=sc[:, ks, 0:nkeys],
                                                                   scalar1=-1.0e38, scalar2=-BIG, op0=ALU.is_gt,
                                                                   op1=ALU.mult), reads=[SCB[ks]], writes=[MNB])
                        S.op(dve, lambda h, lo=lo, hi=hi, m=m: h.tensor_tensor(
                            out=mneg[:, lo:hi], in0=mneg[:, lo:hi], in1=cmb[:, m % 2, 0:hi - lo], op=ALU.add),
                            reads=[MNB, B_ld], writes=[MNB])
                        nkt = nkeys // 128
                        pendc = None
                        for kt in range(nkt):
                            kcg = kcol(kt)
                            D0 = gsN * 512 - 128 * kt
                            c0 = (D0 + 384 if D0 < 2176 else 2560) + 128 * qb
                            b = 2 + kt % 2
                            k = kt % 2
                            for hc in range(4):
                                o = ps[:, b, hc * 128:(hc + 1) * 128]
                                mm(o, KK[0:64, kcg:kcg + 128], QQ[0:64, hc, ql], True, False, [B_ld, QLB], [PB[b]])
                                mm(o, antiI, G[:, hc, c0:c0 + 128], False, False, [GLB, CB], [PB[b]])
                                mm(o, mneg[:, kt * 128:(kt + 1) * 128], ident, False, True, [MNB, CB], [PB[b]])
                            S.op(act, lambda h, b=b, k=k: h.activation(out=Pt[:, k, :], in_=ps[:, b, :], func=AF.Exp),
                                 reads=[PB[b]], writes=[PtB[k]])
                            if pendc is not None:
                                pendc()

                            def pendc(kt=kt, k=k, nkt=nkt):
                                for hc in range(4):
                                    mm(ps[0:64, 4, hc * 128:(hc + 1) * 128], Vc[:, vtile(kt), :],
                                       Pt[:, k, hc * 128:(hc + 1) * 128], kt == 0 and hc == 0, kt == nkt - 1,
                                       [B_ld, PtB[k]], [PB[4]])
                                mm(ps[0:64, 5, :], ones[:, 0:64], Pt[:, k, :], kt == 0, kt == nkt - 1, [CB, PtB[k]], [PB[5]])
                        pendc()
                        S.op(dve, lambda h: h.reciprocal(out=ev[:], in_=ps[0:64, 5, :]), reads=[PB[5]], writes=[EVB])
                        for hc in range(4):
                            S.op(dve, lambda h, hc=hc, q0=q0: h.tensor_tensor(
                                out=oT_c[:, hc, q0:q0 + 128], in0=ps[0:64, 4, hc * 128:(hc + 1) * 128],
                                in1=ev[:, hc * 128:(hc + 1) * 128], op=ALU.mult), reads=[PB[4], EVB], writes=[OCB])
                S.barrier()

        def emit_post(l, oT_a, oT_b, oT_c, OAB, OBB, OCB):
            with ExitStack() as fs:
                def fsb(name, shape, dt):
                    uid[0] += 1
                    return fs.enter_context(nc.sbuf_tensor(f"{name}_{uid[0]}", list(shape), dt))
                hT = fsb("o_hT", [128, 1, 8, 512], BF16)
                yT = fsb("o_yT", [128, 8, 512], BF16)
                wga = fsb("o_wg", [128, 3, 8, 128], BF16)
                wa = fsb("o_wa", [64, 4, D], BF16)
                wb = fsb("o_wb", [128, 4, D], BF16)
                wc = fsb("o_wc", [64, 4, D], BF16)
                wo = fsb("o_wo", [128, 8, D], BF16)
                sg = fsb("o_sg", [128, 2, 512], F32)
                tmp = fsb("o_tmp", [128, 2, 512], F32)
                yacc = fsb("o_yacc", [128, 512], F32)
                HB = [Buf(), Buf()]
                YB = Buf()
                WGB = [Buf() for _ in range(3)]
                WB = Buf()
                SGB = [Buf(), Buf()]
                TMB = [Buf(), Buf()]
                YAB = Buf()
                winv = w_in[l].rearrange("(c p) n -> p c n", p=128)
                S.dma(pool, wa[:], w_br[0][l].rearrange("(h p) n -> p h n", p=64), writes=[WB])
                S.dma(pool, wb[:], w_br[1][l].rearrange("(h p) n -> p h n", p=128), writes=[WB])
                S.dma(pool, wc[:], w_br[2][l].rearrange("(h p) n -> p h n", p=64), writes=[WB])
                S.dma(pool, wo[:], w_out[l].rearrange("(c p) n -> p c n", p=128), writes=[WB])
                nw = 0
                ns = 0
                for tt in range(NTT):
                    tok = slice(tt * 512, tt * 512 + 512)
                    kh = 0
                    S.dma(sp, hT[:, kh], hT_si.rearrange("(c p) n -> p c n", p=128)[:, :, tok], writes=[HB[kh]])
                    for oc in range(8):
                        for i in range(3):
                            k3 = nw % 3
                            nw += 1
                            g0 = 4808 + i * 1024 + oc * 128
                            S.dma(pool, wga[:, k3], winv[:, :, g0:g0 + 128], writes=[WGB[k3]])
                            bg = (oc * 3 + i) % 2
                            bb = 2 + (oc * 3 + i) % 2
                            for c in range(8):
                                mm(ps[:, bg, :], wga[:, k3, c, :], hT[:, kh, c, :], c == 0, c == 7, [WGB[k3], HB[kh]], [PB[bg]])
                            ocs = slice(oc * 128, oc * 128 + 128)
                            if i == 0:
                                for hh in range(4):
                                    mm(ps[:, bb, :], wa[:, hh, ocs], oT_a[:, hh, tok], hh == 0, hh == 3, [WB, OAB], [PB[bb]])
                            elif i == 1:
                                for hh in range(4):
                                    mm(ps[:, bb, :], wb[:, hh, ocs], oT_b[:, hh, tok], hh == 0, hh == 3, [WB, OBB], [PB[bb]])
                            else:
                                for hh in range(4):
                                    mm(ps[:, bb, :], wc[:, hh, ocs], oT_c[:, hh, tok], hh == 0, hh == 3, [WB, OCB], [PB[bb]])
                            k = ns % 2
                            ns += 1
                            S.op(act, lambda h, k=k, bg=bg: h.activation(out=sg[:, k, :], in_=ps[:, bg, :], func=AF.Sigmoid),
                                 reads=[PB[bg]], writes=[SGB[k]])
                            if i == 0:
                                S.op(dve, lambda h, k=k, bb=bb: h.tensor_tensor(out=yacc[:], in0=sg[:, k, :], in1=ps[:, bb, :],
                                                                               op=ALU.mult), reads=[SGB[k], PB[bb]], writes=[YAB])
                            else:
                                S.op(dve, lambda h, k=k, bb=bb: h.tensor_tensor(out=tmp[:, k, :], in0=sg[:, k, :],
                                                                               in1=ps[:, bb, :], op=ALU.mult),
                                     reads=[SGB[k], PB[bb]], writes=[TMB[k]])
                                if i == 1:
                                    S.op(pool, lambda h, k=k: h.tensor_tensor(out=yacc[:], in0=yacc[:], in1=tmp[:, k, :],
                                                                             op=ALU.add), reads=[YAB, TMB[k]], writes=[YAB])
                                else:
                                    S.op(pool, lambda h, k=k, oc=oc: h.tensor_tensor(out=yT[:, oc, :], in0=yacc[:],
                                                                                    in1=tmp[:, k, :], op=ALU.add),
                                         reads=[YAB, TMB[k]], writes=[YB])
                    for oc in range(8):
                        bo = 4 + oc % 2
                        for c in range(8):
                            mm(ps[:, bo, :], wo[:, c, oc * 128:(oc + 1) * 128], yT[:, c, :], c == 0, c == 7, [WB, YB], [PB[bo]])
                        S.op(dve, lambda h, bo=bo, oc=oc, tok=tok: h.tensor_tensor(out=xT[:, oc, tok], in0=ps[:, bo, :],
                                                                                  in1=xT[:, oc, tok], op=ALU.add),
                             reads=[PB[bo], XB[tt]], writes=[XB[tt]])
                S.barrier()

        S.barrier()
        if has_b:
            emit_vec()
        for (ph, l) in parts:
            if fused:
                KT_l, V_l, KT_f, V_f = KT_l2[l % 2], V_l2[l % 2], KT_f2[l % 2], V_f2[l % 2]
            if ph == "a":
                emit_ffn(l, 0)
                emit_pre(l)
                if fused:
                    S.drain(pool)
                    groups = [[2 * i, 2 * i + 1] for i in range(ncores // 2)]
                    pieces = [(KT_l[g * 384:(g + 1) * 384, :], KT_f[g * 768:(g + 1) * 768, :]) for g in range(4)]
                    pieces += [(V_l[g * 512:(g + 1) * 512, :], V_f[g * 1024:(g + 1) * 1024, :]) for g in range(NM)]
                    for (src_, dst_) in pieces:
                        ins = nc.gpsimd.collective_compute("AllGather", ALU.bypass, replica_groups=groups,
                                                           ins=[src_.opt()], outs=[dst_.opt()])
                        S.cc_val += 1
                        ins.then_inc(S.cc_sem)
                    S.barrier()
            elif ph == "b":
                if not fused and (ph, l) == parts[0]:
                    S.dma(sp, widx[:], widx_i, writes=[WIB])
                with ExitStack() as bs:
                    oT_c = bs.enter_context(nc.sbuf_tensor(f"oT_c{l}", [64, 4, NT], BF16))
                    OAB, OBB, OCB = Buf(), Buf(), Buf()
                    if "c" in MIX:
                        emit_dsa(l, oT_c, OCB)
                    oT_a = bs.enter_context(nc.sbuf_tensor(f"oT_a{l}", [64, 4, NT], BF16))
                    oT_b = bs.enter_context(nc.sbuf_tensor(f"oT_b{l}", [128, 4, NT], BF16))
                    emit_att(l, oT_a, oT_b, OAB, OBB)
                    emit_post(l, oT_a, oT_b, oT_c, OAB, OBB, OCB)
                emit_ffn(l, 1)
        for tt in range(NTT):
            S.dma(sp, xT_out.rearrange("(c p) n -> p c n", p=128)[:, :, tt * 512:(tt + 1) * 512],
                  xT[:, :, tt * 512:(tt + 1) * 512], reads=[XB[tt]])
        S.barrier()
    nc._n_ins = S.n_ins
    return nc


def rel_bucket_np(dist):
    n = np.maximum(dist, 0)
    nf = np.maximum(n, 1).astype(np.float32)
    large = 16 + (np.log(nf / np.float32(16)) / np.float32(math.log(2048 / 16)) * np.float32(16)).astype(np.int32)
    large = np.minimum(large, 31)
    return np.where(n < 16, n, large)


def make_onehot(e_par):
    oh = np.zeros((NKIND * 2, 33, LV), np.float32)
    v = np.arange(LV)
    for kind in range(NKIND):
        for p in range(2):
            dist = v - VOFF - 512 * e_par[p]
            if kind < 3:
                w, d = DIL[kind]
                valid = (dist >= 0) & (dist <= w) & (dist % d == 0)
            else:
                valid = dist >= 0
            bk = rel_bucket_np(dist)
            o = oh[kind * 2 + p]
            o[bk[valid], v[valid]] = 1.0
            o[32, v[~valid]] = 1.0
    return oh


def make_cm(e_par):
    cm = np.zeros((2, 128, 1024), np.float32)
    u = np.arange(1024)[None, :]
    qi = np.arange(128)[:, None]
    for p in range(2):
        cm[p] = np.where(u - 512 + 512 * e_par[p] <= qi, 0.0, -BIG)
    return cm


def local_superblocks(hf, ns):
    return [gs for gs in range(ns) if (gs % 4 in (0, 3)) == (hf == 0)]


_PROG = {}


def _get_prog(T, L, parts, fused, lbase=0, ncores=8):
    key = (T, L, tuple(parts), fused, lbase, ncores)
    if key not in _PROG:
        _PROG[key] = build(T, L, list(parts), fused, lbase, ncores)
    return _PROG[key]


A_KEYS = ("ffn1_w_gate", "ffn1_w_up", "ffn1_w_down", "w_in")
B_KEYS = ("ffn2_w_gate", "ffn2_w_up", "ffn2_w_down", "w_in", "w_branch_a", "w_branch_b", "w_branch_c", "w_out")


def run_model(inputs, T, L, B, fused=True):
    x = np.asarray(inputs["x"], np.float32)
    NT = T // 2
    NS = T // 512
    ncores = 2 * B
    consts = np.zeros((4, 128, 128), np.float32)
    consts[0] = np.eye(128)
    consts[1] = np.eye(128)[::-1]
    consts[2] = 1.0
    consts[3, :64, :64] = 1.0
    consts[3, 64:, 64:] = 1.0
    norms = np.stack([inputs["ffn1_norm"], inputs["mix_norm"], inputs["ffn2_norm"]], 1)
    normsT = np.ascontiguousarray(norms.reshape(L, 3, 8, 128).transpose(3, 0, 1, 2).reshape(128, L * 24)).astype(np.float32)
    qg = np.asarray(inputs["qk_gain"], np.float32).reshape(L * 6, 64)
    qkg = np.ascontiguousarray(np.concatenate([qg, qg], 1).T)
    dnorm = np.ascontiguousarray(np.asarray(inputs["diff_out_norm"], np.float32).T)
    dlam = np.ascontiguousarray(np.broadcast_to(np.asarray(inputs["diff_lambda"], np.float32).reshape(1, L * 256), (128, L * 256)))
    shared = {"consts": consts, "normsT": normsT, "qkg": qkg, "dnorm": dnorm, "dlam": dlam,
              "rel_bias": np.asarray(inputs["rel_bias"], np.float32)}
    for k in ("ffn1_w_gate", "ffn1_w_up", "ffn1_w_down", "ffn2_w_gate", "ffn2_w_up", "ffn2_w_down", "w_in",
              "w_branch_a", "w_branch_b", "w_branch_c", "w_out"):
        shared[k] = np.asarray(inputs[k], np.float32)
    percore = []
    for c in range(ncores):
        b, hf = c // 2, c % 2
        sbs = local_superblocks(hf, NS)
        xs = np.concatenate([x[b, gs * 512:(gs + 1) * 512] for gs in sbs], 0)
        e_par = [1, 0] if hf == 0 else [0, 1]
        percore.append({"xT_in": np.ascontiguousarray(xs.T), "onehot": make_onehot(e_par), "cm": make_cm(e_par)})
    cores = list(range(ncores))
    if fused:
        parts = []
        for l in range(L):
            parts += [("a", l), ("b", l)]
        nc = _get_prog(T, L, parts, True, 0, ncores)
        res = run_bass_kernel_spmd(nc, [dict(shared, **pc) for pc in percore], core_ids=cores)
        outs = [r["xT_out"] for r in res.results]
    else:
        state = [pc["xT_in"] for pc in percore]
        small = {k: shared[k] for k in ("consts",)}
        for l in range(L):
            sm = dict(small)
            sm["normsT"] = np.ascontiguousarray(normsT[:, l * 24:(l + 1) * 24])
            sm["qkg"] = np.ascontiguousarray(qkg[:, l * 6:(l + 1) * 6])
            sm["dnorm"] = np.ascontiguousarray(dnorm[:, l:l + 1])
            sm["dlam"] = np.ascontiguousarray(dlam[:, l * 256:(l + 1) * 256])
            sa = dict(sm)
            for k in A_KEYS:
                sa[k] = shared[k][l:l + 1]
            nca = _get_prog(T, 1, [("a", 0)], False, 0)
            res = run_bass_kernel_spmd(nca, [dict(sa, xT_in=state[i]) for i in range(ncores)], core_ids=cores)
            ra = res.results
            sbm = dict(sm)
            for k in B_KEYS:
                sbm[k] = shared[k][l:l + 1]
            sbm["rel_bias"] = shared["rel_bias"]
            ncb = _get_prog(T, 1, [("b", 0)], False, l)
            maps = []
            for i, pc in enumerate(percore):
                p0 = (i // 2) * 2
                m = dict(sbm, onehot=pc["onehot"], cm=pc["cm"])
                m["xT_in"] = ra[i]["xT_out"]
                m["hT_i"] = ra[i]["hT_o"]
                m["QT_i"] = ra[i]["QT_o"]
                m["widx_i"] = ra[i]["widx_o"]
                k0, k1 = np.asarray(ra[p0]["KT_o"]), np.asarray(ra[p0 + 1]["KT_o"])
                m["KT_f"] = np.concatenate([np.concatenate([k0[g * 384:(g + 1) * 384], k1[g * 384:(g + 1) * 384]], 0)
                                            for g in range(4)], 0)
                v0, v1 = np.asarray(ra[p0]["V_o"]), np.asarray(ra[p0 + 1]["V_o"])
                m["V_f"] = np.concatenate([np.concatenate([v0[g * 512:(g + 1) * 512], v1[g * 512:(g + 1) * 512]], 0)
                                           for g in range(NT // 512)], 0)
                maps.append(m)
            res = run_bass_kernel_spmd(ncb, maps, core_ids=cores)
            state = [r["xT_out"] for r in res.results]
        outs = state
    out = np.zeros((B, T, D), np.float32)
    for c in range(ncores):
        b, hf = c // 2, c % 2
        sbs = local_superblocks(hf, NS)
        xo = np.asarray(outs[c]).T
        for i, gs in enumerate(sbs):
            out[b, gs * 512:(gs + 1) * 512] = xo[i * 512:(i + 1) * 512]
    return out


FUSED = True


def kernel(**inputs):
    x = np.asarray(inputs["x"])
    B, T, _ = x.shape
    L = np.asarray(inputs["w_in"]).shape[0]
    return run_model(inputs, T, L, B, fused=FUSED)
```

```python
import math
from contextlib import ExitStack
import numpy as np
import concourse.bass as bass
import concourse.mybir as mybir
from concourse.bass_utils import run_bass_kernel_spmd

F32 = mybir.dt.float32
BF16 = mybir.dt.bfloat16
AF = mybir.ActivationFunctionType
ALU = mybir.AluOpType
AX = mybir.AxisListType

D = 1024
DFF = 2816
NF = DFF // 128
NIN = 7880
EPS = 1e-6
BIG = 30000.0
LV = 3840
WD = 3712
VOFF = 511
NKIND = 4
DIL = ((128, 1), (512, 4), (2048, 16))
R_DMA = 8
MIX = "abc"


class Buf:
    __slots__ = ("w", "r")

    def __init__(self):
        self.w = None
        self.r = {}


class Eng:
    def __init__(self, name, h, sem, same):
        self.name = name
        self.h = h
        self.sem = sem
        self.cnt = 0
        self.seen = {}
        self.same = same


class Sched:
    def __init__(self, nc, es):
        self.nc = nc
        mk = lambda n: es.enter_context(nc.semaphore(n))
        self.pe = Eng("pe", nc.tensor, mk("s_pe"), False)
        self.act = Eng("act", nc.scalar, mk("s_act"), True)
        self.dve = Eng("dve", nc.vector, mk("s_dve"), True)
        self.pool = Eng("pool", nc.gpsimd, mk("s_pool"), True)
        self.sp = Eng("sp", nc.sync, mk("s_sp"), False)
        self.engs = [self.pe, self.act, self.dve, self.pool, self.sp]
        self.dq = {}
        for e in (self.sp, self.pool):
            self.dq[e.name] = {"sems": [mk(f"d_{e.name}{i}") for i in range(R_DMA)], "vals": [0] * R_DMA, "i": 0}
        self.n_ins = 0
        self.cc_sem = mk("cc_sem")
        self.cc_val = 0

    def _deps(self, reads, writes):
        d = {}

        def add(t):
            if t is None:
                return
            k, s, v = t
            if k not in d or d[k][1] < v:
                d[k] = (s, v)

        for b in reads:
            add(b.w)
        for b in writes:
            add(b.w)
            for t in b.r.values():
                add(t)
        return d

    def _filter(self, eng, deps):
        out = []
        for key, (sem, val) in deps.items():
            if key == eng.name and not eng.same:
                continue
            if eng.seen.get(key, 0) >= val:
                continue
            eng.seen[key] = val
            out.append((key, sem, val))
        out.sort(key=lambda t: 0 if t[0] == eng.name else 1)
        return out

    def _emit(self, eng, fn, waits):
        for (_, sem, val) in waits[1:]:
            eng.h.wait_ge(sem, val)
        ins = fn(eng.h)
        if waits:
            ins._wait_ge(waits[0][1], waits[0][2])
        self.n_ins += 1 + max(0, len(waits) - 1)
        return ins

    def op(self, eng, fn, reads=(), writes=()):
        waits = self._filter(eng, self._deps(reads, writes))
        ins = self._emit(eng, fn, waits)
        ins.then_inc(eng.sem, 1)
        eng.cnt += 1
        tok = (eng.name, eng.sem, eng.cnt)
        for b in reads:
            b.r[eng.name] = tok
        for b in writes:
            b.w = tok
            b.r = {}
        return tok

    def dma(self, eng, out, in_, reads=(), writes=(), **kw):
        q = self.dq[eng.name]
        i = q["i"]
        q["i"] = (i + 1) % R_DMA
        sem = q["sems"][i]
        key = f"d_{eng.name}{i}"
        deps = self._deps(reads, writes)
        if q["vals"][i] > 0:
            deps[key] = (sem, q["vals"][i])
        waits = self._filter(eng, deps)
        ins = self._emit(eng, lambda h: h.dma_start(out=out, in_=in_, **kw), waits)
        q["vals"][i] += 16
        ins.then_inc(sem, 16)
        tok = (key, sem, q["vals"][i])
        for b in reads:
            b.r[key] = tok
        for b in writes:
            b.w = tok
            b.r = {}
        return tok

    def drain(self, eng):
        for e in self.engs:
            if e.cnt > 0 and eng.seen.get(e.name, 0) < e.cnt and e is not eng:
                eng.h.wait_ge(e.sem, e.cnt)
                eng.seen[e.name] = e.cnt
        if self.cc_val > 0 and eng.seen.get("cc", 0) < self.cc_val:
            eng.h.wait_ge(self.cc_sem, self.cc_val)
            eng.seen["cc"] = self.cc_val
        for qn, q in self.dq.items():
            for i in range(R_DMA):
                key = f"d_{qn}{i}"
                if q["vals"][i] > 0 and eng.seen.get(key, 0) < q["vals"][i]:
                    eng.h.wait_ge(q["sems"][i], q["vals"][i])
                    eng.seen[key] = q["vals"][i]

    def barrier(self):
        for e in self.engs:
            self.drain(e)


def gpos(gs):
    r = 0 if gs % 4 in (0, 3) else 1
    li = gs // 2
    return r, li


def build(T, L, parts, fused, lbase=0, ncores=8):
    NT = T // 2
    NS = T // 512
    NM = NS // 2
    NTT = NT // 512
    nc = bass.Bass("TRN2", target_bir_lowering=False)

    def din(name, shape, dt=F32):
        return nc.dram_tensor(name, list(shape), dt, kind="ExternalInput").ap()

    def dout(name, shape, dt=F32):
        return nc.dram_tensor(name, list(shape), dt, kind="ExternalOutput").ap()

    def dscr(name, shape, dt, io):
        if fused:
            return nc.dram_tensor(name, list(shape), dt, kind="Internal").ap()
        return nc.dram_tensor(name, list(shape), dt, kind="ExternalInput" if io == "in" else "ExternalOutput").ap()

    has_a = any(p[0] == "a" for p in parts)
    has_b = any(p[0] == "b" for p in parts)
    first_is_b = parts[0][0] == "b"
    last_is_a = parts[-1][0] == "a"

    xT_in = din("xT_in", [D, NT])
    xT_out = dout("xT_out", [D, NT])
    consts = din("consts", [4, 128, 128])
    normsT = din("normsT", [128, L * 24])
    qkg = din("qkg", [128, L * 6])
    dnorm = din("dnorm", [128, L])
    dlam = din("dlam", [128, L * 256])
    w_gate = [din("ffn1_w_gate", [L, D, DFF]) if has_a else None, din("ffn2_w_gate", [L, D, DFF]) if has_b else None]
    w_up = [din("ffn1_w_up", [L, D, DFF]) if has_a else None, din("ffn2_w_up", [L, D, DFF]) if has_b else None]
    w_down = [din("ffn1_w_down", [L, DFF, D]) if has_a else None, din("ffn2_w_down", [L, DFF, D]) if has_b else None]
    w_in = din("w_in", [L, D, NIN])
    if has_b:
        w_br = [din("w_branch_a", [L, 256, D]), din("w_branch_b", [L, 512, D]), din("w_branch_c", [L, 256, D])]
        w_out = din("w_out", [L, D, D])
        rel_bias = din("rel_bias", [32, 20])
        onehot = din("onehot", [NKIND * 2, 33, LV])
        cm_in = din("cm", [2, 128, 1024])

    NQC = 16
    NKC = 12
    VW = 1344
    if fused:
        hT_s = dscr("hT_s", [8 * 128, NT], BF16, None)
        QT_s = dscr("QT_s", [NQC * 128, NT], BF16, None)
        KT_l2 = [dscr(f"KT_l{i}", [NKC * 128, NT], BF16, None) for i in range(2)]
        V_l2 = [dscr(f"V_l{i}", [NT, VW], BF16, None) for i in range(2)]
        KT_f2 = [dscr(f"KT_f{i}", [2 * NKC * 128, NT], BF16, None) for i in range(2)]
        V_f2 = [dscr(f"V_f{i}", [2 * NT, VW], BF16, None) for i in range(2)]
        KT_l, V_l, KT_f, V_f = KT_l2[0], V_l2[0], KT_f2[0], V_f2[0]
        hT_si = hT_s
        QT_si = QT_s
    else:
        if last_is_a:
            hT_s = dscr("hT_o", [8 * 128, NT], BF16, "out")
            QT_s = dscr("QT_o", [NQC * 128, NT], BF16, "out")
            KT_l = dscr("KT_o", [NKC * 128, NT], BF16, "out")
            V_l = dscr("V_o", [NT, VW], BF16, "out")
            widx_o = dscr("widx_o", [128, NT // 128 * 8], F32, "out")
        if first_is_b:
            hT_si = dscr("hT_i", [8 * 128, NT], BF16, "in")
            QT_si = dscr("QT_i", [NQC * 128, NT], BF16, "in")
            KT_f = dscr("KT_f", [2 * NKC * 128, NT], BF16, "in")
            V_f = dscr("V_f", [2 * NT, VW], BF16, "in")
            widx_i = dscr("widx_i", [128, NT // 128 * 8], F32, "in")
    vec_s = nc.dram_tensor("vec_s", [NKIND * 2 * 20, LV], BF16, kind="Internal").ap()

    es = ExitStack()
    uid = [0]
    with es:
        S = Sched(nc, es)
        pe, act, dve, pool, sp = S.pe, S.act, S.dve, S.pool, S.sp

        def sb(name, shape, dt):
            return es.enter_context(nc.sbuf_tensor(name, list(shape), dt))

        xT = sb("xT", [128, 8, NT], F32)
        XB = [Buf() for _ in range(NTT)]
        ps = es.enter_context(nc.psum_tensor("ps", [128, 8, 512], F32))
        PB = [Buf() for _ in range(8)]
        cst = sb("cst", [128, 4, 128], BF16)
        CB = Buf()
        ident, antiI, ones, bones = cst[:, 0, :], cst[:, 1, :], cst[:, 2, :], cst[:, 3, :]
        g32 = sb("g32", [128, L * 24], F32)
        qkgs = sb("qkgs", [128, L * 6], F32)
        dnrm = sb("dnrm", [128, L], F32)
        lamt = sb("lamt", [128, L * 256], F32)
        neglam = sb("neglam", [128, L], F32)
        widx = sb("widx", [128, NT // 128 * 8], F32)
        WIB = Buf()
        GB = Buf()

        S.dma(pool, cst[:], consts.rearrange("k p n -> p k n"), writes=[CB])
        epsb = sb("epsb", [128, 3], F32)
        ci = {1024.0 * EPS: 0, 64.0 * EPS: 1, 128.0 * EPS: 2}
        for cval, cidx in ci.items():
            S.op(dve, lambda h, cval=cval, cidx=cidx: h.memset(epsb[:, cidx:cidx + 1], cval), writes=[CB])
        for tt in range(NTT):
            S.dma(sp, xT[:, :, tt * 512:(tt + 1) * 512],
                  xT_in.rearrange("(c p) n -> p c n", p=128)[:, :, tt * 512:(tt + 1) * 512], writes=[XB[tt]])
        S.dma(sp, g32[:], normsT, writes=[GB])
        S.dma(sp, qkgs[:], qkg, writes=[GB])
        S.dma(sp, dnrm[:], dnorm, writes=[GB])
        S.dma(sp, lamt[:], dlam, writes=[GB])
        S.op(dve, lambda h: h.tensor_scalar(out=g32[:], in0=g32[:], scalar1=32.0, scalar2=None, op0=ALU.mult),
             reads=[GB], writes=[GB])
        for l in range(L):
            for i in range(3):
                c = l * 6 + i * 2 + 1
                S.op(dve, lambda h, c=c: h.tensor_scalar(out=qkgs[:, c:c + 1], in0=qkgs[:, c:c + 1], scalar1=8.0,
                                                         scalar2=None, op0=ALU.mult), reads=[GB], writes=[GB])
        if has_b:
            ltmp = sb("ltmp", [128, 64], F32)
            lsum = sb("lsum", [128, 4], F32)
            LB = Buf()
            for l in range(L):
                lam_init = 0.8 - 0.6 * math.exp(-0.3 * (l + lbase))
                for k in range(2):
                    a0 = l * 256 + k * 128
                    S.op(dve, lambda h, a0=a0: h.tensor_tensor(out=ltmp[:], in0=lamt[:, a0:a0 + 64],
                                                               in1=lamt[:, a0 + 64:a0 + 128], op=ALU.mult),
                         reads=[GB], writes=[LB])
                    S.op(dve, lambda h, k=k: h.tensor_reduce(out=lsum[:, k:k + 1], in_=ltmp[:], axis=AX.X, op=ALU.add),
                         reads=[LB], writes=[LB])
                    S.op(act, lambda h, k=k: h.activation(out=lsum[:, 2 + k:3 + k], in_=lsum[:, k:k + 1], func=AF.Exp),
                         reads=[LB], writes=[LB])
                S.op(dve, lambda h, l=l, li=lam_init: h.scalar_tensor_tensor(
                    out=neglam[:, l:l + 1], in0=lsum[:, 3:4], scalar=-li, in1=lsum[:, 2:3], op0=ALU.add,
                    op1=ALU.subtract), reads=[LB], writes=[GB])
                S.op(dve, lambda h, l=l, li=lam_init: h.tensor_scalar(
                    out=dnrm[:, l:l + 1], in0=dnrm[:, l:l + 1], scalar1=(1.0 - li) * math.sqrt(128.0), scalar2=None,
                    op0=ALU.mult), reads=[GB], writes=[GB])

        bank_rr = {"i": 0}

        def mm(out, lhsT, rhs, start, stop, reads, writes):
            S.op(pe, lambda h: h.matmul(out, lhsT, rhs, start=start, stop=stop), reads=reads, writes=writes)

        def rsq(out, in_, c, reads, wbuf):
            S.op(act, lambda h: h.activation(out=out, in_=in_, func=AF.Sqrt, bias=epsb[0:out.shape[0], ci[c]:ci[c] + 1], scale=1.0),
                 reads=list(reads) + [CB], writes=[wbuf])
            S.op(dve, lambda h: h.reciprocal(out=out, in_=out), reads=[wbuf], writes=[wbuf])

        def emit_rms(gcol0, tgs, hT, HB, tok0, sq, SQB, rstd, RB, banks):
            for n, tg in enumerate(tgs):
                b = banks[n % len(banks)]
                tok = slice(tg * 512, tg * 512 + 512)
                lt = slice((tg - tok0) * 512, (tg - tok0) * 512 + 512)
                for c in range(8):
                    k = c % 2
                    S.op(act, lambda h, c=c, k=k: h.activation(out=sq[:, k, :], in_=xT[:, c, tok], func=AF.Square),
                         reads=[XB[tg]], writes=[SQB[k]])
                    mm(ps[:, b, :], ones, sq[:, k, :], c == 0, c == 7, [SQB[k], CB], [PB[b]])
                rsq(rstd[:], ps[:, b, :], 1024.0 * EPS, [PB[b]], RB)
                for c in range(8):
                    S.op(dve, lambda h, c=c: h.scalar_tensor_tensor(
                        out=hT[:, c, lt], in0=xT[:, c, tok], scalar=g32[:, gcol0 + c:gcol0 + c + 1], in1=rstd[:],
                        op0=ALU.mult, op1=ALU.mult), reads=[XB[tg], RB, GB], writes=[HB[tg - tok0]])

        def emit_ffn(l, which):
            with ExitStack() as fs:
                def fsb(name, shape, dt):
                    uid[0] += 1
                    return fs.enter_context(nc.sbuf_tensor(f"{name}_{uid[0]}", list(shape), dt))
                NH = 1024 if NT >= 1024 else NT
                ntt = NH // 512
                hT = fsb("f_hT", [128, 8, NH], BF16)
                aT = fsb("f_aT", [128, NF, NH], BF16)
                sq = fsb("f_sq", [128, 2, 512], BF16)
                rstd = fsb("f_rstd", [128, 512], F32)
                sg = fsb("f_sg", [128, 2, 512], F32)
                wg = fsb("f_wg", [128, 3, 8, 128], BF16)
                wu = fsb("f_wu", [128, 3, 8, 128], BF16)
                wd = fsb("f_wd", [128, 2, NF, 128], BF16)
                HB = [Buf() for _ in range(ntt)]
                AB = [[Buf() for _ in range(ntt)] for _ in range(NF)]
                SQB = [Buf(), Buf()]
                RB = Buf()
                SGB = [Buf(), Buf()]
                WGB = [Buf() for _ in range(3)]
                WUB = [Buf() for _ in range(3)]
                WDB = [Buf(), Buf()]
                wgv = w_gate[which][l].rearrange("(c p) n -> p c n", p=128)
                wuv = w_up[which][l].rearrange("(c p) n -> p c n", p=128)
                wdv = w_down[which][l].rearrange("(j p) n -> p j n", p=128)
                gcol0 = l * 24 + (0 if which == 0 else 16)
                nsg = 0
                for th in range(NT // NH):
                    tgs = [th * ntt + i for i in range(ntt)]
                    emit_rms(gcol0, tgs, hT, HB, th * ntt, sq, SQB, rstd, RB, [6, 7])
                    for j in range(NF):
                        k3 = j % 3
                        S.dma(pool, wg[:, k3], wgv[:, :, j * 128:(j + 1) * 128], writes=[WGB[k3]])
                        S.dma(pool, wu[:, k3], wuv[:, :, j * 128:(j + 1) * 128], writes=[WUB[k3]])
                        for tt in range(ntt):
                            bg = (j * ntt + tt) % 2
                            bu = 2 + (j * ntt + tt) % 2
                            tl = slice(tt * 512, tt * 512 + 512)
                            for c in range(8):
                                mm(ps[:, bg, :], wg[:, k3, c, :], hT[:, c, tl], c == 0, c == 7, [WGB[k3], HB[tt]], [PB[bg]])
                            for c in range(8):
                                mm(ps[:, bu, :], wu[:, k3, c, :], hT[:, c, tl], c == 0, c == 7, [WUB[k3], HB[tt]], [PB[bu]])
                            k = nsg % 2
                            nsg += 1
                            S.op(act, lambda h, k=k, bg=bg: h.activation(out=sg[:, k, :], in_=ps[:, bg, :], func=AF.Silu),
                                 reads=[PB[bg]], writes=[SGB[k]])
                            S.op(dve, lambda h, k=k, bu=bu, j=j, tl=tl: h.tensor_tensor(
                                out=aT[:, j, tl], in0=sg[:, k, :], in1=ps[:, bu, :], op=ALU.mult),
                                reads=[SGB[k], PB[bu]], writes=[AB[j][tt]])
                    for oc in range(8):
                        k2 = oc % 2
                        S.dma(pool, wd[:, k2], wdv[:, :, oc * 128:(oc + 1) * 128], writes=[WDB[k2]])
                        for tt in range(ntt):
                            bo = 4 + (oc * ntt + tt) % 2
                            tg = th * ntt + tt
                            tl = slice(tt * 512, tt * 512 + 512)
                            tok = slice(tg * 512, tg * 512 + 512)
                            for j in range(NF):
                                mm(ps[:, bo, :], wd[:, k2, j, :], aT[:, j, tl], j == 0, j == NF - 1,
                                   [WDB[k2], AB[j][tt]], [PB[bo]])
                            S.op(dve, lambda h, bo=bo, oc=oc, tok=tok: h.scalar_tensor_tensor(
                                out=xT[:, oc, tok], in0=ps[:, bo, :], scalar=0.5, in1=xT[:, oc, tok], op0=ALU.mult,
                                op1=ALU.add), reads=[PB[bo], XB[tg]], writes=[XB[tg]])
                S.barrier()

        def emit_pre(l):
            with ExitStack() as fs:
                def fsb(name, shape, dt):
                    uid[0] += 1
                    return fs.enter_context(nc.sbuf_tensor(f"{name}_{uid[0]}", list(shape), dt))
                hT = fsb("p_hT", [128, 8, NT], BF16)
                sq = fsb("p_sq", [128, 2, 512], BF16)
                rstd = fsb("p_rstd", [128, 512], F32)
                wt = fsb("p_wt", [128, 3, 8, 128], BF16)
                st = fsb("p_st", [128, 2, NT], BF16)
                wv = fsb("p_wv", [128, 8, VW + 8], BF16)
                vst = fsb("p_vst", [128, 2, VW], BF16)
                HB = [Buf() for _ in range(NTT)]
                SQB = [Buf(), Buf()]
                RB = Buf()
                WTB = [Buf() for _ in range(3)]
                STB = [Buf(), Buf()]
                WVB = Buf()
                VSB = [Buf(), Buf()]
                winv = w_in[l].rearrange("(c p) n -> p c n", p=128)
                emit_rms(l * 24 + 8, list(range(NTT)), hT, HB, 0, sq, SQB, rstd, RB, [6, 7])
                for tt in range(NTT):
                    S.dma(sp, hT_s.rearrange("(c p) n -> p c n", p=128)[:, :, tt * 512:(tt + 1) * 512],
                          hT[:, :, tt * 512:(tt + 1) * 512], reads=[HB[tt]])
                S.dma(pool, wv[:, :, 0:768], winv[:, :, 1536:2304], writes=[WVB])
                S.dma(pool, wv[:, :, 768:1280], winv[:, :, 3328:3840], writes=[WVB])
                S.dma(pool, wv[:, :, 1280:1344], winv[:, :, 4160:4224], writes=[WVB])
                S.dma(pool, wv[:, :, 1344:1352], winv[:, :, 4800:4808], writes=[WVB])
                qc = l * 6
                chunks = []
                for i in range(6):
                    chunks.append(("n", i * 128, None, qc + 0, QT_s, i))
                for i in range(6):
                    chunks.append(("n", 768 + i * 128, None, qc + 1, KT_l, i))
                for i in range(4):
                    chunks.append(("n", 2304 + i * 128, None, qc + 2, QT_s, 6 + i))
                for i in range(4):
                    chunks.append(("n", 2816 + i * 128, None, qc + 3, KT_l, 6 + i))
                for i in range(2):
                    chunks.append(("n", 3840 + i * 128, None, qc + 4, QT_s, 10 + i))
                chunks.append(("ck", 4096, 4736, qc + 5, KT_l, 10))
                for i in range(4):
                    chunks.append(("p", 4224 + i * 128, None, None, QT_s, 12 + i))
                for ci, (kind, c0, c1, gcol, dst, dchunk) in enumerate(chunks):
                    k3 = ci % 3
                    if kind == "ck":
                        S.dma(pool, wt[:, k3, :, 0:64], winv[:, :, c0:c0 + 64], writes=[WTB[k3]])
                        S.dma(pool, wt[:, k3, :, 64:128], winv[:, :, c1:c1 + 64], writes=[WTB[k3]])
                    else:
                        S.dma(pool, wt[:, k3], winv[:, :, c0:c0 + 128], writes=[WTB[k3]])
                    ks = ci % 2
                    for tt in range(NTT):
                        b = (ci * NTT + tt) % 2
                        tl = slice(tt * 512, tt * 512 + 512)
                        for c in range(8):
                            mm(ps[:, b, :], wt[:, k3, c, :], hT[:, c, tl], c == 0, c == 7, [WTB[k3], HB[tt]], [PB[b]])
                        if kind == "p":
                            S.op(act, lambda h, b=b, ks=ks, tl=tl: h.activation(out=st[:, ks, tl], in_=ps[:, b, :],
                                                                                func=AF.Copy, scale=0.125),
                                 reads=[PB[b]], writes=[STB[ks]])
                            continue
                        np_ = 64 if kind == "ck" else 128
                        kq = (ci * NTT + tt) % 2
                        b2 = 2 + (ci * NTT + tt) % 2
                        S.op(act, lambda h, b=b, kq=kq, np_=np_: h.activation(out=sq[0:np_, kq, :], in_=ps[0:np_, b, :],
                                                                               func=AF.Square),
                             reads=[PB[b]], writes=[SQB[kq]])
                        mm(ps[0:np_, b2, :], bones[0:np_, 0:np_], sq[0:np_, kq, :], True, True, [SQB[kq], CB], [PB[b2]])
                        rsq(rstd[0:np_, :], ps[0:np_, b2, :], 64.0 * EPS, [PB[b2]], RB)
                        S.op(dve, lambda h, b=b, ks=ks, tl=tl, np_=np_, gcol=gcol: h.scalar_tensor_tensor(
                            out=st[0:np_, ks, tl], in0=ps[0:np_, b, :], scalar=qkgs[0:np_, gcol:gcol + 1],
                            in1=rstd[0:np_, :], op0=ALU.mult, op1=ALU.mult), reads=[PB[b], RB, GB], writes=[STB[ks]])
                        if kind == "ck":
                            S.op(act, lambda h, b=b, ks=ks, tl=tl: h.activation(
                                out=st[64:128, ks, tl], in_=ps[64:128, b, :], func=AF.Copy), reads=[PB[b]],
                                writes=[STB[ks]])
                    S.dma(sp, dst[dchunk * 128:(dchunk + 1) * 128, :], st[:, ks, :], reads=[STB[ks]])
                for t128 in range(NT // 128):
                    tl = slice(t128 * 128, t128 * 128 + 128)
                    kv = t128 % 2
                    groups = [(0, 512), (512, 768), (768, 1280), (1280, 1352)]
                    for gi, (a0, a1) in enumerate(groups):
                        b = 4 + (t128 * 4 + gi) % 4
                        n = a1 - a0
                        for c in range(8):
                            mm(ps[:, b, 0:n], hT[:, c, tl], wv[:, c, a0:a1], c == 0, c == 7, [HB[t128 // 4], WVB], [PB[b]])
                        if gi < 3:
                            eng = act if gi % 2 == 0 else dve
                            if eng is act:
                                S.op(act, lambda h, b=b, n=n, a0=a0, a1=a1, kv=kv: h.activation(
                                    out=vst[:, kv, a0:a1], in_=ps[:, b, 0:n], func=AF.Copy), reads=[PB[b]], writes=[VSB[kv]])
                            else:
                                S.op(dve, lambda h, b=b, n=n, a0=a0, a1=a1, kv=kv: h.tensor_copy(
                                    out=vst[:, kv, a0:a1], in_=ps[:, b, 0:n]), reads=[PB[b]], writes=[VSB[kv]])
                        else:
                            S.op(act, lambda h, b=b, kv=kv: h.activation(
                                out=vst[:, kv, 1280:1344], in_=ps[:, b, 0:64], func=AF.Copy), reads=[PB[b]], writes=[VSB[kv]])
                            S.op(dve, lambda h, b=b, t128=t128: h.tensor_scalar(
                                out=widx[:, t128 * 8:t128 * 8 + 8], in0=ps[:, b, 64:72], scalar1=8.0 ** -0.5,
                                scalar2=None, op0=ALU.mult), reads=[PB[b]], writes=[WIB])
                    S.dma(sp, V_l[tl, :], vst[:, kv, :], reads=[VSB[kv]])
                if not fused:
                    S.dma(sp, widx_o, widx[:], reads=[WIB])
                S.barrier()

        def emit_vec():
            with ExitStack() as fs:
                def fsb(name, shape, dt):
                    uid[0] += 1
                    return fs.enter_context(nc.sbuf_tensor(f"{name}_{uid[0]}", list(shape), dt))
                tbl = fsb("v_tbl", [33, 20], F32)
                oh = fsb("v_oh", [33, 2, 512], F32)
                vs = fsb("v_vs", [20, 2, 512], BF16)
                TB = Buf()
                OB = [Buf(), Buf()]
                VB = [Buf(), Buf()]
                S.op(dve, lambda h: h.memset(tbl[:], -BIG), writes=[TB])
                S.dma(sp, tbl[0:32, :], rel_bias, writes=[TB])
                n = 0
                for kp in range(NKIND * 2):
                    for c0 in range(0, LV, 512):
                        cw = min(512, LV - c0)
                        k = n % 2
                        n += 1
                        S.dma(sp, oh[:, k, 0:cw], onehot[kp, :, c0:c0 + cw], writes=[OB[k]])
                        b = k
                        mm(ps[0:20, b, 0:cw], tbl[:], oh[:, k, 0:cw], True, True, [TB, OB[k]], [PB[b]])
                        S.op(act, lambda h, b=b, k=k, cw=cw: h.activation(out=vs[:, k, 0:cw], in_=ps[0:20, b, 0:cw],
                                                                          func=AF.Copy), reads=[PB[b]], writes=[VB[k]])
                        S.dma(sp, vec_s.rearrange("(k h) n -> k h n", h=20)[kp, :, c0:c0 + cw], vs[:, k, 0:cw],
                              reads=[VB[k]])
                S.barrier()

        def strip_src(kind, par, head, width):
            row = (kind * 2 + par) * 20 + head
            return bass.AP(tensor=vec_s.tensor, offset=row * LV, ap=[[1, 128], [1, width]])

        def kcol(kt):
            gs = kt // 4
            r, li = gpos(gs)
            return r * NT + li * 512 + (kt % 4) * 128

        def vtile(kt):
            gs = kt // 4
            r, li = gpos(gs)
            return li * 8 + r * 4 + kt % 4

        def emit_att(l, oT_a, oT_b, OAB, OBB):
            with ExitStack() as fs:
                def fsb(name, shape, dt):
                    uid[0] += 1
                    return fs.enter_context(nc.sbuf_tensor(f"{name}_{uid[0]}", list(shape), dt))
                KT = fsb("a_KT", [64, 2, 2 * NT], BF16)
                QTt = fsb("a_QT", [64, 2, NT], BF16)
                Vt = fsb("a_V", [128, 2 * NT // 128, 128], BF16)
                G = fsb("a_G", [128, 2, WD], BF16)
                Pt = fsb("a_P", [128, 3, 512], BF16)
                ev = fsb("a_ev", [128, 4, 512], F32)
                sqb = fsb("a_sq", [128, 512], BF16)
                KB = [Buf(), Buf()]
                QB = [Buf(), Buf()]
                VB = Buf()
                GBf = Buf()
                PtB = [Buf() for _ in range(3)]
                EB = [Buf() for _ in range(4)]
                SQ = Buf()
                KTf = KT_f.rearrange("(g r c p) n -> g c p r n", g=4, r=2, c=3, p=128)
                QTi = QT_si.rearrange("(c p) n -> c p n", p=128)
                Vf = V_f.rearrange("(t p) c -> p t c", p=128)
                npt = [0]

                def attend(m, terms, nmap, dv):
                    first = {0: True, 1: True}
                    total = {0: 0, 1: 0}
                    for (mp, ksl, qsl, kts, c0f) in terms:
                        total[mp] += len(kts)
                    cnt = {0: 0, 1: 0}
                    for (mp, ksl, qsl, kts, c0f) in terms:
                        for kt in kts:
                            k = npt[0] % 3
                            npt[0] += 1
                            b = k
                            kc = kcol(kt)
                            mm(ps[:, b, :], KT[:, ksl, kc:kc + 128], QTt[:, qsl, m * 512:(m + 1) * 512], True, False,
                               [KB[ksl], QB[qsl]], [PB[b]])
                            c0 = c0f(kt)
                            mm(ps[:, b, :], antiI, G[:, m % 2, c0:c0 + 512], False, True, [GBf, CB], [PB[b]])
                            S.op(act, lambda h, b=b, k=k: h.activation(out=Pt[:, k, :], in_=ps[:, b, :], func=AF.Exp),
                                 reads=[PB[b]], writes=[PtB[k]])
                            cnt[mp] += 1
                            st_, sp_ = cnt[mp] == 1, cnt[mp] == total[mp]
                            mm(ps[0:dv, 3 + mp, :], Vt[:, vtile(kt), 0:dv], Pt[:, k, :], st_, sp_, [VB, PtB[k]], [PB[3 + mp]])
                            mm(ps[0:dv, 5 + mp, :], ones[:, 0:dv], Pt[:, k, :], st_, sp_, [CB, PtB[k]], [PB[5 + mp]])

                for s in range(4):
                    for m in range(NM):
                        gsN = 2 * m + 1
                        terms = []
                        first = True
                        ngrp = 0
                        kt_lists = []
                        for g, (w, d) in enumerate(DIL):
                            kts = [kt for kt in range((gsN + 1) * 4)
                                   if -384 <= gsN * 512 - 128 * kt <= w + 639]
                            kt_lists.append(kts)
                        tot = sum(len(k) for k in kt_lists)
                        cnt = 0
                        for g, (w, d) in enumerate(DIL):
                            head = 4 * g + s
                            ch, hf_ = head // 2, head % 2
                            sl = (s * 3 * NM + m * 3 + g) % 2
                            S.dma(sp, KT[:, sl, :].rearrange("p (r n) -> p r n", r=2), KTf[ch // 3, ch % 3, hf_ * 64:hf_ * 64 + 64],
                                  writes=[KB[sl]])
                            S.dma(sp, QTt[:, sl, :], QTi[ch, hf_ * 64:hf_ * 64 + 64, :], writes=[QB[sl]])
                            if m == 0 or True:
                                S.dma(sp, Vt[:, :, 0:64], Vf[:, :, head * 64:head * 64 + 64], writes=[VB])
                            wdt = min(WD, w + 639 + 384 + 512 + 128)
                            S.dma(sp, G[:, m % 2, 0:wdt], strip_src(g, m % 2, head, wdt), writes=[GBf])
                            pend = None
                            for kt in kt_lists[g]:
                                k = npt[0] % 3
                                npt[0] += 1
                                b = k
                                kc = kcol(kt)
                                mm(ps[:, b, :], KT[:, sl, kc:kc + 128], QTt[:, sl, m * 512:(m + 1) * 512], True, False,
                                   [KB[sl], QB[sl]], [PB[b]])
                                c0 = gsN * 512 - 128 * kt + 384
                                mm(ps[:, b, :], antiI, G[:, m % 2, c0:c0 + 512], False, True, [GBf, CB], [PB[b]])
                                S.op(act, lambda h, b=b, k=k: h.activation(out=Pt[:, k, :], in_=ps[:, b, :], func=AF.Exp),
                                     reads=[PB[b]], writes=[PtB[k]])
                                if pend is not None:
                                    pend()
                                cnt += 1

                                def pend(kt=kt, k=k, st_=(cnt == 1), sp_=(cnt == tot)):
                                    mm(ps[0:64, 3, :], Vt[:, vtile(kt), 0:64], Pt[:, k, :], st_, sp_, [VB, PtB[k]], [PB[3]])
                                    mm(ps[0:64, 5, :], ones[:, 0:64], Pt[:, k, :], st_, sp_, [CB, PtB[k]], [PB[5]])
                            if pend is not None:
                                pend()
                        S.op(dve, lambda h: h.reciprocal(out=ev[0:64, 0, :], in_=ps[0:64, 5, :]), reads=[PB[5]], writes=[EB[0]])
                        S.op(dve, lambda h, s=s, m=m: h.tensor_tensor(out=oT_a[:, s, m * 512:(m + 1) * 512],
                                                                       in0=ps[0:64, 3, :], in1=ev[0:64, 0, :], op=ALU.mult),
                             reads=[PB[3], EB[0]], writes=[OAB])
                for hb in range(4):
                    ch, hf_ = hb // 2, hb % 2
                    for mp in range(2):
                        S.dma(sp, KT[:, mp, :].rearrange("p (r n) -> p r n", r=2),
                              KTf[(6 + 2 * mp + ch) // 3, (6 + 2 * mp + ch) % 3, hf_ * 64:hf_ * 64 + 64], writes=[KB[mp]])
                        S.dma(sp, QTt[:, mp, :], QTi[6 + 2 * mp + ch, hf_ * 64:hf_ * 64 + 64, :], writes=[QB[mp]])
                    S.dma(sp, Vt[:], Vf[:, :, 768 + hb * 128:768 + hb * 128 + 128], writes=[VB])
                    for par in range(2):
                        S.dma(sp, G[:, par, :], strip_src(3, par, 12 + hb, WD), writes=[GBf])
                    for m in range(NM):
                        gsN = 2 * m + 1
                        nkt = (gsN + 1) * 4
                        pend = None
                        for kt in range(nkt):
                            D0 = gsN * 512 - 128 * kt
                            c0 = D0 + 384 if D0 < 2176 else 2560
                            kc = kcol(kt)
                            for mp in range(2):
                                k = npt[0] % 3
                                npt[0] += 1
                                b = k
                                mm(ps[:, b, :], KT[:, mp, kc:kc + 128], QTt[:, mp, m * 512:(m + 1) * 512], True, False,
                                   [KB[mp], QB[mp]], [PB[b]])
                                mm(ps[:, b, :], antiI, G[:, m % 2, c0:c0 + 512], False, True, [GBf, CB], [PB[b]])
                                S.op(act, lambda h, b=b, k=k: h.activation(out=Pt[:, k, :], in_=ps[:, b, :], func=AF.Exp),
                                     reads=[PB[b]], writes=[PtB[k]])
                                if pend is not None:
                                    pend()

                                def pend(kt=kt, k=k, mp=mp, nkt=nkt):
                                    mm(ps[:, 3 + mp, :], Vt[:, vtile(kt), :], Pt[:, k, :], kt == 0, kt == nkt - 1,
                                       [VB, PtB[k]], [PB[3 + mp]])
                                    mm(ps[:, 5 + mp, :], ones, Pt[:, k, :], kt == 0, kt == nkt - 1, [CB, PtB[k]], [PB[5 + mp]])
                        if pend is not None:
                            pend()
                        for mp in range(2):
                            S.op(dve, lambda h, mp=mp: h.reciprocal(out=ev[:, mp, :], in_=ps[:, 5 + mp, :]),
                                 reads=[PB[5 + mp]], writes=[EB[mp]])
                            S.op(dve, lambda h, mp=mp: h.tensor_tensor(out=ev[:, mp, :], in0=ps[:, 3 + mp, :],
                                                                       in1=ev[:, mp, :], op=ALU.mult),
                                 reads=[PB[3 + mp], EB[mp]], writes=[EB[mp]])
                        S.op(dve, lambda h: h.scalar_tensor_tensor(out=ev[:, 2, :], in0=ev[:, 1, :],
                                                                   scalar=neglam[:, l:l + 1], in1=ev[:, 0, :],
                                                                   op0=ALU.mult, op1=ALU.add),
                             reads=[EB[0], EB[1], GB], writes=[EB[2]])
                        S.op(act, lambda h: h.activation(out=sqb[:], in_=ev[:, 2, :], func=AF.Square), reads=[EB[2]],
                             writes=[SQ])
                        mm(ps[:, 7, :], ones, sqb[:], True, True, [SQ, CB], [PB[7]])
                        rsq(ev[:, 3, :], ps[:, 7, :], 128.0 * EPS, [PB[7]], EB[3])
                        S.op(dve, lambda h, hb=hb, m=m: h.scalar_tensor_tensor(
                            out=oT_b[:, hb, m * 512:(m + 1) * 512], in0=ev[:, 2, :], scalar=dnrm[:, l:l + 1],
                            in1=ev[:, 3, :], op0=ALU.mult, op1=ALU.mult), reads=[EB[2], EB[3], GB], writes=[OBB])
                S.barrier()

        WC = 3072

        def emit_dsa(l, oT_c, OCB):
            with ExitStack() as fs:
                def fsb(name, shape, dt):
                    uid[0] += 1
                    return fs.enter_context(nc.sbuf_tensor(f"{name}_{uid[0]}", list(shape), dt))
                KK = fsb("c_KK", [128, 2 * NT], BF16)
                QQ = fsb("c_QQ", [128, 8, 512], BF16)
                Vc = fsb("c_V", [128, 2 * NT // 128, 64], BF16)
                G = fsb("c_G", [128, 4, WC], BF16)
                cmb = fsb("c_cmb", [128, 2, 1024], BF16)
                sc = fsb("c_sc", [128, 2, T], F32)
                rlb = fsb("c_rl", [128, 3, 512], BF16)
                dg = fsb("c_dg", [128, 2, 8, 128], BF16)
                mx = fsb("c_mx", [128, 8], F32)
                mneg = fsb("c_mneg", [128, T], BF16)
                Pt = fsb("c_P", [128, 2, 512], BF16)
                ev = fsb("c_ev", [64, 512], F32)
                B_ld = Buf()
                QLB = Buf()
                GLB = Buf()
                SCB = [Buf(), Buf()]
                RLB = [Buf(), Buf(), Buf()]
                DGB = [Buf(), Buf()]
                MXB = Buf()
                MNB = Buf()
                PtB = [Buf(), Buf()]
                EVB = Buf()
                KTf = KT_f.rearrange("(g r c p) n -> g c p r n", g=4, r=2, c=3, p=128)
                QTi = QT_si.rearrange("(c p) n -> c p n", p=128)
                Vf = V_f.rearrange("(t p) c -> p t c", p=128)
                S.dma(sp, KK[:, :].rearrange("p (r n) -> p r n", r=2), KTf[3, 1], writes=[B_ld])
                S.dma(sp, Vc[:], Vf[:, :, 1280:1344], writes=[B_ld])
                S.dma(pool, cmb[:], cm_in.rearrange("k p n -> p k n"), writes=[B_ld])
                nq = 0
                nr = 0
                nacc = 0
                for m in range(NM):
                    gsN = 2 * m + 1
                    nkeys = (gsN + 1) * 512
                    nch = nkeys // 512
                    ms = slice(m * 512, m * 512 + 512)
                    for i in range(8):
                        S.dma(sp, QQ[64:128, i, :], QTi[12 + i // 2, (i % 2) * 64:(i % 2) * 64 + 64, ms], writes=[QLB])
                    for i in range(4):
                        S.dma(sp, QQ[0:64, i, :], QTi[10 + i // 2, (i % 2) * 64:(i % 2) * 64 + 64, ms], writes=[QLB])
                        S.dma(sp, G[:, i, :], strip_src(3, m % 2, 16 + i, WC), writes=[GLB])
                    for qb in range(4):
                        ql = slice(qb * 128, qb * 128 + 128)
                        q0 = m * 512 + qb * 128
                        t128 = q0 // 128
                        ks = nq % 2
                        nq += 1
                        for hh in range(8):
                            wcol = widx[:, t128 * 8 + hh:t128 * 8 + hh + 1]
                            S.op(pool, lambda h, ks=ks, hh=hh, wcol=wcol: h.tensor_scalar(
                                out=dg[:, ks, hh, :], in0=ident, scalar1=wcol, scalar2=None, op0=ALU.mult),
                                reads=[CB, WIB], writes=[DGB[ks]])
                        for kc in range(nch):
                            r, li = gpos(kc)
                            col = r * NT + li * 512
                            ab = 6 + nacc % 2
                            nacc += 1
                            pend = None
                            for hh in range(8):
                                b = nr % 2
                                k = nr % 3
                                nr += 1
                                mm(ps[:, b, :], QQ[64:128, hh, ql], KK[64:128, col:col + 512],
                                   True, True, [B_ld, QLB], [PB[b]])
                                S.op(act, lambda h, b=b, k=k: h.activation(out=rlb[:, k, :], in_=ps[:, b, :], func=AF.Relu),
                                     reads=[PB[b]], writes=[RLB[k]])
                                if pend is not None:
                                    pend()

                                def pend(hh=hh, k=k, ab=ab, ks=ks):
                                    mm(ps[:, ab, :], dg[:, ks, hh, :], rlb[:, k, :], hh == 0, hh == 7, [DGB[ks], RLB[k]], [PB[ab]])
                            pend()
                            S.op(act, lambda h, ab=ab, ks=ks, kc=kc: h.activation(
                                out=sc[:, ks, kc * 512:(kc + 1) * 512], in_=ps[:, ab, :], func=AF.Copy),
                                reads=[PB[ab]], writes=[SCB[ks]])
                        lo = gsN * 512 + 128 * qb - 512
                        nke = gsN * 512 + 128 * (qb + 1)
                        hi = nke
                        S.op(dve, lambda h, ks=ks, lo=lo, hi=hi, m=m: h.tensor_tensor(
                            out=sc[:, ks, lo:hi], in0=sc[:, ks, lo:hi], in1=cmb[:, m % 2, 0:hi - lo], op=ALU.add),
                            reads=[SCB[ks], B_ld], writes=[SCB[ks]])
                        for it in range(32):
                            S.op(dve, lambda h, ks=ks: h.max(out=mx[:], in_=sc[:, ks, 0:nke]), reads=[SCB[ks]], writes=[MXB])
                            S.op(dve, lambda h, ks=ks: h.match_replace(out=sc[:, ks, 0:nke], in_to_replace=mx[:],
                                                                       in_values=sc[:, ks, 0:nke], imm_value=-3.0e38),
                                 reads=[SCB[ks], MXB], writes=[SCB[ks]])
                        S.op(dve, lambda h, ks=ks: h.tensor_scalar(out=mneg[:, 0:nke], in0=sc[:, ks, 0:nke],
                                                                   scalar1=-1.0e38, scalar2=-BIG, op0=ALU.is_gt,
                                                                   op1=ALU.mult), reads=[SCB[ks]], writes=[MNB])
                        S.op(dve, lambda h, lo=lo, hi=hi, m=m: h.tensor_tensor(
                            out=mneg[:, lo:hi], in0=mneg[:, lo:hi], in1=cmb[:, m % 2, 0:hi - lo], op=ALU.add),
                            reads=[MNB, B_ld], writes=[MNB])
                        nkt = nke // 128
                        pendc = None
                        for kt in range(nkt):
                            kcg = kcol(kt)
                            D0 = gsN * 512 - 128 * kt
                            c0 = (D0 + 384 if D0 < 2176 else 2560) + 128 * qb
                            b = 2 + kt % 2
                            k = kt % 2
                            for hc in range(4):
                                o = ps[:, b, hc * 128:(hc + 1) * 128]
                                mm(o, KK[0:64, kcg:kcg + 128], QQ[0:64, hc, ql], True, False, [B_ld, QLB], [PB[b]])
                                mm(o, antiI, G[:, hc, c0:c0 + 128], False, False, [GLB, CB], [PB[b]])
                                mm(o, mneg[:, kt * 128:(kt + 1) * 128], ident, False, True, [MNB, CB], [PB[b]])
                            S.op(act, lambda h, b=b, k=k: h.activation(out=Pt[:, k, :], in_=ps[:, b, :], func=AF.Exp),
                                 reads=[PB[b]], writes=[PtB[k]])
                            if pendc is not None:
                                pendc()

                            def pendc(kt=kt, k=k, nkt=nkt):
                                for hc in range(4):
                                    mm(ps[0:64, 4, hc * 128:(hc + 1) * 128], Vc[:, vtile(kt), :],
                                       Pt[:, k, hc * 128:(hc + 1) * 128], kt == 0 and hc == 0, kt == nkt - 1,
                                       [B_ld, PtB[k]], [PB[4]])
                                mm(ps[0:64, 5, :], ones[:, 0:64], Pt[:, k, :], kt == 0, kt == nkt - 1, [CB, PtB[k]], [PB[5]])
                        pendc()
                        S.op(dve, lambda h: h.reciprocal(out=ev[:], in_=ps[0:64, 5, :]), reads=[PB[5]], writes=[EVB])
                        for hc in range(4):
                            S.op(dve, lambda h, hc=hc, q0=q0: h.tensor_tensor(
                                out=oT_c[:, hc, q0:q0 + 128], in0=ps[0:64, 4, hc * 128:(hc + 1) * 128],
                                in1=ev[:, hc * 128:(hc + 1) * 128], op=ALU.mult), reads=[PB[4], EVB], writes=[OCB])
                S.barrier()

        def emit_post(l, oT_a, oT_b, oT_c, OAB, OBB, OCB):
            with ExitStack() as fs:
                def fsb(name, shape, dt):
                    uid[0] += 1
                    return fs.enter_context(nc.sbuf_tensor(f"{name}_{uid[0]}", list(shape), dt))
                hT = fsb("o_hT", [128, 1, 8, 512], BF16)
                yT = fsb("o_yT", [128, 8, 512], BF16)
                wga = fsb("o_wg", [128, 3, 8, 128], BF16)
                wa = fsb("o_wa", [64, 4, D], BF16)
                wb = fsb("o_wb", [128, 4, D], BF16)
                wc = fsb("o_wc", [64, 4, D], BF16)
                wo = fsb("o_wo", [128, 8, D], BF16)
                sg = fsb("o_sg", [128, 2, 512], F32)
                tmp = fsb("o_tmp", [128, 2, 512], F32)
                yacc = fsb("o_yacc", [128, 512], F32)
                HB = [Buf(), Buf()]
                YB = Buf()
                WGB = [Buf() for _ in range(3)]
                WB = Buf()
                SGB = [Buf(), Buf()]
                TMB = [Buf(), Buf()]
                YAB = Buf()
                winv = w_in[l].rearrange("(c p) n -> p c n", p=128)
                S.dma(pool, wa[:], w_br[0][l].rearrange("(h p) n -> p h n", p=64), writes=[WB])
                S.dma(pool, wb[:], w_br[1][l].rearrange("(h p) n -> p h n", p=128), writes=[WB])
                S.dma(pool, wc[:], w_br[2][l].rearrange("(h p) n -> p h n", p=64), writes=[WB])
                S.dma(pool, wo[:], w_out[l].rearrange("(c p) n -> p c n", p=128), writes=[WB])
                nw = 0
                ns = 0
                for tt in range(NTT):
                    tok = slice(tt * 512, tt * 512 + 512)
                    kh = 0
                    S.dma(sp, hT[:, kh], hT_si.rearrange("(c p) n -> p c n", p=128)[:, :, tok], writes=[HB[kh]])
                    for oc in range(8):
                        for i in range(3):
                            k3 = nw % 3
                            nw += 1
                            g0 = 4808 + i * 1024 + oc * 128
                            S.dma(pool, wga[:, k3], winv[:, :, g0:g0 + 128], writes=[WGB[k3]])
                            bg = (oc * 3 + i) % 2
                            bb = 2 + (oc * 3 + i) % 2
                            for c in range(8):
                                mm(ps[:, bg, :], wga[:, k3, c, :], hT[:, kh, c, :], c == 0, c == 7, [WGB[k3], HB[kh]], [PB[bg]])
                            ocs = slice(oc * 128, oc * 128 + 128)
                            if i == 0:
                                for hh in range(4):
                                    mm(ps[:, bb, :], wa[:, hh, ocs], oT_a[:, hh, tok], hh == 0, hh == 3, [WB, OAB], [PB[bb]])
                            elif i == 1:
                                for hh in range(4):
                                    mm(ps[:, bb, :], wb[:, hh, ocs], oT_b[:, hh, tok], hh == 0, hh == 3, [WB, OBB], [PB[bb]])
                            else:
                                for hh in range(4):
                                    mm(ps[:, bb, :], wc[:, hh, ocs], oT_c[:, hh, tok], hh == 0, hh == 3, [WB, OCB], [PB[bb]])
                            k = ns % 2
                            ns += 1
                            S.op(act, lambda h, k=k, bg=bg: h.activation(out=sg[:, k, :], in_=ps[:, bg, :], func=AF.Sigmoid),
                                 reads=[PB[bg]], writes=[SGB[k]])
                            if i == 0:
                                S.op(dve, lambda h, k=k, bb=bb: h.tensor_tensor(out=yacc[:], in0=sg[:, k, :], in1=ps[:, bb, :],
                                                                               op=ALU.mult), reads=[SGB[k], PB[bb]], writes=[YAB])
                            else:
                                S.op(dve, lambda h, k=k, bb=bb: h.tensor_tensor(out=tmp[:, k, :], in0=sg[:, k, :],
                                                                               in1=ps[:, bb, :], op=ALU.mult),
                                     reads=[SGB[k], PB[bb]], writes=[TMB[k]])
                                if i == 1:
                                    S.op(pool, lambda h, k=k: h.tensor_tensor(out=yacc[:], in0=yacc[:], in1=tmp[:, k, :],
                                                                             op=ALU.add), reads=[YAB, TMB[k]], writes=[YAB])
                                else:
                                    S.op(pool, lambda h, k=k, oc=oc: h.tensor_tensor(out=yT[:, oc, :], in0=yacc[:],
                                                                                    in1=tmp[:, k, :], op=ALU.add),
                                         reads=[YAB, TMB[k]], writes=[YB])
                    for oc in range(8):
                        bo = 4 + oc % 2
                        for c in range(8):
                            mm(ps[:, bo, :], wo[:, c, oc * 128:(oc + 1) * 128], yT[:, c, :], c == 0, c == 7, [WB, YB], [PB[bo]])
                        S.op(dve, lambda h, bo=bo, oc=oc, tok=tok: h.tensor_tensor(out=xT[:, oc, tok], in0=ps[:, bo, :],
                                                                                  in1=xT[:, oc, tok], op=ALU.add),
                             reads=[PB[bo], XB[tt]], writes=[XB[tt]])
                S.barrier()

        S.barrier()
        if has_b:
            emit_vec()
        for (ph, l) in parts:
            if fused:
                KT_l, V_l, KT_f, V_f = KT_l2[l % 2], V_l2[l % 2], KT_f2[l % 2], V_f2[l % 2]
            if ph == "a":
                emit_ffn(l, 0)
                emit_pre(l)
                if fused:
                    S.drain(pool)
                    groups = [[2 * i, 2 * i + 1] for i in range(ncores // 2)]
                    pieces = [(KT_l[g * 384:(g + 1) * 384, :], KT_f[g * 768:(g + 1) * 768, :]) for g in range(4)]
                    pieces += [(V_l[g * 512:(g + 1) * 512, :], V_f[g * 1024:(g + 1) * 1024, :]) for g in range(NM)]
                    for (src_, dst_) in pieces:
                        ins = nc.gpsimd.collective_compute("AllGather", ALU.bypass, replica_groups=groups,
                                                           ins=[src_.opt()], outs=[dst_.opt()])
                        S.cc_val += 1
                        ins.then_inc(S.cc_sem)
                    S.barrier()
            elif ph == "b":
                if not fused and (ph, l) == parts[0]:
                    S.dma(sp, widx[:], widx_i, writes=[WIB])
                with ExitStack() as bs:
                    oT_c = bs.enter_context(nc.sbuf_tensor(f"oT_c{l}", [64, 4, NT], BF16))
                    OAB, OBB, OCB = Buf(), Buf(), Buf()
                    if "c" in MIX:
                        emit_dsa(l, oT_c, OCB)
                    oT_a = bs.enter_context(nc.sbuf_tensor(f"oT_a{l}", [64, 4, NT], BF16))
                    oT_b = bs.enter_context(nc.sbuf_tensor(f"oT_b{l}", [128, 4, NT], BF16))
                    emit_att(l, oT_a, oT_b, OAB, OBB)
                    emit_post(l, oT_a, oT_b, oT_c, OAB, OBB, OCB)
                emit_ffn(l, 1)
        for tt in range(NTT):
            S.dma(sp, xT_out.rearrange("(c p) n -> p c n", p=128)[:, :, tt * 512:(tt + 1) * 512],
                  xT[:, :, tt * 512:(tt + 1) * 512], reads=[XB[tt]])
        S.barrier()
    nc._n_ins = S.n_ins
    return nc


def rel_bucket_np(dist):
    n = np.maximum(dist, 0)
    nf = np.maximum(n, 1).astype(np.float32)
    large = 16 + (np.log(nf / np.float32(16)) / np.float32(math.log(2048 / 16)) * np.float32(16)).astype(np.int32)
    large = np.minimum(large, 31)
    return np.where(n < 16, n, large)


def make_onehot(e_par):
    oh = np.zeros((NKIND * 2, 33, LV), np.float32)
    v = np.arange(LV)
    for kind in range(NKIND):
        for p in range(2):
            dist = v - VOFF - 512 * e_par[p]
            if kind < 3:
                w, d = DIL[kind]
                valid = (dist >= 0) & (dist <= w) & (dist % d == 0)
            else:
                valid = dist >= 0
            bk = rel_bucket_np(dist)
            o = oh[kind * 2 + p]
            o[bk[valid], v[valid]] = 1.0
            o[32, v[~valid]] = 1.0
    return oh


def make_cm(e_par):
    cm = np.zeros((2, 128, 1024), np.float32)
    u = np.arange(1024)[None, :]
    qi = np.arange(128)[:, None]
    for p in range(2):
        cm[p] = np.where(u - 512 + 512 * e_par[p] <= qi, 0.0, -BIG)
    return cm


def local_superblocks(hf, ns):
    return [gs for gs in range(ns) if (gs % 4 in (0, 3)) == (hf == 0)]


_PROG = {}


def _get_prog(T, L, parts, fused, lbase=0, ncores=8):
    key = (T, L, tuple(parts), fused, lbase, ncores)
    if key not in _PROG:
        _PROG[key] = build(T, L, list(parts), fused, lbase, ncores)
    return _PROG[key]


A_KEYS = ("ffn1_w_gate", "ffn1_w_up", "ffn1_w_down", "w_in")
B_KEYS = ("ffn2_w_gate", "ffn2_w_up", "ffn2_w_down", "w_in", "w_branch_a", "w_branch_b", "w_branch_c", "w_out")


def run_model(inputs, T, L, B, fused=True):
    x = np.asarray(inputs["x"], np.float32)
    NT = T // 2
    NS = T // 512
    ncores = 2 * B
    consts = np.zeros((4, 128, 128), np.float32)
    consts[0] = np.eye(128)
    consts[1] = np.eye(128)[::-1]
    consts[2] = 1.0
    consts[3, :64, :64] = 1.0
    consts[3, 64:, 64:] = 1.0
    norms = np.stack([inputs["ffn1_norm"], inputs["mix_norm"], inputs["ffn2_norm"]], 1)
    normsT = np.ascontiguousarray(norms.reshape(L, 3, 8, 128).transpose(3, 0, 1, 2).reshape(128, L * 24)).astype(np.float32)
    qg = np.asarray(inputs["qk_gain"], np.float32).reshape(L * 6, 64)
    qkg = np.ascontiguousarray(np.concatenate([qg, qg], 1).T)
    dnorm = np.ascontiguousarray(np.asarray(inputs["diff_out_norm"], np.float32).T)
    dlam = np.ascontiguousarray(np.broadcast_to(np.asarray(inputs["diff_lambda"], np.float32).reshape(1, L * 256), (128, L * 256)))
    shared = {"consts": consts, "normsT": normsT, "qkg": qkg, "dnorm": dnorm, "dlam": dlam,
              "rel_bias": np.asarray(inputs["rel_bias"], np.float32)}
    for k in ("ffn1_w_gate", "ffn1_w_up", "ffn1_w_down", "ffn2_w_gate", "ffn2_w_up", "ffn2_w_down", "w_in",
              "w_branch_a", "w_branch_b", "w_branch_c", "w_out"):
        shared[k] = np.asarray(inputs[k], np.float32)
    percore = []
    for c in range(ncores):
        b, hf = c // 2, c % 2
        sbs = local_superblocks(hf, NS)
        xs = np.concatenate([x[b, gs * 512:(gs + 1) * 512] for gs in sbs], 0)
        e_par = [1, 0] if hf == 0 else [0, 1]
        percore.append({"xT_in": np.ascontiguousarray(xs.T), "onehot": make_onehot(e_par), "cm": make_cm(e_par)})
    cores = list(range(ncores))
    if fused:
        parts = []
        for l in range(L):
            parts += [("a", l), ("b", l)]
        nc = _get_prog(T, L, parts, True, 0, ncores)
        res = run_bass_kernel_spmd(nc, [dict(shared, **pc) for pc in percore], core_ids=cores)
        outs = [r["xT_out"] for r in res.results]
    else:
        state = [pc["xT_in"] for pc in percore]
        small = {k: shared[k] for k in ("consts",)}
        for l in range(L):
            sm = dict(small)
            sm["normsT"] = np.ascontiguousarray(normsT[:, l * 24:(l + 1) * 24])
            sm["qkg"] = np.ascontiguousarray(qkg[:, l * 6:(l + 1) * 6])
            sm["dnorm"] = np.ascontiguousarray(dnorm[:, l:l + 1])
            sm["dlam"] = np.ascontiguousarray(dlam[:, l * 256:(l + 1) * 256])
            sa = dict(sm)
            for k in A_KEYS:
                sa[k] = shared[k][l:l + 1]
            nca = _get_prog(T, 1, [("a", 0)], False, 0)
            res = run_bass_kernel_spmd(nca, [dict(sa, xT_in=state[i]) for i in range(ncores)], core_ids=cores)
            ra = res.results
            sbm = dict(sm)
            for k in B_KEYS:
                sbm[k] = shared[k][l:l + 1]
            sbm["rel_bias"] = shared["rel_bias"]
            ncb = _get_prog(T, 1, [("b", 0)], False, l)
            maps = []
            for i, pc in enumerate(percore):
                p0 = (i // 2) * 2
                m = dict(sbm, onehot=pc["onehot"], cm=pc["cm"])
                m["xT_in"] = ra[i]["xT_out"]
                m["hT_i"] = ra[i]["hT_o"]
                m["QT_i"] = ra[i]["QT_o"]
                m["widx_i"] = ra[i]["widx_o"]
                k0, k1 = np.asarray(ra[p0]["KT_o"]), np.asarray(ra[p0 + 1]["KT_o"])
                m["KT_f"] = np.concatenate([np.concatenate([k0[g * 384:(g + 1) * 384], k1[g * 384:(g + 1) * 384]], 0)
                                            for g in range(4)], 0)
                v0, v1 = np.asarray(ra[p0]["V_o"]), np.asarray(ra[p0 + 1]["V_o"])
                m["V_f"] = np.concatenate([np.concatenate([v0[g * 512:(g + 1) * 512], v1[g * 512:(g + 1) * 512]], 0)
                                           for g in range(NT // 512)], 0)
                maps.append(m)
            res = run_bass_kernel_spmd(ncb, maps, core_ids=cores)
            state = [r["xT_out"] for r in res.results]
        outs = state
    out = np.zeros((B, T, D), np.float32)
    for c in range(ncores):
        b, hf = c // 2, c % 2
        sbs = local_superblocks(hf, NS)
        xo = np.asarray(outs[c]).T
        for i, gs in enumerate(sbs):
            out[b, gs * 512:(gs + 1) * 512] = xo[i * 512:(i + 1) * 512]
    return out


FUSED = True


def kernel(**inputs):
    x = np.asarray(inputs["x"])
    B, T, _ = x.shape
    L = np.asarray(inputs["w_in"]).shape[0]
    return run_model(inputs, T, L, B, fused=FUSED)
```

```python
import math
from contextlib import ExitStack
import numpy as np
import concourse.bass as bass
import concourse.mybir as mybir
from concourse.bass_utils import run_bass_kernel_spmd

F32 = mybir.dt.float32
BF16 = mybir.dt.bfloat16
AF = mybir.ActivationFunctionType
ALU = mybir.AluOpType
AX = mybir.AxisListType

D = 1024
DFF = 2816
NF = DFF // 128
NIN = 7880
EPS = 1e-6
BIG = 30000.0
LV = 3840
WD = 3712
VOFF = 511
NKIND = 4
DIL = ((128, 1), (512, 4), (2048, 16))
R_DMA = 8
MIX = "abc"


class Buf:
    __slots__ = ("w", "r")

    def __init__(self):
        self.w = None
        self.r = {}


class Eng:
    def __init__(self, name, h, sem, same):
        self.name = name
        self.h = h
        self.sem = sem
        self.cnt = 0
        self.seen = {}
        self.same = same


class Sched:
    def __init__(self, nc, es):
        self.nc = nc
        mk = lambda n: es.enter_context(nc.semaphore(n))
        self.pe = Eng("pe", nc.tensor, mk("s_pe"), False)
        self.act = Eng("act", nc.scalar, mk("s_act"), True)
        self.dve = Eng("dve", nc.vector, mk("s_dve"), True)
        self.pool = Eng("pool", nc.gpsimd, mk("s_pool"), True)
        self.sp = Eng("sp", nc.sync, mk("s_sp"), False)
        self.engs = [self.pe, self.act, self.dve, self.pool, self.sp]
        self.dq = {}
        for e in (self.sp, self.pool):
            self.dq[e.name] = {"sems": [mk(f"d_{e.name}{i}") for i in range(R_DMA)], "vals": [0] * R_DMA, "i": 0}
        self.n_ins = 0
        self.cc_sem = mk("cc_sem")
        self.cc_val = 0

    def _deps(self, reads, writes):
        d = {}

        def add(t):
            if t is None:
                return
            k, s, v = t
            if k not in d or d[k][1] < v:
                d[k] = (s, v)

        for b in reads:
            add(b.w)
        for b in writes:
            add(b.w)
            for t in b.r.values():
                add(t)
        return d

    def _filter(self, eng, deps):
        out = []
        for key, (sem, val) in deps.items():
            if key == eng.name and not eng.same:
                continue
            if eng.seen.get(key, 0) >= val:
                continue
            eng.seen[key] = val
            out.append((key, sem, val))
        out.sort(key=lambda t: 0 if t[0] == eng.name else 1)
        return out

    def _emit(self, eng, fn, waits):
        for (_, sem, val) in waits[1:]:
            eng.h.wait_ge(sem, val)
        ins = fn(eng.h)
        if waits:
            ins._wait_ge(waits[0][1], waits[0][2])
        self.n_ins += 1 + max(0, len(waits) - 1)
        return ins

    def op(self, eng, fn, reads=(), writes=()):
        waits = self._filter(eng, self._deps(reads, writes))
        ins = self._emit(eng, fn, waits)
        ins.then_inc(eng.sem, 1)
        eng.cnt += 1
        tok = (eng.name, eng.sem, eng.cnt)
        for b in reads:
            b.r[eng.name] = tok
        for b in writes:
            b.w = tok
            b.r = {}
        return tok

    def dma(self, eng, out, in_, reads=(), writes=(), **kw):
        q = self.dq[eng.name]
        i = q["i"]
        q["i"] = (i + 1) % R_DMA
        sem = q["sems"][i]
        key = f"d_{eng.name}{i}"
        deps = self._deps(reads, writes)
        if q["vals"][i] > 0:
            deps[key] = (sem, q["vals"][i])
        waits = self._filter(eng, deps)
        ins = self._emit(eng, lambda h: h.dma_start(out=out, in_=in_, **kw), waits)
        q["vals"][i] += 16
        ins.then_inc(sem, 16)
        tok = (key, sem, q["vals"][i])
        for b in reads:
            b.r[key] = tok
        for b in writes:
            b.w = tok
            b.r = {}
        return tok

    def drain(self, eng):
        for e in self.engs:
            if e.cnt > 0 and eng.seen.get(e.name, 0) < e.cnt and e is not eng:
                eng.h.wait_ge(e.sem, e.cnt)
                eng.seen[e.name] = e.cnt
        if self.cc_val > 0 and eng.seen.get("cc", 0) < self.cc_val:
            eng.h.wait_ge(self.cc_sem, self.cc_val)
            eng.seen["cc"] = self.cc_val
        for qn, q in self.dq.items():
            for i in range(R_DMA):
                key = f"d_{qn}{i}"
                if q["vals"][i] > 0 and eng.seen.get(key, 0) < q["vals"][i]:
                    eng.h.wait_ge(q["sems"][i], q["vals"][i])
                    eng.seen[key] = q["vals"][i]

    def barrier(self):
        for e in self.engs:
            self.drain(e)


def gpos(gs):
    r = 0 if gs % 4 in (0, 3) else 1
    li = gs // 2
    return r, li


def build(T, L, parts, fused, lbase=0, ncores=8):
    NT = T // 2
    NS = T // 512
    NM = NS // 2
    NTT = NT // 512
    nc = bass.Bass("TRN2", target_bir_lowering=False)

    def din(name, shape, dt=F32):
        return nc.dram_tensor(name, list(shape), dt, kind="ExternalInput").ap()

    def dout(name, shape, dt=F32):
        return nc.dram_tensor(name, list(shape), dt, kind="ExternalOutput").ap()

    def dscr(name, shape, dt, io):
        if fused:
            return nc.dram_tensor(name, list(shape), dt, kind="Internal").ap()
        return nc.dram_tensor(name, list(shape), dt, kind="ExternalInput" if io == "in" else "ExternalOutput").ap()

    has_a = any(p[0] == "a" for p in parts)
    has_b = any(p[0] == "b" for p in parts)
    first_is_b = parts[0][0] == "b"
    last_is_a = parts[-1][0] == "a"

    xT_in = din("xT_in", [D, NT])
    xT_out = dout("xT_out", [D, NT])
    consts = din("consts", [4, 128, 128])
    normsT = din("normsT", [128, L * 24])
    qkg = din("qkg", [128, L * 6])
    dnorm = din("dnorm", [128, L])
    dlam = din("dlam", [128, L * 256])
    w_gate = [din("ffn1_w_gate", [L, D, DFF]) if has_a else None, din("ffn2_w_gate", [L, D, DFF]) if has_b else None]
    w_up = [din("ffn1_w_up", [L, D, DFF]) if has_a else None, din("ffn2_w_up", [L, D, DFF]) if has_b else None]
    w_down = [din("ffn1_w_down", [L, DFF, D]) if has_a else None, din("ffn2_w_down", [L, DFF, D]) if has_b else None]
    w_in = din("w_in", [L, D, NIN])
    if has_b:
        w_br = [din("w_branch_a", [L, 256, D]), din("w_branch_b", [L, 512, D]), din("w_branch_c", [L, 256, D])]
        w_out = din("w_out", [L, D, D])
        rel_bias = din("rel_bias", [32, 20])
        onehot = din("onehot", [NKIND * 2, 33, LV])
        cm_in = din("cm", [2, 128, 1024])

    NQC = 16
    NKC = 12
    VW = 1344
    if fused:
        hT_s = dscr("hT_s", [8 * 128, NT], BF16, None)
        QT_s = dscr("QT_s", [NQC * 128, NT], BF16, None)
        KT_l2 = [dscr(f"KT_l{i}", [NKC * 128, NT], BF16, None) for i in range(2)]
        V_l2 = [dscr(f"V_l{i}", [NT, VW], BF16, None) for i in range(2)]
        KT_f2 = [dscr(f"KT_f{i}", [2 * NKC * 128, NT], BF16, None) for i in range(2)]
        V_f2 = [dscr(f"V_f{i}", [2 * NT, VW], BF16, None) for i in range(2)]
        KT_l, V_l, KT_f, V_f = KT_l2[0], V_l2[0], KT_f2[0], V_f2[0]
        hT_si = hT_s
        QT_si = QT_s
    else:
        if last_is_a:
            hT_s = dscr("hT_o", [8 * 128, NT], BF16, "out")
            QT_s = dscr("QT_o", [NQC * 128, NT], BF16, "out")
            KT_l = dscr("KT_o", [NKC * 128, NT], BF16, "out")
            V_l = dscr("V_o", [NT, VW], BF16, "out")
            widx_o = dscr("widx_o", [128, NT // 128 * 8], F32, "out")
        if first_is_b:
            hT_si = dscr("hT_i", [8 * 128, NT], BF16, "in")
            QT_si = dscr("QT_i", [NQC * 128, NT], BF16, "in")
            KT_f = dscr("KT_f", [2 * NKC * 128, NT], BF16, "in")
            V_f = dscr("V_f", [2 * NT, VW], BF16, "in")
            widx_i = dscr("widx_i", [128, NT // 128 * 8], F32, "in")
    vec_s = nc.dram_tensor("vec_s", [NKIND * 2 * 20, LV], BF16, kind="Internal").ap()

    es = ExitStack()
    uid = [0]
    with es:
        S = Sched(nc, es)
        pe, act, dve, pool, sp = S.pe, S.act, S.dve, S.pool, S.sp

        def sb(name, shape, dt):
            return es.enter_context(nc.sbuf_tensor(name, list(shape), dt))

        xT = sb("xT", [128, 8, NT], F32)
        XB = [Buf() for _ in range(NTT)]
        ps = es.enter_context(nc.psum_tensor("ps", [128, 8, 512], F32))
        PB = [Buf() for _ in range(8)]
        cst = sb("cst", [128, 4, 128], BF16)
        CB = Buf()
        ident, antiI, ones, bones = cst[:, 0, :], cst[:, 1, :], cst[:, 2, :], cst[:, 3, :]
        g32 = sb("g32", [128, L * 24], F32)
        qkgs = sb("qkgs", [128, L * 6], F32)
        dnrm = sb("dnrm", [128, L], F32)
        lamt = sb("lamt", [128, L * 256], F32)
        neglam = sb("neglam", [128, L], F32)
        widx = sb("widx", [128, NT // 128 * 8], F32)
        WIB = Buf()
        GB = Buf()

        S.dma(pool, cst[:], consts.rearrange("k p n -> p k n"), writes=[CB])
        epsb = sb("epsb", [128, 3], F32)
        ci = {1024.0 * EPS: 0, 64.0 * EPS: 1, 128.0 * EPS: 2}
        for cval, cidx in ci.items():
            S.op(dve, lambda h, cval=cval, cidx=cidx: h.memset(epsb[:, cidx:cidx + 1], cval), writes=[CB])
        for tt in range(NTT):
            S.dma(sp, xT[:, :, tt * 512:(tt + 1) * 512],
                  xT_in.rearrange("(c p) n -> p c n", p=128)[:, :, tt * 512:(tt + 1) * 512], writes=[XB[tt]])
        S.dma(sp, g32[:], normsT, writes=[GB])
        S.dma(sp, qkgs[:], qkg, writes=[GB])
        S.dma(sp, dnrm[:], dnorm, writes=[GB])
        S.dma(sp, lamt[:], dlam, writes=[GB])
        S.op(dve, lambda h: h.tensor_scalar(out=g32[:], in0=g32[:], scalar1=32.0, scalar2=None, op0=ALU.mult),
             reads=[GB], writes=[GB])
        for l in range(L):
            for i in range(3):
                c = l * 6 + i * 2 + 1
                S.op(dve, lambda h, c=c: h.tensor_scalar(out=qkgs[:, c:c + 1], in0=qkgs[:, c:c + 1], scalar1=8.0,
                                                         scalar2=None, op0=ALU.mult), reads=[GB], writes=[GB])
        if has_b:
            ltmp = sb("ltmp", [128, 64], F32)
            lsum = sb("lsum", [128, 4], F32)
            LB = Buf()
            for l in range(L):
                lam_init = 0.8 - 0.6 * math.exp(-0.3 * (l + lbase))
                for k in range(2):
                    a0 = l * 256 + k * 128
                    S.op(dve, lambda h, a0=a0: h.tensor_tensor(out=ltmp[:], in0=lamt[:, a0:a0 + 64],
                                                               in1=lamt[:, a0 + 64:a0 + 128], op=ALU.mult),
                         reads=[GB], writes=[LB])
                    S.op(dve, lambda h, k=k: h.tensor_reduce(out=lsum[:, k:k + 1], in_=ltmp[:], axis=AX.X, op=ALU.add),
                         reads=[LB], writes=[LB])
                    S.op(act, lambda h, k=k: h.activation(out=lsum[:, 2 + k:3 + k], in_=lsum[:, k:k + 1], func=AF.Exp),
                         reads=[LB], writes=[LB])
                S.op(dve, lambda h, l=l, li=lam_init: h.scalar_tensor_tensor(
                    out=neglam[:, l:l + 1], in0=lsum[:, 3:4], scalar=-li, in1=lsum[:, 2:3], op0=ALU.add,
                    op1=ALU.subtract), reads=[LB], writes=[GB])
                S.op(dve, lambda h, l=l, li=lam_init: h.tensor_scalar(
                    out=dnrm[:, l:l + 1], in0=dnrm[:, l:l + 1], scalar1=(1.0 - li) * math.sqrt(128.0), scalar2=None,
                    op0=ALU.mult), reads=[GB], writes=[GB])

        bank_rr = {"i": 0}

        def mm(out, lhsT, rhs, start, stop, reads, writes):
            S.op(pe, lambda h: h.matmul(out, lhsT, rhs, start=start, stop=stop), reads=reads, writes=writes)

        def rsq(out, in_, c, reads, wbuf):
            S.op(act, lambda h: h.activation(out=out, in_=in_, func=AF.Sqrt, bias=epsb[0:out.shape[0], ci[c]:ci[c] + 1], scale=1.0),
                 reads=list(reads) + [CB], writes=[wbuf])
            S.op(dve, lambda h: h.reciprocal(out=out, in_=out), reads=[wbuf], writes=[wbuf])

        def emit_rms(gcol0, tgs, hT, HB, tok0, sq, SQB, rstd, RB, banks):
            for n, tg in enumerate(tgs):
                b = banks[n % len(banks)]
                tok = slice(tg * 512, tg * 512 + 512)
                lt = slice((tg - tok0) * 512, (tg - tok0) * 512 + 512)
                for c in range(8):
                    k = c % 2
                    S.op(act, lambda h, c=c, k=k: h.activation(out=sq[:, k, :], in_=xT[:, c, tok], func=AF.Square),
                         reads=[XB[tg]], writes=[SQB[k]])
                    mm(ps[:, b, :], ones, sq[:, k, :], c == 0, c == 7, [SQB[k], CB], [PB[b]])
                rsq(rstd[:], ps[:, b, :], 1024.0 * EPS, [PB[b]], RB)
                for c in range(8):
                    S.op(dve, lambda h, c=c: h.scalar_tensor_tensor(
                        out=hT[:, c, lt], in0=xT[:, c, tok], scalar=g32[:, gcol0 + c:gcol0 + c + 1], in1=rstd[:],
                        op0=ALU.mult, op1=ALU.mult), reads=[XB[tg], RB, GB], writes=[HB[tg - tok0]])

        def emit_ffn(l, which):
            with ExitStack() as fs:
                def fsb(name, shape, dt):
                    uid[0] += 1
                    return fs.enter_context(nc.sbuf_tensor(f"{name}_{uid[0]}", list(shape), dt))
                NH = 1024 if NT >= 1024 else NT
                ntt = NH // 512
                hT = fsb("f_hT", [128, 8, NH], BF16)
                aT = fsb("f_aT", [128, NF, NH], BF16)
                sq = fsb("f_sq", [128, 2, 512], BF16)
                rstd = fsb("f_rstd", [128, 512], F32)
                sg = fsb("f_sg", [128, 2, 512], F32)
                wg = fsb("f_wg", [128, 3, 8, 128], BF16)
                wu = fsb("f_wu", [128, 3, 8, 128], BF16)
                wd = fsb("f_wd", [128, 2, NF, 128], BF16)
                HB = [Buf() for _ in range(ntt)]
                AB = [[Buf() for _ in range(ntt)] for _ in range(NF)]
                SQB = [Buf(), Buf()]
                RB = Buf()
                SGB = [Buf(), Buf()]
                WGB = [Buf() for _ in range(3)]
                WUB = [Buf() for _ in range(3)]
                WDB = [Buf(), Buf()]
                wgv = w_gate[which][l].rearrange("(c p) n -> p c n", p=128)
                wuv = w_up[which][l].rearrange("(c p) n -> p c n", p=128)
                wdv = w_down[which][l].rearrange("(j p) n -> p j n", p=128)
                gcol0 = l * 24 + (0 if which == 0 else 16)
                nsg = 0
                for th in range(NT // NH):
                    tgs = [th * ntt + i for i in range(ntt)]
                    emit_rms(gcol0, tgs, hT, HB, th * ntt, sq, SQB, rstd, RB, [6, 7])
                    for j in range(NF):
                        k3 = j % 3
                        S.dma(pool, wg[:, k3], wgv[:, :, j * 128:(j + 1) * 128], writes=[WGB[k3]])
                        S.dma(pool, wu[:, k3], wuv[:, :, j * 128:(j + 1) * 128], writes=[WUB[k3]])
                        for tt in range(ntt):
                            bg = (j * ntt + tt) % 2
                            bu = 2 + (j * ntt + tt) % 2
                            tl = slice(tt * 512, tt * 512 + 512)
                            for c in range(8):
                                mm(ps[:, bg, :], wg[:, k3, c, :], hT[:, c, tl], c == 0, c == 7, [WGB[k3], HB[tt]], [PB[bg]])
                            for c in range(8):
                                mm(ps[:, bu, :], wu[:, k3, c, :], hT[:, c, tl], c == 0, c == 7, [WUB[k3], HB[tt]], [PB[bu]])
                            k = nsg % 2
                            nsg += 1
                            S.op(act, lambda h, k=k, bg=bg: h.activation(out=sg[:, k, :], in_=ps[:, bg, :], func=AF.Silu),
                                 reads=[PB[bg]], writes=[SGB[k]])
                            S.op(dve, lambda h, k=k, bu=bu, j=j, tl=tl: h.tensor_tensor(
                                out=aT[:, j, tl], in0=sg[:, k, :], in1=ps[:, bu, :], op=ALU.mult),
                                reads=[SGB[k], PB[bu]], writes=[AB[j][tt]])
                    for oc in range(8):
                        k2 = oc % 2
                        S.dma(pool, wd[:, k2], wdv[:, :, oc * 128:(oc + 1) * 128], writes=[WDB[k2]])
                        for tt in range(ntt):
                            bo = 4 + (oc * ntt + tt) % 2
                            tg = th * ntt + tt
                            tl = slice(tt * 512, tt * 512 + 512)
                            tok = slice(tg * 512, tg * 512 + 512)
                            for j in range(NF):
                                mm(ps[:, bo, :], wd[:, k2, j, :], aT[:, j, tl], j == 0, j == NF - 1,
                                   [WDB[k2], AB[j][tt]], [PB[bo]])
                            S.op(dve, lambda h, bo=bo, oc=oc, tok=tok: h.scalar_tensor_tensor(
                                out=xT[:, oc, tok], in0=ps[:, bo, :], scalar=0.5, in1=xT[:, oc, tok], op0=ALU.mult,
                                op1=ALU.add), reads=[PB[bo], XB[tg]], writes=[XB[tg]])
                S.barrier()

        def emit_pre(l):
            with ExitStack() as fs:
                def fsb(name, shape, dt):
                    uid[0] += 1
                    return fs.enter_context(nc.sbuf_tensor(f"{name}_{uid[0]}", list(shape), dt))
                hT = fsb("p_hT", [128, 8, NT], BF16)
                sq = fsb("p_sq", [128, 2, 512], BF16)
                rstd = fsb("p_rstd", [128, 512], F32)
                wt = fsb("p_wt", [128, 3, 8, 128], BF16)
                st = fsb("p_st", [128, 2, NT], BF16)
                wv = fsb("p_wv", [128, 8, VW + 8], BF16)
                vst = fsb("p_vst", [128, 2, VW], BF16)
                HB = [Buf() for _ in range(NTT)]
                SQB = [Buf(), Buf()]
                RB = Buf()
                WTB = [Buf() for _ in range(3)]
                STB = [Buf(), Buf()]
                WVB = Buf()
                VSB = [Buf(), Buf()]
                winv = w_in[l].rearrange("(c p) n -> p c n", p=128)
                emit_rms(l * 24 + 8, list(range(NTT)), hT, HB, 0, sq, SQB, rstd, RB, [6, 7])
                for tt in range(NTT):
                    S.dma(sp, hT_s.rearrange("(c p) n -> p c n", p=128)[:, :, tt * 512:(tt + 1) * 512],
                          hT[:, :, tt * 512:(tt + 1) * 512], reads=[HB[tt]])
                S.dma(pool, wv[:, :, 0:768], winv[:, :, 1536:2304], writes=[WVB])
                S.dma(pool, wv[:, :, 768:1280], winv[:, :, 3328:3840], writes=[WVB])
                S.dma(pool, wv[:, :, 1280:1344], winv[:, :, 4160:4224], writes=[WVB])
                S.dma(pool, wv[:, :, 1344:1352], winv[:, :, 4800:4808], writes=[WVB])
                qc = l * 6
                chunks = []
                for i in range(6):
                    chunks.append(("n", i * 128, None, qc + 0, QT_s, i))
                for i in range(6):
                    chunks.append(("n", 768 + i * 128, None, qc + 1, KT_l, i))
                for i in range(4):
                    chunks.append(("n", 2304 + i * 128, None, qc + 2, QT_s, 6 + i))
                for i in range(4):
                    chunks.append(("n", 2816 + i * 128, None, qc + 3, KT_l, 6 + i))
                for i in range(2):
                    chunks.append(("n", 3840 + i * 128, None, qc + 4, QT_s, 10 + i))
                chunks.append(("ck", 4096, 4736, qc + 5, KT_l, 10))
                for i in range(4):
                    chunks.append(("p", 4224 + i * 128, None, None, QT_s, 12 + i))
                for ci, (kind, c0, c1, gcol, dst, dchunk) in enumerate(chunks):
                    k3 = ci % 3
                    if kind == "ck":
                        S.dma(pool, wt[:, k3, :, 0:64], winv[:, :, c0:c0 + 64], writes=[WTB[k3]])
                        S.dma(pool, wt[:, k3, :, 64:128], winv[:, :, c1:c1 + 64], writes=[WTB[k3]])
                    else:
                        S.dma(pool, wt[:, k3], winv[:, :, c0:c0 + 128], writes=[WTB[k3]])
                    ks = ci % 2
                    for tt in range(NTT):
                        b = (ci * NTT + tt) % 2
                        tl = slice(tt * 512, tt * 512 + 512)
                        for c in range(8):
                            mm(ps[:, b, :], wt[:, k3, c, :], hT[:, c, tl], c == 0, c == 7, [WTB[k3], HB[tt]], [PB[b]])
                        if kind == "p":
                            S.op(act, lambda h, b=b, ks=ks, tl=tl: h.activation(out=st[:, ks, tl], in_=ps[:, b, :],
                                                                                func=AF.Copy, scale=0.125),
                                 reads=[PB[b]], writes=[STB[ks]])
                            continue
                        np_ = 64 if kind == "ck" else 128
                        kq = (ci * NTT + tt) % 2
                        b2 = 2 + (ci * NTT + tt) % 2
                        S.op(act, lambda h, b=b, kq=kq, np_=np_: h.activation(out=sq[0:np_, kq, :], in_=ps[0:np_, b, :],
                                                                               func=AF.Square),
                             reads=[PB[b]], writes=[SQB[kq]])
                        mm(ps[0:np_, b2, :], bones[0:np_, 0:np_], sq[0:np_, kq, :], True, True, [SQB[kq], CB], [PB[b2]])
                        rsq(rstd[0:np_, :], ps[0:np_, b2, :], 64.0 * EPS, [PB[b2]], RB)
                        S.op(dve, lambda h, b=b, ks=ks, tl=tl, np_=np_, gcol=gcol: h.scalar_tensor_tensor(
                            out=st[0:np_, ks, tl], in0=ps[0:np_, b, :], scalar=qkgs[0:np_, gcol:gcol + 1],
                            in1=rstd[0:np_, :], op0=ALU.mult, op1=ALU.mult), reads=[PB[b], RB, GB], writes=[STB[ks]])
                        if kind == "ck":
                            S.op(act, lambda h, b=b, ks=ks, tl=tl: h.activation(
                                out=st[64:128, ks, tl], in_=ps[64:128, b, :], func=AF.Copy), reads=[PB[b]],
                                writes=[STB[ks]])
                    S.dma(sp, dst[dchunk * 128:(dchunk + 1) * 128, :], st[:, ks, :], reads=[STB[ks]])
                for t128 in range(NT // 128):
                    tl = slice(t128 * 128, t128 * 128 + 128)
                    kv = t128 % 2
                    groups = [(0, 512), (512, 768), (768, 1280), (1280, 1352)]
                    for gi, (a0, a1) in enumerate(groups):
                        b = 4 + (t128 * 4 + gi) % 4
                        n = a1 - a0
                        for c in range(8):
                            mm(ps[:, b, 0:n], hT[:, c, tl], wv[:, c, a0:a1], c == 0, c == 7, [HB[t128 // 4], WVB], [PB[b]])
                        if gi < 3:
                            eng = act if gi % 2 == 0 else dve
                            if eng is act:
                                S.op(act, lambda h, b=b, n=n, a0=a0, a1=a1, kv=kv: h.activation(
                                    out=vst[:, kv, a0:a1], in_=ps[:, b, 0:n], func=AF.Copy), reads=[PB[b]], writes=[VSB[kv]])
                            else:
                                S.op(dve, lambda h, b=b, n=n, a0=a0, a1=a1, kv=kv: h.tensor_copy(
                                    out=vst[:, kv, a0:a1], in_=ps[:, b, 0:n]), reads=[PB[b]], writes=[VSB[kv]])
                        else:
                            S.op(act, lambda h, b=b, kv=kv: h.activation(
                                out=vst[:, kv, 1280:1344], in_=ps[:, b, 0:64], func=AF.Copy), reads=[PB[b]], writes=[VSB[kv]])
                            S.op(dve, lambda h, b=b, t128=t128: h.tensor_scalar(
                                out=widx[:, t128 * 8:t128 * 8 + 8], in0=ps[:, b, 64:72], scalar1=8.0 ** -0.5,
                                scalar2=None, op0=ALU.mult), reads=[PB[b]], writes=[WIB])
                    S.dma(sp, V_l[tl, :], vst[:, kv, :], reads=[VSB[kv]])
                if not fused:
                    S.dma(sp, widx_o, widx[:], reads=[WIB])
                S.barrier()

        def emit_vec():
            with ExitStack() as fs:
                def fsb(name, shape, dt):
                    uid[0] += 1
                    return fs.enter_context(nc.sbuf_tensor(f"{name}_{uid[0]}", list(shape), dt))
                tbl = fsb("v_tbl", [33, 20], F32)
                oh = fsb("v_oh", [33, 2, 512], F32)
                vs = fsb("v_vs", [20, 2, 512], BF16)
                TB = Buf()
                OB = [Buf(), Buf()]
                VB = [Buf(), Buf()]
                S.op(dve, lambda h: h.memset(tbl[:], -BIG), writes=[TB])
                S.dma(sp, tbl[0:32, :], rel_bias, writes=[TB])
                n = 0
                for kp in range(NKIND * 2):
                    for c0 in range(0, LV, 512):
                        cw = min(512, LV - c0)
                        k = n % 2
                        n += 1
                        S.dma(sp, oh[:, k, 0:cw], onehot[kp, :, c0:c0 + cw], writes=[OB[k]])
                        b = k
                        mm(ps[0:20, b, 0:cw], tbl[:], oh[:, k, 0:cw], True, True, [TB, OB[k]], [PB[b]])
                        S.op(act, lambda h, b=b, k=k, cw=cw: h.activation(out=vs[:, k, 0:cw], in_=ps[0:20, b, 0:cw],
                                                                          func=AF.Copy), reads=[PB[b]], writes=[VB[k]])
                        S.dma(sp, vec_s.rearrange("(k h) n -> k h n", h=20)[kp, :, c0:c0 + cw], vs[:, k, 0:cw],
                              reads=[VB[k]])
                S.barrier()

        def strip_src(kind, par, head, width):
            row = (kind * 2 + par) * 20 + head
            return bass.AP(tensor=vec_s.tensor, offset=row * LV, ap=[[1, 128], [1, width]])

        def kcol(kt):
            gs = kt // 4
            r, li = gpos(gs)
            return r * NT + li * 512 + (kt % 4) * 128

        def vtile(kt):
            gs = kt // 4
            r, li = gpos(gs)
            return li * 8 + r * 4 + kt % 4

        def emit_att(l, oT_a, oT_b, OAB, OBB):
            with ExitStack() as fs:
                def fsb(name, shape, dt):
                    uid[0] += 1
                    return fs.enter_context(nc.sbuf_tensor(f"{name}_{uid[0]}", list(shape), dt))
                KT = fsb("a_KT", [64, 2, 2 * NT], BF16)
                QTt = fsb("a_QT", [64, 2, NT], BF16)
                Vt = fsb("a_V", [128, 2 * NT // 128, 128], BF16)
                G = fsb("a_G", [128, 2, WD], BF16)
                Pt = fsb("a_P", [128, 3, 512], BF16)
                ev = fsb("a_ev", [128, 4, 512], F32)
                sqb = fsb("a_sq", [128, 512], BF16)
                KB = [Buf(), Buf()]
                QB = [Buf(), Buf()]
                VB = Buf()
                GBf = Buf()
                PtB = [Buf() for _ in range(3)]
                EB = [Buf() for _ in range(4)]
                SQ = Buf()
                KTf = KT_f.rearrange("(g r c p) n -> g c p r n", g=4, r=2, c=3, p=128)
                QTi = QT_si.rearrange("(c p) n -> c p n", p=128)
                Vf = V_f.rearrange("(t p) c -> p t c", p=128)
                npt = [0]

                def attend(m, terms, nmap, dv):
                    first = {0: True, 1: True}
                    total = {0: 0, 1: 0}
                    for (mp, ksl, qsl, kts, c0f) in terms:
                        total[mp] += len(kts)
                    cnt = {0: 0, 1: 0}
                    for (mp, ksl, qsl, kts, c0f) in terms:
                        for kt in kts:
                            k = npt[0] % 3
                            npt[0] += 1
                            b = k
                            kc = kcol(kt)
                            mm(ps[:, b, :], KT[:, ksl, kc:kc + 128], QTt[:, qsl, m * 512:(m + 1) * 512], True, False,
                               [KB[ksl], QB[qsl]], [PB[b]])
                            c0 = c0f(kt)
                            mm(ps[:, b, :], antiI, G[:, m % 2, c0:c0 + 512], False, True, [GBf, CB], [PB[b]])
                            S.op(act, lambda h, b=b, k=k: h.activation(out=Pt[:, k, :], in_=ps[:, b, :], func=AF.Exp),
                                 reads=[PB[b]], writes=[PtB[k]])
                            cnt[mp] += 1
                            st_, sp_ = cnt[mp] == 1, cnt[mp] == total[mp]
                            mm(ps[0:dv, 3 + mp, :], Vt[:, vtile(kt), 0:dv], Pt[:, k, :], st_, sp_, [VB, PtB[k]], [PB[3 + mp]])
                            mm(ps[0:dv, 5 + mp, :], ones[:, 0:dv], Pt[:, k, :], st_, sp_, [CB, PtB[k]], [PB[5 + mp]])

                for s in range(4):
                    for m in range(NM):
                        gsN = 2 * m + 1
                        terms = []
                        first = True
                        ngrp = 0
                        kt_lists = []
                        for g, (w, d) in enumerate(DIL):
                            kts = [kt for kt in range((gsN + 1) * 4)
                                   if -384 <= gsN * 512 - 128 * kt <= w + 639]
                            kt_lists.append(kts)
                        tot = sum(len(k) for k in kt_lists)
                        cnt = 0
                        for g, (w, d) in enumerate(DIL):
                            head = 4 * g + s
                            ch, hf_ = head // 2, head % 2
                            sl = (s * 3 * NM + m * 3 + g) % 2
                            S.dma(sp, KT[:, sl, :].rearrange("p (r n) -> p r n", r=2), KTf[ch // 3, ch % 3, hf_ * 64:hf_ * 64 + 64],
                                  writes=[KB[sl]])
                            S.dma(sp, QTt[:, sl, :], QTi[ch, hf_ * 64:hf_ * 64 + 64, :], writes=[QB[sl]])
                            if m == 0 or True:
                                S.dma(sp, Vt[:, :, 0:64], Vf[:, :, head * 64:head * 64 + 64], writes=[VB])
                            wdt = min(WD, w + 639 + 384 + 512 + 128)
                            S.dma(sp, G[:, m % 2, 0:wdt], strip_src(g, m % 2, head, wdt), writes=[GBf])
                            pend = None
                            for kt in kt_lists[g]:
                                k = npt[0] % 3
                                npt[0] += 1
                                b = k
                                kc = kcol(kt)
                                mm(ps[:, b, :], KT[:, sl, kc:kc + 128], QTt[:, sl, m * 512:(m + 1) * 512], True, False,
                                   [KB[sl], QB[sl]], [PB[b]])
                                c0 = gsN * 512 - 128 * kt + 384
                                mm(ps[:, b, :], antiI, G[:, m % 2, c0:c0 + 512], False, True, [GBf, CB], [PB[b]])
                                S.op(act, lambda h, b=b, k=k: h.activation(out=Pt[:, k, :], in_=ps[:, b, :], func=AF.Exp),
                                     reads=[PB[b]], writes=[PtB[k]])
                                if pend is not None:
                                    pend()
                                cnt += 1

                                def pend(kt=kt, k=k, st_=(cnt == 1), sp_=(cnt == tot)):
                                    mm(ps[0:64, 3, :], Vt[:, vtile(kt), 0:64], Pt[:, k, :], st_, sp_, [VB, PtB[k]], [PB[3]])
                                    mm(ps[0:64, 5, :], ones[:, 0:64], Pt[:, k, :], st_, sp_, [CB, PtB[k]], [PB[5]])
                            if pend is not None:
                                pend()
                        S.op(dve, lambda h: h.reciprocal(out=ev[0:64, 0, :], in_=ps[0:64, 5, :]), reads=[PB[5]], writes=[EB[0]])
                        S.op(dve, lambda h, s=s, m=m: h.tensor_tensor(out=oT_a[:, s, m * 512:(m + 1) * 512],
                                                                       in0=ps[0:64, 3, :], in1=ev[0:64, 0, :], op=ALU.mult),
                             reads=[PB[3], EB[0]], writes=[OAB])
                for hb in range(4):
                    ch, hf_ = hb // 2, hb % 2
                    for mp in range(2):
                        S.dma(sp, KT[:, mp, :].rearrange("p (r n) -> p r n", r=2),
                              KTf[(6 + 2 * mp + ch) // 3, (6 + 2 * mp + ch) % 3, hf_ * 64:hf_ * 64 + 64], writes=[KB[mp]])
                        S.dma(sp, QTt[:, mp, :], QTi[6 + 2 * mp + ch, hf_ * 64:hf_ * 64 + 64, :], writes=[QB[mp]])
                    S.dma(sp, Vt[:], Vf[:, :, 768 + hb * 128:768 + hb * 128 + 128], writes=[VB])
                    for par in range(2):
                        S.dma(sp, G[:, par, :], strip_src(3, par, 12 + hb, WD), writes=[GBf])
                    for m in range(NM):
                        gsN = 2 * m + 1
                        nkt = (gsN + 1) * 4
                        pend = None
                        for kt in range(nkt):
                            D0 = gsN * 512 - 128 * kt
                            c0 = D0 + 384 if D0 < 2176 else 2560
                            kc = kcol(kt)
                            for mp in range(2):
                                k = npt[0] % 3
                                npt[0] += 1
                                b = k
                                mm(ps[:, b, :], KT[:, mp, kc:kc + 128], QTt[:, mp, m * 512:(m + 1) * 512], True, False,
                                   [KB[mp], QB[mp]], [PB[b]])
                                mm(ps[:, b, :], antiI, G[:, m % 2, c0:c0 + 512], False, True, [GBf, CB], [PB[b]])
                                S.op(act, lambda h, b=b, k=k: h.activation(out=Pt[:, k, :], in_=ps[:, b, :], func=AF.Exp),
                                     reads=[PB[b]], writes=[PtB[k]])
                                if pend is not None:
                                    pend()

                                def pend(kt=kt, k=k, mp=mp, nkt=nkt):
                                    mm(ps[:, 3 + mp, :], Vt[:, vtile(kt), :], Pt[:, k, :], kt == 0, kt == nkt - 1,
                                       [VB, PtB[k]], [PB[3 + mp]])
                                    mm(ps[:, 5 + mp, :], ones, Pt[:, k, :], kt == 0, kt == nkt - 1, [CB, PtB[k]], [PB[5 + mp]])
                        if pend is not None:
                            pend()
                        for mp in range(2):
                            S.op(dve, lambda h, mp=mp: h.reciprocal(out=ev[:, mp, :], in_=ps[:, 5 + mp, :]),
                                 reads=[PB[5 + mp]], writes=[EB[mp]])
                            S.op(dve, lambda h, mp=mp: h.tensor_tensor(out=ev[:, mp, :], in0=ps[:, 3 + mp, :],
                                                                       in1=ev[:, mp, :], op=ALU.mult),
                                 reads=[PB[3 + mp], EB[mp]], writes=[EB[mp]])
                        S.op(dve, lambda h: h.scalar_tensor_tensor(out=ev[:, 2, :], in0=ev[:, 1, :],
                                                                   scalar=neglam[:, l:l + 1], in1=ev[:, 0, :],
                                                                   op0=ALU.mult, op1=ALU.add),
                             reads=[EB[0], EB[1], GB], writes=[EB[2]])
                        S.op(act, lambda h: h.activation(out=sqb[:], in_=ev[:, 2, :], func=AF.Square), reads=[EB[2]],
                             writes=[SQ])
                        mm(ps[:, 7, :], ones, sqb[:], True, True, [SQ, CB], [PB[7]])
                        rsq(ev[:, 3, :], ps[:, 7, :], 128.0 * EPS, [PB[7]], EB[3])
                        S.op(dve, lambda h, hb=hb, m=m: h.scalar_tensor_tensor(
                            out=oT_b[:, hb, m * 512:(m + 1) * 512], in0=ev[:, 2, :], scalar=dnrm[:, l:l + 1],
                            in1=ev[:, 3, :], op0=ALU.mult, op1=ALU.mult), reads=[EB[2], EB[3], GB], writes=[OBB])
                S.barrier()

        WC = 3072

        def emit_dsa(l, oT_c, OCB):
            with ExitStack() as fs:
                def fsb(name, shape, dt):
                    uid[0] += 1
                    return fs.enter_context(nc.sbuf_tensor(f"{name}_{uid[0]}", list(shape), dt))
                KK = fsb("c_KK", [128, 2 * NT], BF16)
                QQ = fsb("c_QQ", [128, 8, 512], BF16)
                Vc = fsb("c_V", [128, 2 * NT // 128, 64], BF16)
                G = fsb("c_G", [128, 4, WC], BF16)
                cmb = fsb("c_cmb", [128, 2, 1024], BF16)
                sc = fsb("c_sc", [128, 2, T], F32)
                rlb = fsb("c_rl", [128, 3, 512], BF16)
                dg = fsb("c_dg", [128, 2, 8, 128], BF16)
                mx = fsb("c_mx", [128, 8], F32)
                mneg = fsb("c_mneg", [128, T], BF16)
                Pt = fsb("c_P", [128, 2, 512], BF16)
                ev = fsb("c_ev", [64, 512], F32)
                B_ld = Buf()
                QLB = Buf()
                GLB = Buf()
                SCB = [Buf(), Buf()]
                RLB = [Buf(), Buf(), Buf()]
                DGB = [Buf(), Buf()]
                MXB = Buf()
                MNB = Buf()
                PtB = [Buf(), Buf()]
                EVB = Buf()
                KTf = KT_f.rearrange("(g r c p) n -> g c p r n", g=4, r=2, c=3, p=128)
                QTi = QT_si.rearrange("(c p) n -> c p n", p=128)
                Vf = V_f.rearrange("(t p) c -> p t c", p=128)
                S.dma(sp, KK[:, :].rearrange("p (r n) -> p r n", r=2), KTf[3, 1], writes=[B_ld])
                S.dma(sp, Vc[:], Vf[:, :, 1280:1344], writes=[B_ld])
                S.dma(pool, cmb[:], cm_in.rearrange("k p n -> p k n"), writes=[B_ld])
                nq = 0
                nr = 0
                nacc = 0
                for m in range(NM):
                    gsN = 2 * m + 1
                    nkeys = (gsN + 1) * 512
                    nch = nkeys // 512
                    ms = slice(m * 512, m * 512 + 512)
                    for i in range(8):
                        S.dma(sp, QQ[64:128, i, :], QTi[12 + i // 2, (i % 2) * 64:(i % 2) * 64 + 64, ms], writes=[QLB])
                    for i in range(4):
                        S.dma(sp, QQ[0:64, i, :], QTi[10 + i // 2, (i % 2) * 64:(i % 2) * 64 + 64, ms], writes=[QLB])
                        S.dma(sp, G[:, i, :], strip_src(3, m % 2, 16 + i, WC), writes=[GLB])
                    for qb in range(4):
                        ql = slice(qb * 128, qb * 128 + 128)
                        q0 = m * 512 + qb * 128
                        t128 = q0 // 128
                        ks = nq % 2
                        nq += 1
                        for hh in range(8):
                            wcol = widx[:, t128 * 8 + hh:t128 * 8 + hh + 1]
                            S.op(pool, lambda h, ks=ks, hh=hh, wcol=wcol: h.tensor_scalar(
                                out=dg[:, ks, hh, :], in0=ident, scalar1=wcol, scalar2=None, op0=ALU.mult),
                                reads=[CB, WIB], writes=[DGB[ks]])
                        for kc in range(nch):
                            r, li = gpos(kc)
                            col = r * NT + li * 512
                            ab = 6 + nacc % 2
                            nacc += 1
                            pend = None
                            for hh in range(8):
                                b = nr % 2
                                k = nr % 3
                                nr += 1
                                mm(ps[:, b, :], QQ[64:128, hh, ql], KK[64:128, col:col + 512],
                                   True, True, [B_ld, QLB], [PB[b]])
                                S.op(act, lambda h, b=b, k=k: h.activation(out=rlb[:, k, :], in_=ps[:, b, :], func=AF.Relu),
                                     reads=[PB[b]], writes=[RLB[k]])
                                if pend is not None:
                                    pend()

                                def pend(hh=hh, k=k, ab=ab, ks=ks):
                                    mm(ps[:, ab, :], dg[:, ks, hh, :], rlb[:, k, :], hh == 0, hh == 7, [DGB[ks], RLB[k]], [PB[ab]])
                            pend()
                            S.op(act, lambda h, ab=ab, ks=ks, kc=kc: h.activation(
                                out=sc[:, ks, kc * 512:(kc + 1) * 512], in_=ps[:, ab, :], func=AF.Copy),
                                reads=[PB[ab]], writes=[SCB[ks]])
                        lo = gsN * 512 + 128 * qb - 512
                        nke = gsN * 512 + 128 * (qb + 1)
                        hi = nke
                        S.op(dve, lambda h, ks=ks, lo=lo, hi=hi, m=m: h.tensor_tensor(
                            out=sc[:, ks, lo:hi], in0=sc[:, ks, lo:hi], in1=cmb[:, m % 2, 0:hi - lo], op=ALU.add),
                            reads=[SCB[ks], B_ld], writes=[SCB[ks]])
                        for it in range(32):
                            S.op(dve, lambda h, ks=ks: h.max(out=mx[:], in_=sc[:, ks, 0:nke]), reads=[SCB[ks]], writes=[MXB])
                            S.op(dve, lambda h, ks=ks: h.match_replace(out=sc[:, ks, 0:nke], in_to_replace=mx[:],
                                                                       in_values=sc[:, ks, 0:nke], imm_value=-3.0e38),
                                 reads=[SCB[ks], MXB], writes=[SCB[ks]])
                        S.op(dve, lambda h, ks=ks: h.tensor_scalar(out=mneg[:, 0:nke], in0=sc[:, ks, 0:nke],
                                                                   scalar1=-1.0e38, scalar2=-BIG, op0=ALU.is_gt,
                                                                   op1=ALU.mult), reads=[SCB[ks]], writes=[MNB])
                        S.op(dve, lambda h, lo=lo, hi=hi, m=m: h.tensor_tensor(
                            out=mneg[:, lo:hi], in0=mneg[:, lo:hi], in1=cmb[:, m % 2, 0:hi - lo], op=ALU.add),
                            reads=[MNB, B_ld], writes=[MNB])
                        nkt = nke // 128
                        pendc = None
                        for kt in range(nkt):
                            kcg = kcol(kt)
                            D0 = gsN * 512 - 128 * kt
                            c0 = (D0 + 384 if D0 < 2176 else 2560) + 128 * qb
                            b = 2 + kt % 2
                            k = kt % 2
                            for hc in range(4):
                                o = ps[:, b, hc * 128:(hc + 1) * 128]
                                mm(o, KK[0:64, kcg:kcg + 128], QQ[0:64, hc, ql], True, False, [B_ld, QLB], [PB[b]])
                                mm(o, antiI, G[:, hc, c0:c0 + 128], False, False, [GLB, CB], [PB[b]])
                                mm(o, mneg[:, kt * 128:(kt + 1) * 128], ident, False, True, [MNB, CB], [PB[b]])
                            S.op(act, lambda h, b=b, k=k: h.activation(out=Pt[:, k, :], in_=ps[:, b, :], func=AF.Exp),
                                 reads=[PB[b]], writes=[PtB[k]])
                            if pendc is not None:
                                pendc()

                            def pendc(kt=kt, k=k, nkt=nkt):
                                for hc in range(4):
                                    mm(ps[0:64, 4, hc * 128:(hc + 1) * 128], Vc[:, vtile(kt), :],
                                       Pt[:, k, hc * 128:(hc + 1) * 128], kt == 0 and hc == 0, kt == nkt - 1,
                                       [B_ld, PtB[k]], [PB[4]])
                                mm(ps[0:64, 5, :], ones[:, 0:64], Pt[:, k, :], kt == 0, kt == nkt - 1, [CB, PtB[k]], [PB[5]])
                        pendc()
                        S.op(dve, lambda h: h.reciprocal(out=ev[:], in_=ps[0:64, 5, :]), reads=[PB[5]], writes=[EVB])
                        for hc in range(4):
                            S.op(dve, lambda h, hc=hc, q0=q0: h.tensor_tensor(
                                out=oT_c[:, hc, q0:q0 + 128], in0=ps[0:64, 4, hc * 128:(hc + 1) * 128],
                                in1=ev[:, hc * 128:(hc + 1) * 128], op=ALU.mult), reads=[PB[4], EVB], writes=[OCB])
                S.barrier()

        def emit_post(l, oT_a, oT_b, oT_c, OAB, OBB, OCB):
            with ExitStack() as fs:
                def fsb(name, shape, dt):
                    uid[0] += 1
                    return fs.enter_context(nc.sbuf_tensor(f"{name}_{uid[0]}", list(shape), dt))
                hT = fsb("o_hT", [128, 1, 8, 512], BF16)
                yT = fsb("o_yT", [128, 8, 512], BF16)
                wga = fsb("o_wg", [128, 3, 8, 128], BF16)
                wa = fsb("o_wa", [64, 4, D], BF16)
                wb = fsb("o_wb", [128, 4, D], BF16)
                wc = fsb("o_wc", [64, 4, D], BF16)
                wo = fsb("o_wo", [128, 8, D], BF16)
                sg = fsb("o_sg", [128, 2, 512], F32)
                tmp = fsb("o_tmp", [128, 2, 512], F32)
                yacc = fsb("o_yacc", [128, 512], F32)
                HB = [Buf(), Buf()]
                YB = Buf()
                WGB = [Buf() for _ in range(3)]
                WB = Buf()
                SGB = [Buf(), Buf()]
                TMB = [Buf(), Buf()]
                YAB = Buf()
                winv = w_in[l].rearrange("(c p) n -> p c n", p=128)
                S.dma(pool, wa[:], w_br[0][l].rearrange("(h p) n -> p h n", p=64), writes=[WB])
                S.dma(pool, wb[:], w_br[1][l].rearrange("(h p) n -> p h n", p=128), writes=[WB])
                S.dma(pool, wc[:], w_br[2][l].rearrange("(h p) n -> p h n", p=64), writes=[WB])
                S.dma(pool, wo[:], w_out[l].rearrange("(c p) n -> p c n", p=128), writes=[WB])
                nw = 0
                ns = 0
                for tt in range(NTT):
                    tok = slice(tt * 512, tt * 512 + 512)
                    kh = 0
                    S.dma(sp, hT[:, kh], hT_si.rearrange("(c p) n -> p c n", p=128)[:, :, tok], writes=[HB[kh]])
                    for oc in range(8):
                        for i in range(3):
                            k3 = nw % 3
                            nw += 1
                            g0 = 4808 + i * 1024 + oc * 128
                            S.dma(pool, wga[:, k3], winv[:, :, g0:g0 + 128], writes=[WGB[k3]])
                            bg = (oc * 3 + i) % 2
                            bb = 2 + (oc * 3 + i) % 2
                            for c in range(8):
                                mm(ps[:, bg, :], wga[:, k3, c, :], hT[:, kh, c, :], c == 0, c == 7, [WGB[k3], HB[kh]], [PB[bg]])
                            ocs = slice(oc * 128, oc * 128 + 128)
                            if i == 0:
                                for hh in range(4):
                                    mm(ps[:, bb, :], wa[:, hh, ocs], oT_a[:, hh, tok], hh == 0, hh == 3, [WB, OAB], [PB[bb]])
                            elif i == 1:
                                for hh in range(4):
                                    mm(ps[:, bb, :], wb[:, hh, ocs], oT_b[:, hh, tok], hh == 0, hh == 3, [WB, OBB], [PB[bb]])
                            else:
                                for hh in range(4):
                                    mm(ps[:, bb, :], wc[:, hh, ocs], oT_c[:, hh, tok], hh == 0, hh == 3, [WB, OCB], [PB[bb]])
                            k = ns % 2
                            ns += 1
                            S.op(act, lambda h, k=k, bg=bg: h.activation(out=sg[:, k, :], in_=ps[:, bg, :], func=AF.Sigmoid),
                                 reads=[PB[bg]], writes=[SGB[k]])
                            if i == 0:
                                S.op(dve, lambda h, k=k, bb=bb: h.tensor_tensor(out=yacc[:], in0=sg[:, k, :], in1=ps[:, bb, :],
                                                                               op=ALU.mult), reads=[SGB[k], PB[bb]], writes=[YAB])
                            else:
                                S.op(dve, lambda h, k=k, bb=bb: h.tensor_tensor(out=tmp[:, k, :], in0=sg[:, k, :],
                                                                               in1=ps[:, bb, :], op=ALU.mult),
                                     reads=[SGB[k], PB[bb]], writes=[TMB[k]])
                                if i == 1:
                                    S.op(pool, lambda h, k=k: h.tensor_tensor(out=yacc[:], in0=yacc[:], in1=tmp[:, k, :],
                                                                             op=ALU.add), reads=[YAB, TMB[k]], writes=[YAB])
                                else:
                                    S.op(pool, lambda h, k=k, oc=oc: h.tensor_tensor(out=yT[:, oc, :], in0=yacc[:],
                                                                                    in1=tmp[:, k, :], op=ALU.add),
                                         reads=[YAB, TMB[k]], writes=[YB])
                    for oc in range(8):
                        bo = 4 + oc % 2
                        for c in range(8):
                            mm(ps[:, bo, :], wo[:, c, oc * 128:(oc + 1) * 128], yT[:, c, :], c == 0, c == 7, [WB, YB], [PB[bo]])
                        S.op(dve, lambda h, bo=bo, oc=oc, tok=tok: h.tensor_tensor(out=xT[:, oc, tok], in0=ps[:, bo, :],
                                                                                  in1=xT[:, oc, tok], op=ALU.add),
                             reads=[PB[bo], XB[tt]], writes=[XB[tt]])
                S.barrier()

        if fused:
            with ExitStack() as zs:
                zt = zs.enter_context(nc.sbuf_tensor("zpad", [128, NT], BF16))
                ZB = Buf()
                S.op(dve, lambda h: h.memset(zt[:], 0.0), writes=[ZB])
                for i in range(2):
                    S.dma(sp, KT_l2[i][11 * 128:12 * 128, :], zt[:], reads=[ZB])
                S.barrier()
        S.barrier()
        if has_b:
            emit_vec()
        for (ph, l) in parts:
            if fused:
                KT_l, V_l, KT_f, V_f = KT_l2[l % 2], V_l2[l % 2], KT_f2[l % 2], V_f2[l % 2]
            if ph == "a":
                emit_ffn(l, 0)
                emit_pre(l)
                if fused:
                    S.drain(pool)
                    groups = [[2 * i, 2 * i + 1] for i in range(ncores // 2)]
                    pieces = [(KT_l[g * 384:(g + 1) * 384, :], KT_f[g * 768:(g + 1) * 768, :]) for g in range(4)]
                    pieces += [(V_l[g * 512:(g + 1) * 512, :], V_f[g * 1024:(g + 1) * 1024, :]) for g in range(NM)]
                    for (src_, dst_) in pieces:
                        ins = nc.gpsimd.collective_compute("AllGather", ALU.bypass, replica_groups=groups,
                                                           ins=[src_.opt()], outs=[dst_.opt()])
                        S.cc_val += 1
                        ins.then_inc(S.cc_sem)
                    S.barrier()
            elif ph == "b":
                if not fused and (ph, l) == parts[0]:
                    S.dma(sp, widx[:], widx_i, writes=[WIB])
                with ExitStack() as bs:
                    oT_c = bs.enter_context(nc.sbuf_tensor(f"oT_c{l}", [64, 4, NT], BF16))
                    OAB, OBB, OCB = Buf(), Buf(), Buf()
                    if "c" in MIX:
                        emit_dsa(l, oT_c, OCB)
                    oT_a = bs.enter_context(nc.sbuf_tensor(f"oT_a{l}", [64, 4, NT], BF16))
                    oT_b = bs.enter_context(nc.sbuf_tensor(f"oT_b{l}", [128, 4, NT], BF16))
                    emit_att(l, oT_a, oT_b, OAB, OBB)
                    emit_post(l, oT_a, oT_b, oT_c, OAB, OBB, OCB)
                emit_ffn(l, 1)
        for tt in range(NTT):
            S.dma(sp, xT_out.rearrange("(c p) n -> p c n", p=128)[:, :, tt * 512:(tt + 1) * 512],
                  xT[:, :, tt * 512:(tt + 1) * 512], reads=[XB[tt]])
        S.barrier()
    nc._n_ins = S.n_ins
    return nc


def rel_bucket_np(dist):
    n = np.maximum(dist, 0)
    nf = np.maximum(n, 1).astype(np.float32)
    large = 16 + (np.log(nf / np.float32(16)) / np.float32(math.log(2048 / 16)) * np.float32(16)).astype(np.int32)
    large = np.minimum(large, 31)
    return np.where(n < 16, n, large)


def make_onehot(e_par):
    oh = np.zeros((NKIND * 2, 33, LV), np.float32)
    v = np.arange(LV)
    for kind in range(NKIND):
        for p in range(2):
            dist = v - VOFF - 512 * e_par[p]
            if kind < 3:
                w, d = DIL[kind]
                valid = (dist >= 0) & (dist <= w) & (dist % d == 0)
            else:
                valid = dist >= 0
            bk = rel_bucket_np(dist)
            o = oh[kind * 2 + p]
            o[bk[valid], v[valid]] = 1.0
            o[32, v[~valid]] = 1.0
    return oh


def make_cm(e_par):
    cm = np.zeros((2, 128, 1024), np.float32)
    u = np.arange(1024)[None, :]
    qi = np.arange(128)[:, None]
    for p in range(2):
        cm[p] = np.where(u - 512 + 512 * e_par[p] <= qi, 0.0, -BIG)
    return cm


def local_superblocks(hf, ns):
    return [gs for gs in range(ns) if (gs % 4 in (0, 3)) == (hf == 0)]


_PROG = {}


def _get_prog(T, L, parts, fused, lbase=0, ncores=8):
    key = (T, L, tuple(parts), fused, lbase, ncores)
    if key not in _PROG:
        _PROG[key] = build(T, L, list(parts), fused, lbase, ncores)
    return _PROG[key]


A_KEYS = ("ffn1_w_gate", "ffn1_w_up", "ffn1_w_down", "w_in")
B_KEYS = ("ffn2_w_gate", "ffn2_w_up", "ffn2_w_down", "w_in", "w_branch_a", "w_branch_b", "w_branch_c", "w_out")


def run_model(inputs, T, L, B, fused=True):
    x = np.asarray(inputs["x"], np.float32)
    NT = T // 2
    NS = T // 512
    ncores = 2 * B
    consts = np.zeros((4, 128, 128), np.float32)
    consts[0] = np.eye(128)
    consts[1] = np.eye(128)[::-1]
    consts[2] = 1.0
    consts[3, :64, :64] = 1.0
    consts[3, 64:, 64:] = 1.0
    norms = np.stack([inputs["ffn1_norm"], inputs["mix_norm"], inputs["ffn2_norm"]], 1)
    normsT = np.ascontiguousarray(norms.reshape(L, 3, 8, 128).transpose(3, 0, 1, 2).reshape(128, L * 24)).astype(np.float32)
    qg = np.asarray(inputs["qk_gain"], np.float32).reshape(L * 6, 64)
    qkg = np.ascontiguousarray(np.concatenate([qg, qg], 1).T)
    dnorm = np.ascontiguousarray(np.asarray(inputs["diff_out_norm"], np.float32).T)
    dlam = np.ascontiguousarray(np.broadcast_to(np.asarray(inputs["diff_lambda"], np.float32).reshape(1, L * 256), (128, L * 256)))
    shared = {"consts": consts, "normsT": normsT, "qkg": qkg, "dnorm": dnorm, "dlam": dlam,
              "rel_bias": np.asarray(inputs["rel_bias"], np.float32)}
    for k in ("ffn1_w_gate", "ffn1_w_up", "ffn1_w_down", "ffn2_w_gate", "ffn2_w_up", "ffn2_w_down", "w_in",
              "w_branch_a", "w_branch_b", "w_branch_c", "w_out"):
        shared[k] = np.asarray(inputs[k], np.float32)
    percore = []
    for c in range(ncores):
        b, hf = c // 2, c % 2
        sbs = local_superblocks(hf, NS)
        xs = np.concatenate([x[b, gs * 512:(gs + 1) * 512] for gs in sbs], 0)
        e_par = [1, 0] if hf == 0 else [0, 1]
        percore.append({"xT_in": np.ascontiguousarray(xs.T), "onehot": make_onehot(e_par), "cm": make_cm(e_par)})
    cores = list(range(ncores))
    if fused:
        parts = []
        for l in range(L):
            parts += [("a", l), ("b", l)]
        nc = _get_prog(T, L, parts, True, 0, ncores)
        res = run_bass_kernel_spmd(nc, [dict(shared, **pc) for pc in percore], core_ids=cores)
        outs = [r["xT_out"] for r in res.results]
    else:
        state = [pc["xT_in"] for pc in percore]
        small = {k: shared[k] for k in ("consts",)}
        for l in range(L):
            sm = dict(small)
            sm["normsT"] = np.ascontiguousarray(normsT[:, l * 24:(l + 1) * 24])
            sm["qkg"] = np.ascontiguousarray(qkg[:, l * 6:(l + 1) * 6])
            sm["dnorm"] = np.ascontiguousarray(dnorm[:, l:l + 1])
            sm["dlam"] = np.ascontiguousarray(dlam[:, l * 256:(l + 1) * 256])
            sa = dict(sm)
            for k in A_KEYS:
                sa[k] = shared[k][l:l + 1]
            nca = _get_prog(T, 1, [("a", 0)], False, 0)
            res = run_bass_kernel_spmd(nca, [dict(sa, xT_in=state[i]) for i in range(ncores)], core_ids=cores)
            ra = res.results
            sbm = dict(sm)
            for k in B_KEYS:
                sbm[k] = shared[k][l:l + 1]
            sbm["rel_bias"] = shared["rel_bias"]
            ncb = _get_prog(T, 1, [("b", 0)], False, l)
            maps = []
            for i, pc in enumerate(percore):
                p0 = (i // 2) * 2
                m = dict(sbm, onehot=pc["onehot"], cm=pc["cm"])
                m["xT_in"] = ra[i]["xT_out"]
                m["hT_i"] = ra[i]["hT_o"]
                m["QT_i"] = ra[i]["QT_o"]
                m["widx_i"] = ra[i]["widx_o"]
                k0, k1 = np.asarray(ra[p0]["KT_o"]), np.asarray(ra[p0 + 1]["KT_o"])
                m["KT_f"] = np.concatenate([np.concatenate([k0[g * 384:(g + 1) * 384], k1[g * 384:(g + 1) * 384]], 0)
                                            for g in range(4)], 0)
                v0, v1 = np.asarray(ra[p0]["V_o"]), np.asarray(ra[p0 + 1]["V_o"])
                m["V_f"] = np.concatenate([np.concatenate([v0[g * 512:(g + 1) * 512], v1[g * 512:(g + 1) * 512]], 0)
                                           for g in range(NT // 512)], 0)
                maps.append(m)
            res = run_bass_kernel_spmd(ncb, maps, core_ids=cores)
            state = [r["xT_out"] for r in res.results]
        outs = state
    out = np.zeros((B, T, D), np.float32)
    for c in range(ncores):
        b, hf = c // 2, c % 2
        sbs = local_superblocks(hf, NS)
        xo = np.asarray(outs[c]).T
        for i, gs in enumerate(sbs):
            out[b, gs * 512:(gs + 1) * 512] = xo[i * 512:(i + 1) * 512]
    return out


FUSED = True


def kernel(**inputs):
    x = np.asarray(inputs["x"])
    B, T, _ = x.shape
    L = np.asarray(inputs["w_in"]).shape[0]
    return run_model(inputs, T, L, B, fused=FUSED)
```
